# Optimizing a Trainium2 kernel written in Bass

```python
import math
import jax
import jax.numpy as jnp
from jax import lax
import numpy as np

D_MODEL = 2048
BATCH = 4
SEQ = 8192
DEPTH = 1

PLE_DIM = 256
HG_HEADS = 8
HG_HEAD_K = 128
HG_HEAD_V = 128
HG_WIDTH = HG_HEADS * HG_HEAD_K
SC_GROUPS = 8
SC_GROUP_DIM = 128
SC_WIDTH = SC_GROUPS * SC_GROUP_DIM
CONV_WIDTH = 3
MIX_WIDTH = HG_WIDTH + SC_WIDTH
IN_COLS = 4 * HG_WIDTH + 3 * SC_WIDTH
CHUNK = 64
N_GROUPS = 4
EXPERTS_PER_GROUP = 8
N_EXPERTS = N_GROUPS * EXPERTS_PER_GROUP
TOP_K = 2
EXPERT_FF = 512
BLK = 256
EPS = 1e-6

kernel_name = "hymba_hgrn2_shortconv_hmoe_ple"


def rms_norm(x, gain):
    xf = x.astype(jnp.float32)
    xf = xf * lax.rsqrt(jnp.mean(xf * xf, axis=-1, keepdims=True) + EPS)
    return (xf * gain.astype(jnp.float32)).astype(x.dtype)


def _to_chunks(t, d):
    b, T, _ = t.shape
    return t.reshape(b, T // CHUNK, CHUNK, HG_HEADS, d).transpose(1, 0, 3, 2, 4)


def hgrn2_group(q_raw, f_raw, i_raw, g_raw, lb, norm_gain):
    dt = q_raw.dtype
    b, T, _ = q_raw.shape
    f32 = jnp.float32
    q = jax.nn.silu(q_raw.astype(f32)) * (HG_HEAD_K ** -0.5)
    fz = f_raw.astype(f32)
    lb = lb.astype(f32)
    log_f = jnp.logaddexp(jnp.log(lb), jnp.log1p(-lb) + jax.nn.log_sigmoid(fz))
    k = (1.0 - lb) * jax.nn.sigmoid(-fz)
    v = i_raw.astype(f32)

    qc, kc, vc, gc = (_to_chunks(q, HG_HEAD_K), _to_chunks(k, HG_HEAD_K),
                      _to_chunks(v, HG_HEAD_V), _to_chunks(log_f, HG_HEAD_K))
    causal = jnp.tril(jnp.ones((CHUNK, CHUNK), dtype=bool))

    def step(S, inp):
        qb, kb, vb, gb = inp
        A = jnp.cumsum(gb, axis=2)
        o_inter = jnp.einsum('bhtk,bhkv->bhtv', qb * jnp.exp(A), S)
        diff = A[:, :, :, None, :] - A[:, :, None, :, :]
        decay = jnp.exp(jnp.where(causal[:, :, None], diff, -jnp.inf))
        scores = jnp.einsum('bhtk,bhsk,bhtsk->bhts', qb, kb, decay)
        o_intra = jnp.einsum('bhts,bhsv->bhtv', scores, vb)
        A_last = A[:, :, -1:, :]
        S_new = (jnp.exp(A_last[:, :, 0, :])[..., None] * S
                 + jnp.einsum('bhsk,bhsv->bhkv', kb * jnp.exp(A_last - A), vb))
        return S_new, o_inter + o_intra

    S0 = jnp.zeros((b, HG_HEADS, HG_HEAD_K, HG_HEAD_V), f32)
    _, o = lax.scan(step, S0, (qc, kc, vc, gc))
    o = o.transpose(1, 0, 3, 2, 4).reshape(b, T, HG_HEADS, HG_HEAD_V)
    o = o * lax.rsqrt(jnp.mean(o * o, axis=-1, keepdims=True) + EPS) * norm_gain.astype(f32)
    g = jax.nn.silu(g_raw.astype(f32)).reshape(b, T, HG_HEADS, HG_HEAD_V)
    return (o * g).reshape(b, T, HG_HEADS * HG_HEAD_V).astype(dt)


def short_conv_group(b_gate, c_gate, h, conv_w, out_gain):
    u = c_gate * h
    w = conv_w.astype(u.dtype)[:, None, :]
    y = lax.conv_general_dilated(u, w, window_strides=(1,), padding=((CONV_WIDTH - 1, 0),),
                                 dimension_numbers=('NWC', 'WIO', 'NWC'),
                                 feature_group_count=SC_WIDTH)
    return rms_norm(b_gate * y, out_gain)


def hier_moe(h, w_rg, w_re, w_gate, w_up, w_down):
    N, D = h.shape
    f32 = jnp.float32
    hf = h.astype(f32)
    g_prob = jax.nn.softmax(hf @ w_rg.astype(f32), axis=-1)
    g_p, g_idx = lax.top_k(g_prob, 1)
    e_logits = (hf @ w_re.astype(f32)).reshape(N, N_GROUPS, EXPERTS_PER_GROUP)
    e_logits = jnp.take_along_axis(e_logits, g_idx[:, :, None], axis=1)[:, 0]
    e_prob = jax.nn.softmax(e_logits, axis=-1)
    e_p, e_idx = lax.top_k(e_prob, TOP_K)
    weights = g_p * e_p / jnp.sum(e_p, axis=-1, keepdims=True)
    expert_id = g_idx * EXPERTS_PER_GROUP + e_idx

    A = N * TOP_K
    flat_e = expert_id.reshape(A)
    flat_tok = jnp.repeat(jnp.arange(N, dtype=jnp.int32), TOP_K)
    flat_w = weights.reshape(A)
    order = jnp.argsort(flat_e)
    sorted_e = flat_e[order]
    counts = jnp.bincount(flat_e, length=N_EXPERTS)
    padded = (counts + BLK - 1) // BLK * BLK
    start = jnp.cumsum(counts) - counts
    pstart = jnp.cumsum(padded) - padded
    dest = pstart[sorted_e] + jnp.arange(A, dtype=jnp.int32) - start[sorted_e]
    n_blocks = -(-(A + N_EXPERTS * (BLK - 1)) // BLK)
    P = n_blocks * BLK
    buf_tok = jnp.full((P,), N, jnp.int32).at[dest].set(flat_tok[order])
    buf_w = jnp.zeros((P,), f32).at[dest].set(flat_w[order])
    block_e = jnp.minimum(
        jnp.searchsorted(jnp.cumsum(padded), jnp.arange(n_blocks) * BLK, side='right'),
        N_EXPERTS - 1).astype(jnp.int32)
    h_pad = jnp.concatenate([h, jnp.zeros((1, D), h.dtype)], axis=0)
    xs = h_pad[buf_tok].reshape(n_blocks, BLK, D)

    def expert_block(args):
        xb, e = args
        a = xb @ w_gate[e]
        bb = xb @ w_up[e]
        return (jax.nn.silu(a) * bb) @ w_down[e]

    ys = lax.map(expert_block, (xs, block_e)).reshape(P, D)
    out = jnp.zeros((N + 1, D), h.dtype).at[buf_tok].add(ys * buf_w[:, None].astype(h.dtype))
    return out[:N]


def setup_inputs(seed: int = 0) -> dict:
    key = jax.random.key(seed)
    ks = jax.random.split(key, 20)
    f32 = jnp.float32
    nrm = lambda k, shape, fan_in: jax.random.normal(k, shape, f32) * (fan_in ** -0.5)
    gain = lambda k, shape: 1.0 + 0.02 * jax.random.normal(k, shape, f32)
    return {
        "x": jax.random.normal(ks[0], (BATCH, SEQ, D_MODEL), f32),
        "p": jax.random.normal(ks[1], (DEPTH, BATCH, SEQ, PLE_DIM), f32),
        "g_mix": gain(ks[2], (DEPTH, D_MODEL)),
        "w_in": nrm(ks[3], (DEPTH, D_MODEL, IN_COLS), D_MODEL),
        "lb_logits": 0.5 * jax.random.normal(ks[4], (DEPTH + 1, HG_WIDTH), f32),
        "hg_norm": gain(ks[5], (DEPTH, HG_HEAD_V)),
        "conv_w": nrm(ks[6], (DEPTH, CONV_WIDTH, SC_WIDTH), CONV_WIDTH),
        "sc_norm": gain(ks[7], (DEPTH, SC_WIDTH)),
        "w_out": nrm(ks[8], (DEPTH, MIX_WIDTH, D_MODEL), MIX_WIDTH),
        "g_ffn": gain(ks[9], (DEPTH, D_MODEL)),
        "w_router_group": nrm(ks[10], (DEPTH, D_MODEL, N_GROUPS), D_MODEL),
        "w_router_expert": nrm(ks[11], (DEPTH, D_MODEL, N_EXPERTS), D_MODEL),
        "w_gate": nrm(ks[12], (DEPTH, N_EXPERTS, D_MODEL, EXPERT_FF), D_MODEL),
        "w_up": nrm(ks[13], (DEPTH, N_EXPERTS, D_MODEL, EXPERT_FF), D_MODEL),
        "w_down": nrm(ks[14], (DEPTH, N_EXPERTS, EXPERT_FF, D_MODEL), EXPERT_FF),
        "w_ple": nrm(ks[15], (DEPTH, PLE_DIM, D_MODEL), PLE_DIM),
        "g_ple": gain(ks[16], (DEPTH, D_MODEL)),
        "w_ple_gate": nrm(ks[17], (DEPTH, D_MODEL, D_MODEL), D_MODEL),
        "g_final": gain(ks[18], (D_MODEL,)),
    }


def reference(x, p, g_mix, w_in, lb_logits, hg_norm, conv_w, sc_norm, w_out, g_ffn,
              w_router_group, w_router_expert, w_gate, w_up, w_down, w_ple, g_ple,
              w_ple_gate, g_final):
    b, T, D = x.shape
    lower_bounds = jnp.cumsum(jax.nn.softmax(lb_logits.astype(jnp.float32), axis=0), axis=0)
    for l in range(DEPTH):
        xn = rms_norm(x, g_mix[l])
        z = xn @ w_in[l]
        q_raw, f_raw, i_raw, g_raw, b_gate, c_gate, h_sc = jnp.split(
            z, np.cumsum([HG_WIDTH] * 4 + [SC_WIDTH] * 2).tolist(), axis=-1)
        o_hg = hgrn2_group(q_raw, f_raw, i_raw, g_raw, lower_bounds[l], hg_norm[l])
        o_sc = short_conv_group(b_gate, c_gate, h_sc, conv_w[l], sc_norm[l])
        x = x + jnp.concatenate([o_hg, o_sc], axis=-1) @ w_out[l]
        hn = rms_norm(x, g_ffn[l]).reshape(b * T, D)
        x = x + hier_moe(hn, w_router_group[l], w_router_expert[l],
                         w_gate[l], w_up[l], w_down[l]).reshape(b, T, D)
        ple = rms_norm(p[l] @ w_ple[l], g_ple[l])
        x = x + jax.nn.sigmoid(x @ w_ple_gate[l]) * ple
    return rms_norm(x, g_final)
```

```python
import numpy as np
import ml_dtypes
from contextlib import ExitStack
import concourse.bass as bass
import concourse.mybir as mybir
from concourse.bass_utils import run_bass_kernel_spmd

F32 = mybir.dt.float32
BF16 = mybir.dt.bfloat16
I32 = mybir.dt.int32
AF = mybir.ActivationFunctionType
ALU = mybir.AluOpType
AX = mybir.AxisListType

D = 2048
EPS = 1e-6
EPS_HG = 1e-6 * 128.0
NEXP = 32


class Buf:
    __slots__ = ("name", "t", "last_w", "readers", "aliases", "off", "excl")

    def __init__(self, name, t):
        self.name = name
        self.t = t
        self.last_w = None
        self.readers = {}
        self.aliases = []
        self.off = None
        self.excl = False

    def __getitem__(self, idx):
        return self.t[idx]


class Instr:
    __slots__ = ("eng", "fn", "deps", "is_dma", "sem", "val", "signal", "prewait")

    def __init__(self, eng, fn, deps, is_dma):
        self.eng = eng
        self.fn = fn
        self.deps = deps
        self.is_dma = is_dma
        self.sem = None
        self.val = None
        self.signal = False
        self.prewait = None


class Sched:
    ENGS = ("pe", "act", "dve", "pool", "sp")

    def __init__(self, nc, es, n_dma_sems=8):
        self.nc = nc
        self.streams = {e: [] for e in self.ENGS}
        self.esem = {e: es.enter_context(nc.semaphore("s_" + e)) for e in self.ENGS}
        self.dsems = {}
        for q in ("sp", "pool", "act"):
            self.dsems[q] = [es.enter_context(nc.semaphore("d_%s%d" % (q, i))) for i in range(n_dma_sems)]
        self.dcount = {q: 0 for q in self.dsems}
        self.duse = {q: [0] * n_dma_sems for q in self.dsems}
        self.dlast = {q: [None] * n_dma_sems for q in self.dsems}
        self.n_instr = 0

    @staticmethod
    def _expand(bufs):
        out = {}
        for b in bufs:
            out[id(b)] = b
            for a in b.aliases:
                out[id(a)] = a
        return list(out.values())

    def _deps(self, eng, reads, writes, is_dma):
        deps = {}
        for b in reads:
            if b.last_w is not None:
                deps[id(b.last_w)] = b.last_w
        for b in writes:
            if b.last_w is not None:
                deps[id(b.last_w)] = b.last_w
            for r in b.readers.values():
                deps[id(r)] = r
        out = []
        for d in deps.values():
            if (not is_dma) and (not d.is_dma) and d.eng == "pe" and eng == "pe":
                continue
            if not d.is_dma:
                d.signal = True
            out.append(d)
        return out

    def _commit(self, ins, reads, writes):
        wset = set(id(b) for b in writes)
        for b in reads:
            if id(b) in wset:
                continue
            key = ("dma", id(ins)) if ins.is_dma else ins.eng
            b.readers[key] = ins
        for b in writes:
            b.last_w = ins
            b.readers = {}
        self.n_instr += 1

    def op(self, eng, fn, reads=(), writes=()):
        reads = self._expand(reads)
        writes = self._expand(writes)
        ex = [b for b in reads if b.excl]
        if ex:
            writes = writes + [b for b in ex if all(b is not w for w in writes)]
        ins = Instr(eng, fn, self._deps(eng, reads, writes, False), False)
        self.streams[eng].append(ins)
        self._commit(ins, reads, writes)
        return ins

    def dma(self, q, fn, reads=(), writes=()):
        reads = self._expand(reads)
        writes = self._expand(writes)
        ins = Instr(q, fn, self._deps(q, reads, writes, True), True)
        n = self.dcount[q]
        k = n % len(self.dsems[q])
        self.dcount[q] += 1
        ins.prewait = self.dlast[q][k]
        self.duse[q][k] += 1
        ins.sem = self.dsems[q][k]
        ins.val = 16 * self.duse[q][k]
        self.dlast[q][k] = ins
        self.streams[q].append(ins)
        self._commit(ins, reads, writes)
        return ins

    def barrier(self):
        lasts = []
        for e in self.ENGS:
            for ins in reversed(self.streams[e]):
                if not ins.is_dma:
                    ins.signal = True
                    lasts.append(ins)
                    break
        for q in self.dsems:
            for last in self.dlast[q]:
                if last is not None:
                    lasts.append(last)
        for e in self.ENGS:
            ins = Instr(e, lambda h: h.nop(), list(lasts), False)
            self.streams[e].append(ins)

    def emit(self):
        nc = self.nc
        for e in self.ENGS:
            c = 0
            for ins in self.streams[e]:
                if not ins.is_dma and ins.signal:
                    c += 1
                    ins.sem = self.esem[e]
                    ins.val = c
        with nc.Block() as block:
            def run(e, h):
                waited = {}

                def wait(d):
                    k = id(d.sem)
                    if waited.get(k, 0) >= d.val:
                        return
                    waited[k] = d.val
                    h.wait_ge(d.sem, d.val)

                for ins in self.streams[e]:
                    if ins.is_dma and ins.prewait is not None:
                        wait(ins.prewait)
                    for d in ins.deps:
                        wait(d)
                    r = ins.fn(h)
                    if ins.is_dma:
                        r.then_inc(ins.sem, 16)
                    elif ins.signal:
                        r.then_inc(ins.sem, 1)
                if e in self.dsems:
                    for last in self.dlast[e]:
                        if last is not None:
                            wait(last)

            @block.tensor
            def _(h):
                run("pe", h)

            @block.scalar
            def _(h):
                run("act", h)

            @block.vector
            def _(h):
                run("dve", h)

            @block.gpsimd
            def _(h):
                run("pool", h)

            @block.sync
            def _(h):
                run("sp", h)


class Arena:
    def __init__(self, t, words):
        self.t = t
        self.words = words
        self.off = 0
        self.n = 0

    def alloc(self, name, shape, dt=F32, at=None):
        n = 1
        for s in shape:
            n *= s
        esz = 4 if dt in (F32, I32) else 2
        w = (n * esz + 3) // 4
        w = (w + 7) // 8 * 8
        off = self.off if at is None else at
        assert off + w <= self.words, "arena overflow at %s: %d + %d > %d" % (name, off, w, self.words)
        ap = self.t[:, off:off + w]
        if dt != F32:
            ap = ap.bitcast(dt)
        ap = ap[:, 0:n]
        if len(shape) == 2:
            ap = ap.rearrange("p (a b) -> p a b", a=shape[0])
        elif len(shape) == 3:
            ap = ap.rearrange("p (a b c) -> p a b c", a=shape[0], b=shape[1])
        if at is None:
            self.off += w
        self.n += 1
        b = Buf(name, ap)
        b.off = off
        return b


def build(NG=8, NWG=8, debug=False, stop=99):
    nc = bass.Bass("TRN2", target_bir_lowering=False)
    TOK = NG * 512
    WTOK = NWG * 512
    NT = TOK // 128
    NB = -(-(2 * TOK + NEXP * 255) // 256)
    PR = NB * 256
    okind = "ExternalOutput" if debug else "Internal"

    def din(name, shape, dt=F32):
        return nc.dram_tensor(name, shape, dt, kind="ExternalInput")

    xT_d = din("xT", [D, TOK])
    xTp_d = din("xTp", [D, WTOK])
    xtok_d = din("xtok", [TOK, D])
    pT_d = din("pT", [256, TOK])
    whg_d = din("whg", [8, D, 512])
    wsc_d = din("wsc", [8, D, 384])
    wout_d = din("wout", [D, D])
    wr_d = din("wr", [128, 16, 36])
    wgt_d = din("wgt", [NEXP * 128 * 4, 2048])
    wut_d = din("wut", [NEXP * 128 * 4, 2048])
    wdt_d = din("wdt", [NEXP * 128 * 4, 2048])
    wple_d = din("wple", [256, D])
    wpg_d = din("wpg", [D, D])
    lb_d = din("lb", [2, 1024])
    hgn_d = din("hgn", [1, 128])
    gffn_d = din("gffn", [1, D])
    gple_d = din("gple", [1, D])
    gfin_d = din("gfin", [1, D])
    cols_d = din("cols", [128, 64])
    zeros_d = din("zeros", [256, D], BF16)
    out_d = nc.dram_tensor("out", [TOK, D], F32, kind="ExternalOutput")
    xmid_d = nc.dram_tensor("xmid", [TOK, D], F32, kind=okind)
    hn_d = nc.dram_tensor("hn", [TOK, D], BF16, kind="Internal")
    xs_d = nc.dram_tensor("xs", [PR, D], BF16, kind="Internal")
    ys_d = nc.dram_tensor("ys", [PR, D], F32, kind="Internal")
    if debug:
        dbg_d = nc.dram_tensor("dbg", [128, 8, NT], F32, kind="ExternalOutput")
        dbgb_d = nc.dram_tensor("dbgb", [128, NB], F32, kind="ExternalOutput")

    with ExitStack() as es:
        S = Sched(nc, es)
        AW = 51200
        arena_t = es.enter_context(nc.sbuf_tensor("arena", [128, AW], F32))
        A = Arena(arena_t, AW)
        banks = [Buf("bank%d" % i, es.enter_context(nc.psum_tensor("bank%d" % i, [128, 512], F32))) for i in range(8)]
        for bk_ in banks:
            bk_.excl = True
        B6b = banks[6].t[:, :].bitcast(BF16)
        B5b = banks[5].t[:, :].bitcast(BF16)

        ident_f = A.alloc("ident_f", [128])
        ident_b = A.alloc("ident_b", [128], BF16)
        tri2 = A.alloc("tri2", [128])
        stri = A.alloc("stri", [128])
        ones_f = A.alloc("ones_f", [128])
        ones_b = A.alloc("ones_b", [128], BF16)
        w12_all = A.alloc("w12_all", [2, NT])
        dest_f = A.alloc("dest_f", [2, NT])
        dest_i = A.alloc("dest_i", [2, NT], I32)
        idxw = A.alloc("idxw", [4, NB], I32)
        p4 = A.alloc("p4", [4])
        true_persist_mark = A.off
        ind2 = A.alloc("ind2", [2])
        oml = A.alloc("oml", [1024])
        hgn = A.alloc("hgn", [128])
        gffn = A.alloc("gffn", [D])
        cols = A.alloc("cols", [64])
        wr = A.alloc("wr", [16, 36])
        Sst = [A.alloc("S%d" % h, [128]) for h in range(8)]
        Sbf = [A.alloc("Sb%d" % h, [128], BF16) for h in range(8)]
        tails = A.alloc("tails", [8, 2])
        oh1_all = A.alloc("oh1_all", [NT, 32])
        oh2_all = A.alloc("oh2_all", [NT, 32])
        rank_all = A.alloc("rank_all", [NT, 32])
        Csum = A.alloc("Csum", [32])
        gmix_c = lambda c: cols[:, c:c + 1]
        gffn_c = lambda c: cols[:, 16 + c:17 + c]
        scn_c = lambda j: cols[:, 32 + j:33 + j]
        cw_c = lambda j, k: cols[:, 40 + 3 * j + k:41 + 3 * j + k]

        def memset(eng, buf, ap, val):
            S.op(eng, lambda g: g.memset(ap, val), writes=[buf])

        memset("pool", ident_f, ident_f[:], 1.0)
        S.op("pool", lambda g: g.affine_select(out=ident_f[:], in_=ident_f[:], pattern=[[-1, 128]], compare_op=ALU.is_equal,
                                                fill=0.0, base=0, channel_multiplier=1), reads=[ident_f], writes=[ident_f])
        S.op("dve", lambda g: g.tensor_copy(out=ident_b[:], in_=ident_f[:]), reads=[ident_f], writes=[ident_b])
        memset("pool", tri2, tri2[:], 1.0)
        S.op("pool", lambda g: g.affine_select(out=tri2[:], in_=tri2[:], pattern=[[1, 128]], compare_op=ALU.is_ge,
                                                fill=0.0, base=0, channel_multiplier=-1), reads=[tri2], writes=[tri2])
        memset("pool", tri2, tri2[0:64, 64:128], 0.0)
        memset("pool", stri, stri[:], 1.0)
        S.op("pool", lambda g: g.affine_select(out=stri[:], in_=stri[:], pattern=[[1, 128]], compare_op=ALU.is_ge,
                                                fill=0.0, base=-1, channel_multiplier=-1), reads=[stri], writes=[stri])
        memset("pool", ones_f, ones_f[:], 1.0)
        memset("pool", ones_b, ones_b[:], 1.0)
        memset("pool", ind2, ind2[:], 0.0)
        memset("pool", ind2, ind2[0:64, 0:1], 1.0)
        memset("pool", ind2, ind2[64:128, 1:2], 1.0)
        memset("pool", tails, tails[:], 0.0)
        memset("pool", Csum, Csum[:], 0.0)
        for h in range(8):
            memset("pool", Sst[h], Sst[h][:], 0.0)
            memset("pool", Sbf[h], Sbf[h][:], 0.0)
        S.op("pool", lambda g: g.iota(p4[:], pattern=[[1, 4]], base=0, channel_multiplier=4,
                                      allow_small_or_imprecise_dtypes=True), writes=[p4])
        tmp_lb = A.alloc("tmp_lb", [2, 1024])
        S.dma("sp", lambda g: g.dma_start(out=tmp_lb[:, 0, :], in_=lb_d.ap()[0:1, :].partition_broadcast(128)), writes=[tmp_lb])
        S.dma("sp", lambda g: g.dma_start(out=tmp_lb[:, 1, :], in_=lb_d.ap()[1:2, :].partition_broadcast(128)), writes=[tmp_lb])
        S.dma("sp", lambda g: g.dma_start(out=hgn[:], in_=hgn_d.ap().partition_broadcast(128)), writes=[hgn])
        S.dma("sp", lambda g: g.dma_start(out=gffn[:], in_=gffn_d.ap().partition_broadcast(128)), writes=[gffn])
        S.dma("sp", lambda g: g.dma_start(out=cols[:], in_=cols_d.ap()), writes=[cols])
        S.dma("sp", lambda g: g.dma_start(out=wr[:], in_=wr_d.ap()), writes=[wr])
        S.op("dve", lambda g: g.tensor_tensor(out=oml[:], in0=tmp_lb[:, 0, :], in1=tmp_lb[:, 1, :], op=ALU.subtract),
             reads=[tmp_lb], writes=[oml])
        S.op("act", lambda g: g.activation(out=oml[:], in_=oml[:], func=AF.Exp), reads=[oml], writes=[oml])
        S.op("dve", lambda g: g.tensor_scalar(out=oml[:], in0=oml[:], scalar1=1.0, scalar2=None, op0=ALU.add), reads=[oml], writes=[oml])
        S.op("dve", lambda g: g.reciprocal(out=oml[:], in_=oml[:]), reads=[oml], writes=[oml])
        for c in range(16):
            S.op("pool", (lambda c: lambda g: g.tensor_scalar(out=wr[:, c, :], in0=wr[:, c, :], scalar1=gffn_c(c), scalar2=None,
                                                              op0=ALU.mult))(c), reads=[wr, cols], writes=[wr])
        A.off -= 2048 + 0
        S.barrier()
        persist_mark = A.off

        wslab = [A.alloc("wslab%d" % i, [16, 512], BF16) for i in range(2)]
        xb = A.alloc("xb", [16, 512], BF16)
        xp = [A.alloc("xp%d" % i, [2, 512]) for i in range(4)]
        xmid_off = None
        sq = [A.alloc("sq%d" % i, [2, 512], BF16) for i in range(2)]
        omixT = A.alloc("omixT", [16, 512], BF16)
        byb = A.alloc("byb", [8, 512], BF16)
        ubuf = A.alloc("ubuf", [514])
        t1 = A.alloc("t1", [512])
        yv = A.alloc("yv", [512])
        yv2 = A.alloc("yv2", [512])
        sqy = A.alloc("sqy", [512], BF16)
        rstd_bc = A.alloc("rstd_bc", [512])
        rstd2_bc = A.alloc("rstd2_bc", [512])
        rstd_sc = A.alloc("rstd_sc", [512])
        lnbc = A.alloc("lnbc", [512])
        rcol = A.alloc("rcol", [8])
        xmid = [A.alloc("xmid0", [D], at=xb.off), A.alloc("xmid1", [D], at=xb.off + 2048),
                A.alloc("xmid2", [D], at=xp[0].off), A.alloc("xmid3", [D], at=xp[2].off)]
        assert xp[1].off == xp[0].off + 1024 and xp[3].off == xp[2].off + 1024
        for xm_, al_ in ((xmid[0], [xb]), (xmid[1], [xb]), (xmid[2], [xp[0], xp[1]]), (xmid[3], [xp[2], xp[3]])):
            xm_.aliases = list(al_)
            for a_ in al_:
                a_.aliases.append(xm_)
        xmT = A.alloc("xmT", [16, 128])
        hnb = A.alloc("hnb", [D], BF16)
        junk = A.alloc("junk", [D], BF16)
        etmp = [A.alloc("etmp%d" % t, [256]) for t in range(2)]
        qg = [[A.alloc("qg%d_%d" % (p_, t), [256]) for t in range(4)] for p_ in range(2)]
        kk = [[A.alloc("kk%d_%d" % (p_, t), [128]) for t in range(4)] for p_ in range(2)]
        lf = [[A.alloc("lf%d_%d" % (p_, t), [128]) for t in range(4)] for p_ in range(2)]
        vb = [[A.alloc("vb%d_%d" % (p_, t), [128], BF16) for t in range(4)] for p_ in range(2)]
        NTMP = 3
        eA = [A.alloc("eA%d" % i, [128]) for i in range(NTMP)]
        enA = [A.alloc("enA%d" % i, [128]) for i in range(NTMP)]
        Ecol = [A.alloc("Ecol%d" % i, [2]) for i in range(NTMP)]
        qt = [A.alloc("qt%d" % i, [128], BF16) for i in range(NTMP)]
        kt = [A.alloc("kt%d" % i, [128], BF16) for i in range(NTMP)]
        qtT = [A.alloc("qtT%d" % i, [128], BF16) for i in range(NTMP)]
        ktT = [A.alloc("ktT%d" % i, [128], BF16) for i in range(NTMP)]
        scTm = [A.alloc("scTm%d" % i, [128], BF16) for i in range(NTMP)]
        U0 = [A.alloc("U0%d" % i, [128]) for i in range(NTMP)]
        S1 = [A.alloc("S1%d" % i, [128]) for i in range(NTMP)]
        S1b = [A.alloc("S1b%d" % i, [128], BF16) for i in range(NTMP)]
        gsn = [A.alloc("gsn%d" % i, [128]) for i in range(NTMP)]
        ogb = [A.alloc("ogb%d" % i, [128], BF16) for i in range(NTMP)]
        sm = [A.alloc("sm%d" % i, [8]) for i in range(NTMP)]
        lg = A.alloc("lg", [36])
        rt = A.alloc("rt", [64])
        Ct = A.alloc("Ct", [32])

        import os
        _dbg2 = os.environ.get("K_DBG2", "")
        _dbg3 = os.environ.get("K_DBG3", "")

        def load_x_group(src_d, g0):
            for i in range(8):
                xpi = xp[i % 4]
                sqi = sq[i % 2]
                S.dma("sp", (lambda i, xpi: lambda g: g.dma_start(
                    out=xpi[:], in_=src_d.ap()[2 * i * 128:(2 * i + 2) * 128, g0 * 512:(g0 + 1) * 512]
                    .rearrange("(c p) t -> p c t", p=128)))(i, xpi), writes=[xpi])
                S.op("act", (lambda xpi, sqi: lambda g: g.activation(out=sqi[:], in_=xpi[:], func=AF.Square))(xpi, sqi),
                     reads=[xpi], writes=[sqi])
                for cc in range(2):
                    c = 2 * i + cc
                    S.op("act", (lambda c, cc, xpi: lambda g: g.activation(out=xb[:, c, :], in_=xpi[:, cc, :], func=AF.Copy, scale=gmix_c(c)))(c, cc, xpi),
                         reads=[xpi, cols], writes=[xb])
                    S.op("pe", (lambda c, cc, sqi: lambda g: g.matmul(banks[7][:, :], lhsT=ones_b[:], rhs=sqi[:, cc, :],
                                                                      start=(c == 0), stop=(c == 15)))(c, cc, sqi),
                         reads=[sqi, ones_b], writes=[banks[7]])
            S.op("act", lambda g: g.activation(out=lnbc[:], in_=banks[7][:, :], func=AF.Ln, scale=1.0 / D, bias=EPS),
                 reads=[banks[7]], writes=[lnbc])
            S.op("act", lambda g: g.activation(out=rstd_bc[:], in_=lnbc[:], func=AF.Exp, scale=-0.5), reads=[lnbc], writes=[rstd_bc])
            S.op("act", lambda g: g.activation(out=rstd2_bc[:], in_=lnbc[:], func=AF.Exp, scale=-1.0), reads=[lnbc], writes=[rstd2_bc])
            if _dbg2 == "nok1":
                return
            for t in range(4):
                S.op("pe", (lambda t: lambda g: g.transpose(out=banks[3][:, 384 + 32 * t:384 + 32 * (t + 1)], in_=rstd_bc[0:32, t * 128:(t + 1) * 128],
                                                            identity=ident_f[0:32, 0:32]))(t),
                     reads=[rstd_bc, ident_f], writes=[banks[3]])
            S.op("dve", lambda g: g.tensor_copy(out=rcol[:, 0:4], in_=banks[3][:, 384:512].rearrange("p (a b) -> p a b", a=4)[:, :, 0]),
                 reads=[banks[3]], writes=[rcol])
            S.op("dve", lambda g: g.tensor_scalar(out=rcol[:, 4:8], in0=rcol[:, 0:4], scalar1=-1.0, scalar2=None, op0=ALU.mult),
                 reads=[rcol], writes=[rcol])


        slab_plan = []
        slab_state = {"i": 0, "issued": 0}

        def plan_slabs():
            for wg in range(NWG):
                for h in range(8):
                    slab_plan.append((whg_d.ap()[h, :, 256:512].rearrange("(c p) n -> p c n", p=128), 256))
                if wg == NWG - 1:
                    for j in range(8):
                        slab_plan.append((wsc_d.ap()[j, :, :].rearrange("(c p) n -> p c n", p=128), 384))
            for g0 in range(NG):
                for h in range(8):
                    slab_plan.append((whg_d.ap()[h, :, :].rearrange("(c p) n -> p c n", p=128), 512))
                for j in range(8):
                    slab_plan.append((wsc_d.ap()[j, :, :].rearrange("(c p) n -> p c n", p=128), 384))
                for n in range(4):
                    slab_plan.append((wout_d.ap()[:, n * 512:(n + 1) * 512].rearrange("(c p) n -> p c n", p=128), 512))

        def issue_slab(i):
            src_ap, ncols = slab_plan[i]
            ws = wslab[i % 2]
            S.dma("pool", (lambda ws, src_ap, ncols: lambda g: g.dma_start(out=ws[:, :, 0:ncols], in_=src_ap))(ws, src_ap, ncols), writes=[ws])

        def next_slab(ncols_expected):
            i = slab_state["i"]
            slab_state["i"] += 1
            while slab_state["issued"] <= min(i + 1, len(slab_plan) - 1):
                issue_slab(slab_state["issued"])
                slab_state["issued"] += 1
            assert slab_plan[i][1] == ncols_expected, (i, slab_plan[i][1], ncols_expected)
            return wslab[i % 2]

        tmp_ctr = {"n": 0, "e": 0}

        def gen_inproj(h, full, par):
            c0 = 0 if full else 256
            ncols = 512 if full else 256
            ws = next_slab(ncols)
            oml_h = oml[:, h * 128:(h + 1) * 128]
            fo = 256 - c0
            for t in range(4):
                bk = banks[t % 2]
                for c in range(16):
                    S.op("pe", (lambda c, t, bk: lambda g: g.matmul(bk[:, 0:ncols], lhsT=xb[:, c, t * 128:(t + 1) * 128],
                                                                     rhs=ws[:, c, 0:ncols], start=(c == 0), stop=(c == 15)))(c, t, bk),
                         reads=[xb, ws], writes=[bk])
                    if c % 2 == 1 and c < 15:
                        yield
                rs = rcol[:, t:t + 1]
                nrs = rcol[:, 4 + t:5 + t]
                qg_t, kk_t, vb_t, lf_t = qg[par][t], kk[par][t], vb[par][t], lf[par][t]
                et = etmp[tmp_ctr["e"] % 2]
                tmp_ctr["e"] += 1
                if full:
                    S.op("act", (lambda et, bk, nrs: lambda g: g.activation(out=et[:], in_=bk[:, 0:256], func=AF.Exp, scale=nrs))(et, bk, nrs),
                         reads=[bk, rcol], writes=[et])
                S.op("act", (lambda kk_t, bk, rs: lambda g: g.activation(out=kk_t[:], in_=bk[:, fo:fo + 128], func=AF.Exp, scale=rs))(kk_t, bk, rs),
                     reads=[bk, rcol], writes=[kk_t])
                S.op("dve", (lambda vb_t, bk, rs: lambda g: g.tensor_scalar(out=vb_t[:], in0=bk[:, fo + 128:fo + 256], scalar1=rs, scalar2=None,
                                                                        op0=ALU.mult))(vb_t, bk, rs), reads=[bk, rcol], writes=[vb_t])
                if full:
                    S.op("act", (lambda et: lambda g: g.activation(out=et[:], in_=et[:], func=AF.Ln, bias=1.0))(et), reads=[et], writes=[et])
                    S.op("act", (lambda et: lambda g: g.activation(out=et[:], in_=et[:], func=AF.Exp, scale=-1.0))(et), reads=[et], writes=[et])
                    S.op("dve", (lambda qg_t, bk, rs, et: lambda g: g.scalar_tensor_tensor(out=qg_t[:], in0=bk[:, 0:256], scalar=rs, in1=et[:],
                                                                                       op0=ALU.mult, op1=ALU.mult))(qg_t, bk, rs, et),
                         reads=[bk, rcol, et], writes=[qg_t])
                S.op("act", (lambda kk_t: lambda g: g.activation(out=kk_t[:], in_=kk_t[:], func=AF.Ln, bias=1.0))(kk_t), reads=[kk_t], writes=[kk_t])
                S.op("act", (lambda kk_t: lambda g: g.activation(out=kk_t[:], in_=kk_t[:], func=AF.Exp, scale=-1.0))(kk_t), reads=[kk_t], writes=[kk_t])
                S.op("dve", (lambda kk_t: lambda g: g.tensor_tensor(out=kk_t[:], in0=kk_t[:], in1=oml_h, op=ALU.mult))(kk_t),
                     reads=[kk_t, oml], writes=[kk_t])
                S.op("act", (lambda kk_t, lf_t: lambda g: g.activation(out=lf_t[:], in_=kk_t[:], func=AF.Ln, scale=-1.0, bias=1.0))(kk_t, lf_t),
                     reads=[kk_t], writes=[lf_t])
                yield

        B2b = banks[2].t[:, :].bitcast(BF16)

        def gen_pre(h, t, full, par, i):
            b3 = banks[3]
            qg_t, kk_t, lf_t = qg[par][t], kk[par][t], lf[par][t]
            S.op("pe", lambda g: g.matmul(b3[:, 0:128], lhsT=tri2[:], rhs=lf_t[:], start=True, stop=True), reads=[tri2, lf_t], writes=[b3])
            S.op("pe", lambda g: g.matmul(b3[:, 128:130], lhsT=lf_t[:], rhs=ind2[:], start=True, stop=True), reads=[lf_t, ind2], writes=[b3])
            yield
            S.op("act", lambda g: g.activation(out=enA[i][:], in_=b3[:, 0:128], func=AF.Exp, scale=-1.0), reads=[b3], writes=[enA[i]])
            if full:
                S.op("act", lambda g: g.activation(out=eA[i][:], in_=b3[:, 0:128], func=AF.Exp), reads=[b3], writes=[eA[i]])
            S.op("act", lambda g: g.activation(out=Ecol[i][:], in_=b3[:, 128:130], func=AF.Exp), reads=[b3], writes=[Ecol[i]])
            S.op("dve", lambda g: g.tensor_tensor(out=kt[i][:], in0=kk_t[:], in1=enA[i][:], op=ALU.mult), reads=[kk_t, enA[i]], writes=[kt[i]])
            if not full:
                return
            S.op("dve", lambda g: g.tensor_tensor(out=qt[i][:], in0=qg_t[:, 0:128], in1=eA[i][:], op=ALU.mult), reads=[qg_t, eA[i]], writes=[qt[i]])
            S.op("pool", lambda g: g.tensor_tensor(out=gsn[i][:], in0=qg_t[:, 128:256], in1=hgn[:], op=ALU.mult), reads=[qg_t, hgn], writes=[gsn[i]])
            S.op("pe", lambda g: g.transpose(out=B6b[:, 0:128], in_=qt[i][:], identity=ident_b[:]), reads=[qt[i], ident_b], writes=[banks[6]])
            S.op("pe", lambda g: g.transpose(out=B6b[:, 128:256], in_=kt[i][:], identity=ident_b[:]), reads=[kt[i], ident_b], writes=[banks[6]])
            yield
            S.op("act", lambda g: g.copy(out=qtT[i][:], in_=B6b[:, 0:128]), reads=[banks[6]], writes=[qtT[i]])
            S.op("act", lambda g: g.copy(out=ktT[i][:], in_=B6b[:, 128:256]), reads=[banks[6]], writes=[ktT[i]])
            S.op("pe", lambda g: g.matmul(b3[:, 256:384], lhsT=ktT[i][:], rhs=qtT[i][:], start=True, stop=True), reads=[ktT[i], qtT[i]], writes=[b3])
            yield
            S.op("dve", lambda g: g.tensor_tensor(out=scTm[i][:], in0=b3[:, 256:384], in1=tri2[:], op=ALU.mult), reads=[b3, tri2], writes=[scTm[i]])

        def gen_state(h, t, full, par, i):
            Sh, Sb = Sst[h], Sbf[h]
            b4, b5 = banks[4], banks[5]
            vb_t = vb[par][t]
            S.op("dve", lambda g: g.tensor_scalar(out=U0[i][:], in0=Sh[:], scalar1=Ecol[i][:, 0:1], scalar2=None, op0=ALU.mult),
                 reads=[Sh, Ecol[i]], writes=[U0[i]])
            S.op("pe", lambda g: g.matmul(b5[:, 0:128], lhsT=kt[i][0:64, :], rhs=vb_t[0:64, :], start=True, stop=True), reads=[kt[i], vb_t], writes=[b5])
            yield
            S.op("dve", lambda g: g.scalar_tensor_tensor(out=S1[i][:], in0=b5[:, 0:128], scalar=Ecol[i][:, 0:1], in1=U0[i][:], op0=ALU.mult, op1=ALU.add),
                 reads=[b5, Ecol[i], U0[i]], writes=[S1[i]])
            S.op("dve", lambda g: g.tensor_scalar(out=U0[i][:], in0=S1[i][:], scalar1=Ecol[i][:, 1:2], scalar2=None, op0=ALU.mult),
                 reads=[S1[i], Ecol[i]], writes=[U0[i]])
            if full:
                S.op("act", lambda g: g.copy(out=S1b[i][:], in_=S1[i][:]), reads=[S1[i]], writes=[S1b[i]])
                S.op("pe", lambda g: g.matmul(b4[:, 0:128], lhsT=scTm[i][:], rhs=vb_t[:], start=True, stop=False), reads=[scTm[i], vb_t], writes=[b4])
                S.op("pe", lambda g: g.matmul(b4[0:64, 0:128], lhsT=qtT[i][:, 0:64], rhs=Sb[:], start=False, stop=True), reads=[qtT[i], Sb], writes=[b4])
                S.op("pe", lambda g: g.matmul(b4[64:128, 0:128], lhsT=qtT[i][:, 64:128], rhs=S1b[i][:], start=False, stop=True),
                     reads=[qtT[i], S1b[i]], writes=[b4])
            S.op("pe", lambda g: g.matmul(b5[:, 128:256], lhsT=kt[i][64:128, :], rhs=vb_t[64:128, :], start=True, stop=True), reads=[kt[i], vb_t], writes=[b5])
            yield
            S.op("dve", lambda g: g.scalar_tensor_tensor(out=Sh[:], in0=b5[:, 128:256], scalar=Ecol[i][:, 1:2], in1=U0[i][:], op0=ALU.mult, op1=ALU.add),
                 reads=[b5, Ecol[i], U0[i]], writes=[Sh])
            if not full:
                return
            S.op("act", lambda g: g.copy(out=Sb[:], in_=Sh[:]), reads=[Sh], writes=[Sb])
            S.op("act", lambda g: g.activation(out=junk[:, 0:128], in_=b4[:, 0:128], func=AF.Square, accum_out=sm[i][:, 0:1]), reads=[b4], writes=[junk, sm[i]])
            S.op("act", lambda g: g.activation(out=sm[i][:, 1:2], in_=sm[i][:, 0:1], func=AF.Ln, scale=1.0 / 128, bias=EPS_HG), reads=[sm[i]], writes=[sm[i]])
            S.op("act", lambda g: g.activation(out=sm[i][:, 2:3], in_=sm[i][:, 1:2], func=AF.Exp, scale=-0.5), reads=[sm[i]], writes=[sm[i]])
            S.op("dve", lambda g: g.scalar_tensor_tensor(out=ogb[i][:], in0=b4[:, 0:128], scalar=sm[i][:, 2:3], in1=gsn[i][:], op0=ALU.mult, op1=ALU.mult),
                 reads=[b4, sm[i], gsn[i]], writes=[ogb[i]])
            S.op("pe", lambda g: g.transpose(out=B2b[:, 0:128], in_=ogb[i][:], identity=ident_b[:]), reads=[ogb[i], ident_b], writes=[banks[2]])
            yield
            S.op("act", lambda g: g.copy(out=omixT[:, h, t * 128:(t + 1) * 128], in_=B2b[:, 0:128]), reads=[banks[2]], writes=[omixT])

        def exhaust(gen):
            if gen is None:
                return
            for _ in gen:
                pass

        def roundrobin(gens, steps):
            live = [g is not None for g in gens]
            while any(live[:-1]):
                for k, g in enumerate(gens):
                    if not live[k]:
                        continue
                    for _ in range(steps[k]):
                        try:
                            next(g)
                        except StopIteration:
                            live[k] = False
                            break

        def run_heads(full):
            exhaust(gen_inproj(0, full, 0))
            units = [(h, t) for h in range(8) for t in range(4)]
            bg = None
            prev = None
            for s_i in range(len(units) + 1):
                cur = units[s_i] if s_i < len(units) else None
                if cur is not None and cur[1] == 1 and cur[0] + 1 < 8:
                    bg = gen_inproj(cur[0] + 1, full, (cur[0] + 1) % 2)
                if cur is not None and cur[1] == 0 and bg is not None:
                    exhaust(bg)
                    bg = None
                gs = []
                if prev is not None:
                    u = s_i - 1
                    gs.append(gen_state(prev[0], prev[1], full, prev[0] % 2, u % NTMP))
                if cur is not None:
                    gs.append(gen_pre(cur[0], cur[1], full, cur[0] % 2, s_i % NTMP))
                gs.append(bg)
                roundrobin(gs, [1] * (len(gs) - 1) + [3])
                prev = cur
            exhaust(bg)

        sc_ctr = {"n": 0}

        def sc_chunk(j, tail_only=False):
            ws = next_slab(384)
            bset = [banks[0], banks[1], banks[2]] if sc_ctr["n"] % 2 == 0 else [banks[3], banks[4], banks[5]]
            sc_ctr["n"] += 1
            for part in range(3):
                if tail_only and part == 0:
                    continue
                bk = bset[part]
                for c in range(16):
                    S.op("pe", (lambda c, part, bk: lambda g: g.matmul(bk[:, :], lhsT=ws[:, c, part * 128:(part + 1) * 128], rhs=xb[:, c, :],
                                                                        start=(c == 0), stop=(c == 15)))(c, part, bk),
                         reads=[ws, xb], writes=[bk])
            S.op("pool", lambda g: g.tensor_copy(out=ubuf[:, 0:2], in_=tails[:, j, :]), reads=[tails], writes=[ubuf])
            S.op("dve", lambda g: g.tensor_tensor(out=t1[:], in0=bset[1][:, :], in1=rstd2_bc[:], op=ALU.mult),
                 reads=[bset[1], rstd2_bc], writes=[t1])
            S.op("dve", lambda g: g.tensor_tensor(out=ubuf[:, 2:514], in0=t1[:], in1=bset[2][:, :], op=ALU.mult),
                 reads=[bset[2], t1], writes=[ubuf])
            S.op("pool", lambda g: g.tensor_copy(out=tails[:, j, :], in_=ubuf[:, 512:514]), reads=[ubuf], writes=[tails])
            if tail_only:
                return
            S.op("act", lambda g: g.activation(out=yv[:], in_=ubuf[:, 0:512], func=AF.Copy, scale=cw_c(j, 0)), reads=[ubuf, cols], writes=[yv])
            S.op("dve", lambda g: g.scalar_tensor_tensor(out=yv[:], in0=ubuf[:, 1:513], scalar=cw_c(j, 1), in1=yv[:], op0=ALU.mult, op1=ALU.add),
                 reads=[ubuf, cols, yv], writes=[yv])
            S.op("dve", lambda g: g.scalar_tensor_tensor(out=yv[:], in0=ubuf[:, 2:514], scalar=cw_c(j, 2), in1=yv[:], op0=ALU.mult, op1=ALU.add),
                 reads=[ubuf, cols, yv], writes=[yv])
            S.op("dve", lambda g: g.tensor_tensor(out=yv[:], in0=yv[:], in1=rstd_bc[:], op=ALU.mult), reads=[yv, rstd_bc], writes=[yv])
            S.op("dve", lambda g: g.tensor_tensor(out=byb[:, j, :], in0=bset[0][:, :], in1=yv[:], op=ALU.mult),
                 reads=[bset[0], yv], writes=[byb])
            S.op("act", lambda g: g.activation(out=sqy[:], in_=byb[:, j, :], func=AF.Square), reads=[byb], writes=[sqy])
            S.op("pe", lambda g: g.matmul(banks[7][:, :], lhsT=ones_b[:], rhs=sqy[:], start=(j == 0), stop=(j == 7)),
                 reads=[ones_b, sqy], writes=[banks[7]])

        def sc_finish():
            S.op("act", lambda g: g.activation(out=lnbc[:], in_=banks[7][:, :], func=AF.Ln, scale=1.0 / 1024, bias=EPS),
                 reads=[banks[7]], writes=[lnbc])
            S.op("act", lambda g: g.activation(out=rstd_sc[:], in_=lnbc[:], func=AF.Exp, scale=-0.5), reads=[lnbc], writes=[rstd_sc])
            for j in range(8):
                S.op("dve", (lambda j: lambda g: g.scalar_tensor_tensor(out=omixT[:, 8 + j, :], in0=byb[:, j, :], scalar=scn_c(j), in1=rstd_sc[:],
                                                                        op0=ALU.mult, op1=ALU.mult))(j),
                     reads=[byb, cols, rstd_sc], writes=[omixT])

        def outproj_and_route(g0):
            for t in range(4):
                r0 = g0 * 512 + t * 128
                S.dma("sp", (lambda t, r0: lambda g: g.dma_start(out=xmid[t][:], in_=xtok_d.ap()[r0:r0 + 128, :]))(t, r0), writes=[xmid[t]])
            k = 0
            for n in range(4):
                ws = next_slab(512)
                for t in range(4):
                    bk = banks[k % 3]
                    k += 1
                    for c in range(16):
                        S.op("pe", (lambda c, t, bk, ws: lambda g: g.matmul(bk[:, :], lhsT=omixT[:, c, t * 128:(t + 1) * 128], rhs=ws[:, c, :],
                                                                            start=(c == 0), stop=(c == 15)))(c, t, bk, ws),
                             reads=[omixT, ws], writes=[bk])
                    S.op("dve", (lambda t, n, bk: lambda g: g.tensor_tensor(out=xmid[t][:, n * 512:(n + 1) * 512], in0=bk[:, :],
                                                                           in1=xmid[t][:, n * 512:(n + 1) * 512], op=ALU.add))(t, n, bk),
                         reads=[bk, xmid[t]], writes=[xmid[t]])
            for t in range(4):
                tile = g0 * 4 + t
                r0 = tile * 128
                xm = xmid[t]
                S.dma("act", (lambda xm, r0: lambda g: g.dma_start(out=xmid_d.ap()[r0:r0 + 128, :], in_=xm[:]))(xm, r0), reads=[xm])
                S.op("act", (lambda xm: lambda g: g.activation(out=junk[:], in_=xm[:], func=AF.Square, accum_out=rt[:, 0:1]))(xm),
                     reads=[xm], writes=[junk, rt])
                S.op("act", lambda g: g.activation(out=rt[:, 1:2], in_=rt[:, 0:1], func=AF.Ln, scale=1.0 / D, bias=EPS), reads=[rt], writes=[rt])
                S.op("act", lambda g: g.activation(out=rt[:, 2:3], in_=rt[:, 1:2], func=AF.Exp, scale=-0.5), reads=[rt], writes=[rt])
                S.op("dve", (lambda xm: lambda g: g.scalar_tensor_tensor(out=hnb[:], in0=xm[:], scalar=rt[:, 2:3], in1=gffn[:],
                                                                        op0=ALU.mult, op1=ALU.mult))(xm), reads=[xm, rt, gffn], writes=[hnb])
                S.dma("act", (lambda r0: lambda g: g.dma_start(out=hn_d.ap()[r0:r0 + 128, :], in_=hnb[:]))(r0), reads=[hnb])
                for q4 in range(4):
                    bk = banks[q4 % 3]
                    for cc in range(4):
                        c = q4 * 4 + cc
                        S.op("pe", (lambda c, cc, bk, xm: lambda g: g.transpose(out=bk[:, cc * 128:(cc + 1) * 128], in_=xm[:, c * 128:(c + 1) * 128],
                                                                              identity=ident_f[:]))(c, cc, bk, xm),
                             reads=[xm, ident_f], writes=[bk])
                    eng = "act" if q4 % 2 == 0 else "dve"
                    if eng == "act":
                        S.op("act", (lambda q4, bk: lambda g: g.copy(out=xmT[:, q4 * 4:(q4 + 1) * 4, :], in_=bk[:, :].rearrange("p (a b) -> p a b", a=4)))(q4, bk),
                             reads=[bk], writes=[xmT])
                    else:
                        S.op("dve", (lambda q4, bk: lambda g: g.tensor_copy(out=xmT[:, q4 * 4:(q4 + 1) * 4, :], in_=bk[:, :].rearrange("p (a b) -> p a b", a=4)))(q4, bk),
                             reads=[bk], writes=[xmT])
                for c in range(16):
                    S.op("pe", (lambda c: lambda g: g.matmul(banks[7][:, 0:36], lhsT=xmT[:, c, :], rhs=wr[:, c, :], start=(c == 0), stop=(c == 15)))(c),
                         reads=[xmT, wr], writes=[banks[7]])
                S.op("dve", lambda g: g.tensor_scalar(out=lg[:], in0=banks[7][:, 0:36], scalar1=rt[:, 2:3], scalar2=None, op0=ALU.mult),
                     reads=[banks[7], rt], writes=[lg])
                route_tile(tile)

        def route_tile(tile):
            P = "dve"
            R = [lg, rt]
            S.op("dve", lambda g: g.tensor_reduce(out=rt[:, 3:4], in_=lg[:, 0:4], axis=AX.X, op=ALU.max), reads=R, writes=[rt])
            S.op("dve", lambda g: g.tensor_scalar(out=rt[:, 12:16], in0=lg[:, 0:4], scalar1=rt[:, 3:4], scalar2=None, op0=ALU.is_equal), reads=R, writes=[rt])
            S.op("dve", lambda g: g.tensor_scalar(out=rt[:, 48:52], in0=lg[:, 0:4], scalar1=rt[:, 3:4], scalar2=None, op0=ALU.subtract), reads=R, writes=[rt])
            S.op("act", lambda g: g.activation(out=rt[:, 48:52], in_=rt[:, 48:52], func=AF.Exp, accum_out=rt[:, 4:5]), reads=R, writes=[rt])
            S.op("dve", lambda g: g.reciprocal(out=rt[:, 5:6], in_=rt[:, 4:5]), reads=R, writes=[rt])
            S.op("dve", lambda g: g.tensor_scalar(out=rt[:, 16:24], in0=lg[:, 4:12], scalar1=rt[:, 12:13], scalar2=None, op0=ALU.mult), reads=R, writes=[rt])
            for gi in range(1, 4):
                S.op("dve", (lambda gi: lambda g: g.scalar_tensor_tensor(out=rt[:, 16:24], in0=lg[:, 4 + 8 * gi:12 + 8 * gi], scalar=rt[:, 12 + gi:13 + gi],
                                                                         in1=rt[:, 16:24], op0=ALU.mult, op1=ALU.add))(gi), reads=R, writes=[rt])
            S.op("dve", lambda g: g.tensor_reduce(out=rt[:, 6:7], in_=rt[:, 16:24], axis=AX.X, op=ALU.max), reads=R, writes=[rt])
            S.op("dve", lambda g: g.tensor_scalar(out=rt[:, 24:32], in0=rt[:, 16:24], scalar1=rt[:, 6:7], scalar2=None, op0=ALU.is_equal), reads=R, writes=[rt])
            S.op("dve", lambda g: g.scalar_tensor_tensor(out=rt[:, 32:40], in0=rt[:, 24:32], scalar=-1e30, in1=rt[:, 16:24], op0=ALU.mult, op1=ALU.add),
                 reads=R, writes=[rt])
            S.op("dve", lambda g: g.tensor_reduce(out=rt[:, 7:8], in_=rt[:, 32:40], axis=AX.X, op=ALU.max), reads=R, writes=[rt])
            S.op("dve", lambda g: g.tensor_scalar(out=rt[:, 40:48], in0=rt[:, 32:40], scalar1=rt[:, 7:8], scalar2=None, op0=ALU.is_equal), reads=R, writes=[rt])
            S.op("dve", lambda g: g.tensor_tensor(out=rt[:, 8:9], in0=rt[:, 7:8], in1=rt[:, 6:7], op=ALU.subtract), reads=R, writes=[rt])
            S.op("act", lambda g: g.activation(out=rt[:, 8:9], in_=rt[:, 8:9], func=AF.Exp), reads=R, writes=[rt])
            S.op("dve", lambda g: g.tensor_scalar(out=rt[:, 8:9], in0=rt[:, 8:9], scalar1=1.0, scalar2=None, op0=ALU.add), reads=R, writes=[rt])
            S.op("dve", lambda g: g.reciprocal(out=rt[:, 8:9], in_=rt[:, 8:9]), reads=R, writes=[rt])
            S.op("dve", lambda g: g.tensor_tensor(out=w12_all[:, 0, tile:tile + 1], in0=rt[:, 8:9], in1=rt[:, 5:6], op=ALU.mult), reads=R, writes=[w12_all])
            S.op("dve", lambda g: g.tensor_tensor(out=w12_all[:, 1, tile:tile + 1], in0=rt[:, 5:6], in1=w12_all[:, 0, tile:tile + 1], op=ALU.subtract),
                 reads=R + [w12_all], writes=[w12_all])
            for gi in range(4):
                S.op(P, (lambda gi: lambda g: g.tensor_scalar(out=oh1_all[:, tile, gi * 8:(gi + 1) * 8], in0=rt[:, 24:32], scalar1=rt[:, 12 + gi:13 + gi],
                                                               scalar2=None, op0=ALU.mult))(gi), reads=R, writes=[oh1_all])
                S.op(P, (lambda gi: lambda g: g.tensor_scalar(out=oh2_all[:, tile, gi * 8:(gi + 1) * 8], in0=rt[:, 40:48], scalar1=rt[:, 12 + gi:13 + gi],
                                                               scalar2=None, op0=ALU.mult))(gi), reads=R, writes=[oh2_all])
            S.op(P, lambda g: g.tensor_tensor(out=Ct[:], in0=oh1_all[:, tile, :], in1=oh2_all[:, tile, :], op=ALU.add), reads=[oh1_all, oh2_all], writes=[Ct])
            S.op("pe", lambda g: g.matmul(banks[3][:, 400:432], lhsT=stri[:], rhs=Ct[:], start=True, stop=False), reads=[stri, Ct], writes=[banks[3]])
            S.op("pe", lambda g: g.matmul(banks[3][:, 400:432], lhsT=ones_f[:], rhs=Csum[:], start=False, stop=True), reads=[ones_f, Csum], writes=[banks[3]])
            S.op("dve", lambda g: g.tensor_copy(out=rank_all[:, tile, :], in_=banks[3][:, 400:432]), reads=[banks[3]], writes=[rank_all])
            S.op(P, lambda g: g.tensor_tensor(out=Csum[:], in0=Csum[:], in1=Ct[:], op=ALU.add), reads=[Csum, Ct], writes=[Csum])

        if stop <= 0:
            S.emit()
            return nc
        import os
        _dbg = os.environ.get("K_DBG", "")
        plan_slabs()
        for wg in range(NWG):
            load_x_group(xTp_d, wg)
            if _dbg == "lx":
                S.emit()
                return nc
            run_heads(False)
            if _dbg == "h8":
                S.emit()
                return nc
            if wg == NWG - 1:
                for j in range(8):
                    sc_chunk(j, tail_only=True)
        if stop <= 1:
            S.emit()
            return nc
        for h in range(8):
            S.op("act", (lambda h: lambda g: g.copy(out=Sbf[h][:], in_=Sst[h][:]))(h), reads=[Sst[h]], writes=[Sbf[h]])
        for zb in range(NB):
            S.dma("act", (lambda zb: lambda g: g.dma_start(out=xs_d.ap()[zb * 256:(zb + 1) * 256, :], in_=zeros_d.ap()))(zb))
        for g0 in range(NG):
            load_x_group(xT_d, g0)
            run_heads(True)
            for j in range(8):
                sc_chunk(j)
            sc_finish()
            outproj_and_route(g0)

        if stop <= 2:
            S.emit()
            return nc
        S.barrier()
        A.off = persist_mark
        fin = A.alloc("fin", [512])
        fin_i = A.alloc("fin_i", [128], I32)
        b3 = banks[3]
        S.op("pe", lambda g: g.matmul(b3[0:32, 0:128], lhsT=Csum[:], rhs=ones_f[:], start=True, stop=True), reads=[Csum, ones_f], writes=[b3])
        S.op("dve", lambda g: g.tensor_scalar(out=fin[0:32, 0:128], in0=b3[0:32, 0:128], scalar1=255.0, scalar2=None, op0=ALU.add), reads=[b3], writes=[fin])
        S.op("dve", lambda g: g.tensor_copy(out=fin_i[0:32, :], in_=fin[0:32, 0:128]), reads=[fin], writes=[fin_i])
        S.op("dve", lambda g: g.tensor_scalar(out=fin_i[0:32, :], in0=fin_i[0:32, :], scalar1=8, scalar2=8, op0=ALU.arith_shift_right,
                                              op1=ALU.logical_shift_left), reads=[fin_i], writes=[fin_i])
        S.op("dve", lambda g: g.tensor_copy(out=fin[0:32, 0:128], in_=fin_i[0:32, :]), reads=[fin_i], writes=[fin])
        S.op("pe", lambda g: g.matmul(b3[:, 128:160], lhsT=fin[0:32, 0:128], rhs=tri2[0:32, 0:32], start=True, stop=True), reads=[fin, tri2], writes=[b3])
        S.op("pe", lambda g: g.matmul(b3[:, 160:192], lhsT=fin[0:32, 0:128], rhs=stri[0:32, 0:32], start=True, stop=True), reads=[fin, stri], writes=[b3])
        S.op("dve", lambda g: g.tensor_copy(out=fin[:, 128:192], in_=b3[:, 128:192]), reads=[b3], writes=[fin])
        pend = fin[:, 128:160]
        pstart = fin[:, 160:192]
        tmp32 = fin[:, 192:224]
        for tile in range(NT):
            for k, oh in ((0, oh1_all), (1, oh2_all)):
                S.op("dve", (lambda tile: lambda g: g.tensor_tensor(out=tmp32, in0=rank_all[:, tile, :], in1=pstart, op=ALU.add))(tile),
                     reads=[rank_all, fin], writes=[fin])
                S.op("dve", (lambda tile, oh: lambda g: g.tensor_tensor(out=tmp32, in0=tmp32, in1=oh[:, tile, :], op=ALU.mult))(tile, oh),
                     reads=[oh, fin], writes=[fin])
                S.op("dve", (lambda tile, k: lambda g: g.tensor_reduce(out=dest_f[:, k, tile:tile + 1], in_=tmp32, axis=AX.X, op=ALU.add))(tile, k),
                     reads=[fin], writes=[dest_f])
        S.op("dve", lambda g: g.tensor_copy(out=dest_i[:], in_=dest_f[:]), reads=[dest_f], writes=[dest_i])
        bthr = A.alloc("bthr", [NB])
        bacc = A.alloc("bacc", [NB])
        idxf = A.alloc("idxf", [4, NB])
        S.op("pool", lambda g: g.iota(bthr[:], pattern=[[256, NB]], base=0, channel_multiplier=0, allow_small_or_imprecise_dtypes=True), writes=[bthr])
        memset("pool", bacc, bacc[:], 0.0)
        for e in range(NEXP):
            S.op("dve", (lambda e: lambda g: g.scalar_tensor_tensor(out=bacc[:], in0=bthr[:], scalar=fin[:, 128 + e:129 + e], in1=bacc[:],
                                                                   op0=ALU.is_ge, op1=ALU.add))(e), reads=[bthr, fin, bacc], writes=[bacc])
        S.op("dve", lambda g: g.tensor_scalar(out=bacc[:], in0=bacc[:], scalar1=float(NEXP - 1), scalar2=None, op0=ALU.min), reads=[bacc], writes=[bacc])
        for cc in range(4):
            S.op("dve", (lambda cc: lambda g: g.tensor_scalar(out=idxf[:, cc, :], in0=bacc[:], scalar1=512.0, scalar2=p4[:, cc:cc + 1],
                                                             op0=ALU.mult, op1=ALU.add))(cc), reads=[bacc, p4], writes=[idxf])
        S.op("dve", lambda g: g.tensor_copy(out=idxw[:], in_=idxf[:]), reads=[idxf], writes=[idxw])
        if debug:
            dbgt = A.alloc("dbgt", [8, NT])
            S.op("dve", lambda g: g.tensor_copy(out=dbgt[:, 0:2, :], in_=dest_f[:]), reads=[dest_f], writes=[dbgt])
            S.op("dve", lambda g: g.tensor_copy(out=dbgt[:, 2:4, :], in_=w12_all[:]), reads=[w12_all], writes=[dbgt])
            S.op("dve", lambda g: g.memset(dbgt[:, 4:8, :], 0.0), writes=[dbgt])
            S.dma("sp", lambda g: g.dma_start(out=dbg_d.ap(), in_=dbgt[:]), reads=[dbgt])
            S.dma("sp", lambda g: g.dma_start(out=dbgb_d.ap(), in_=bacc[:]), reads=[bacc])
        S.barrier()

        if stop <= 3:
            S.emit()
            return nc
        A.off = true_persist_mark
        hnt = [A.alloc("hnt%d" % i, [D], BF16) for i in range(2)]
        for tile in range(NT):
            ht = hnt[tile % 2]
            r0 = tile * 128
            S.dma("sp", (lambda ht, r0: lambda g: g.dma_start(out=ht[:], in_=hn_d.ap()[r0:r0 + 128, :]))(ht, r0), writes=[ht])
            for k in range(2):
                S.dma("pool", (lambda ht, k, tile: lambda g: g.indirect_dma_start(
                    out=xs_d.ap(), out_offset=bass.IndirectOffsetOnAxis(ap=dest_i[:, k, tile:tile + 1], axis=0),
                    in_=ht[:], in_offset=None))(ht, k, tile), reads=[ht, dest_i])
        S.barrier()
        if stop <= 4:
            S.emit()
            return nc
        A.off = true_persist_mark
        wgb = [A.alloc("wgb%d" % i, [16, 512], BF16) for i in range(2)]
        wub = [A.alloc("wub%d" % i, [16, 512], BF16) for i in range(2)]
        wdb = [A.alloc("wdb%d" % i, [4, 2048], BF16) for i in range(2)]
        xt = [A.alloc("xt%d" % i, [D], BF16) for i in range(2)]
        xsT2 = [A.alloc("xsT", [16, 256], BF16)]
        actT2 = [A.alloc("actT%d" % i, [4, 256], BF16) for i in range(2)]
        ee = A.alloc("ee", [256])
        ga = A.alloc("ga", [256])
        yrow = [A.alloc("yrow0", [D])]
        yrow.append(yrow[0])
        NSTG = min(8, (A.words - A.off) // 2048)
        assert NSTG >= 3, NSTG
        stage = [A.alloc("stage%d" % i, [2048]) for i in range(NSTG)]
        stg = {"n": 0}
        cast_engs = ["act", "dve"]

        assert NSTG == 8, NSTG

        def w_piece(b, i):
            k = b % 2
            if i < 4:
                return wgt_d, wgb[k], i, True
            if i < 8:
                return wut_d, wub[k], i - 4, True
            return wdt_d, wdb[k], i - 8, False

        def gather_w(b, i):
            tab, dst, cc, gu = w_piece(b, i)
            st = stage[i % NSTG]
            S.dma("pool", (lambda st, tab, cc, b: lambda g: g.indirect_dma_start(
                out=st[:], out_offset=None, in_=tab.ap(),
                in_offset=bass.IndirectOffsetOnAxis(ap=idxw[:, cc, b:b + 1], axis=0)))(st, tab, cc, b), reads=[idxw], writes=[st])

        def cast_w(b, i):
            tab, dst, cc, gu = w_piece(b, i)
            st = stage[i % NSTG]
            if gu:
                dview = dst[:, 4 * cc:4 * cc + 4, :]
                sview = st[:].rearrange("p (a b) -> p a b", a=4)
            else:
                dview = dst[:, cc, :]
                sview = st[:]
            if i % 2 == 0:
                S.op("act", (lambda dview, sview: lambda g: g.copy(out=dview, in_=sview))(dview, sview), reads=[st], writes=[dst])
            else:
                S.op("dve", (lambda dview, sview: lambda g: g.tensor_copy(out=dview, in_=sview))(dview, sview), reads=[st], writes=[dst])
            if i + NSTG < 12:
                gather_w(b, i + NSTG)

        for i in range(NSTG):
            gather_w(0, i)
        for i in range(12):
            cast_w(0, i)
        for b in range(NB):
            k = b % 2
            xsT = xsT2[b % len(xsT2)]
            actT = actT2[b % 2]
            if b + 1 < NB:
                for i in range(NSTG):
                    gather_w(b + 1, i)
            for r in range(2):
                r0 = b * 256 + r * 128
                S.dma("sp", (lambda r, r0: lambda g: g.dma_start(out=xt[r][:], in_=xs_d.ap()[r0:r0 + 128, :]))(r, r0), writes=[xt[r]])
                for hh in range(2):
                    bkb, bk = (B6b, banks[6]) if hh == 0 else (B5b, banks[5])
                    for cc in range(8):
                        c = hh * 8 + cc
                        S.op("pe", (lambda c, cc, r, bkb: lambda g: g.transpose(out=bkb[:, cc * 128:(cc + 1) * 128], in_=xt[r][:, c * 128:(c + 1) * 128],
                                                                             identity=ident_b[:]))(c, cc, r, bkb), reads=[xt[r], ident_b], writes=[bk])
                    if hh == 0:
                        S.op("act", (lambda r, bkb, xsT: lambda g: g.copy(out=xsT[:, 0:8, r * 128:(r + 1) * 128], in_=bkb.rearrange("p (a b) -> p a b", a=8)))(r, bkb, xsT),
                             reads=[bk], writes=[xsT])
                    else:
                        S.op("dve", (lambda r, bkb, xsT: lambda g: g.tensor_copy(out=xsT[:, 8:16, r * 128:(r + 1) * 128], in_=bkb.rearrange("p (a b) -> p a b", a=8)))(r, bkb, xsT),
                             reads=[bk], writes=[xsT])
            for fc in range(4):
                bg, bu = banks[0 + 2 * (fc % 2)], banks[1 + 2 * (fc % 2)]
                for c in range(16):
                    S.op("pe", (lambda c, fc, bg, k, xsT: lambda g: g.matmul(bg[:, 0:256], lhsT=wgb[k][:, c, fc * 128:(fc + 1) * 128], rhs=xsT[:, c, :],
                                                                     start=(c == 0), stop=(c == 15)))(c, fc, bg, k, xsT), reads=[wgb[k], xsT], writes=[bg])
                for c in range(16):
                    S.op("pe", (lambda c, fc, bu, k, xsT: lambda g: g.matmul(bu[:, 0:256], lhsT=wub[k][:, c, fc * 128:(fc + 1) * 128], rhs=xsT[:, c, :],
                                                                     start=(c == 0), stop=(c == 15)))(c, fc, bu, k, xsT), reads=[wub[k], xsT], writes=[bu])
                S.op("act", (lambda bg: lambda g: g.activation(out=ee[:], in_=bg[:, 0:256], func=AF.Exp, scale=-1.0))(bg), reads=[bg], writes=[ee])
                S.op("act", lambda g: g.activation(out=ee[:], in_=ee[:], func=AF.Ln, bias=1.0), reads=[ee], writes=[ee])
                S.op("act", lambda g: g.activation(out=ee[:], in_=ee[:], func=AF.Exp, scale=-1.0), reads=[ee], writes=[ee])
                S.op("dve", (lambda bg: lambda g: g.tensor_tensor(out=ga[:], in0=bg[:, 0:256], in1=ee[:], op=ALU.mult))(bg), reads=[bg, ee], writes=[ga])
                S.op("dve", (lambda bu, fc, actT: lambda g: g.tensor_tensor(out=actT[:, fc, :], in0=bu[:, 0:256], in1=ga[:], op=ALU.mult))(bu, fc, actT),
                     reads=[bu, ga], writes=[actT])
                if b + 1 < NB:
                    for i in (3 * fc, 3 * fc + 1, 3 * fc + 2):
                        cast_w(b + 1, i)
            kk2 = 0
            for r in range(2):
                for n in range(4):
                    bk = banks[4 + (kk2 % 2) * 3]
                    kk2 += 1
                    for fc in range(4):
                        S.op("pe", (lambda fc, r, n, bk, k, actT: lambda g: g.matmul(bk[:, :], lhsT=actT[:, fc, r * 128:(r + 1) * 128],
                                                                            rhs=wdb[k][:, fc, n * 512:(n + 1) * 512], start=(fc == 0), stop=(fc == 3)))(fc, r, n, bk, k, actT),
                             reads=[actT, wdb[k]], writes=[bk])
                    S.op("act", (lambda r, n, bk: lambda g: g.copy(out=yrow[r][:, n * 512:(n + 1) * 512], in_=bk[:, :]))(r, n, bk), reads=[bk], writes=[yrow[r]])
                r0 = b * 256 + r * 128
                S.dma("act", (lambda r, r0: lambda g: g.dma_start(out=ys_d.ap()[r0:r0 + 128, :], in_=yrow[r][:]))(r, r0), reads=[yrow[r]])
        S.barrier()

        if stop <= 5:
            S.emit()
            return nc
        A.off = true_persist_mark
        wpg = A.alloc("wpg", [16, D], BF16)
        wple = A.alloc("wple", [2, D], BF16)
        gple = A.alloc("gple", [D])
        gfin = A.alloc("gfin", [D])
        xm3s = [A.alloc("xm3_%d" % i, [D]) for i in range(2)]
        y12 = [A.alloc("y12_%d" % i, [D]) for i in range(2)]
        pTfs = [A.alloc("pTf%d" % i, [2, 128]) for i in range(2)]
        pTbs = [A.alloc("pTb%d" % i, [2, 128], BF16) for i in range(2)]
        x2b = A.alloc("x2b", [D], BF16)
        x2T = A.alloc("x2T", [16, 128], BF16)
        pler = A.alloc("pler", [D])
        eg = A.alloc("eg", [512])
        junk3 = A.alloc("junk3", [D], BF16)
        s3s = [A.alloc("s3_%d" % i, [8]) for i in range(2)]
        for c4 in range(4):
            S.dma("pool", (lambda c4: lambda g: g.dma_start(out=wpg[:, 4 * c4:4 * c4 + 4, :],
                                                           in_=wpg_d.ap()[c4 * 512:(c4 + 1) * 512, :].rearrange("(c p) n -> p c n", p=128)))(c4), writes=[wpg])
        S.dma("pool", lambda g: g.dma_start(out=wple[:], in_=wple_d.ap().rearrange("(c p) n -> p c n", p=128)), writes=[wple])
        S.dma("sp", lambda g: g.dma_start(out=gple[:], in_=gple_d.ap().partition_broadcast(128)), writes=[gple])
        S.dma("sp", lambda g: g.dma_start(out=gfin[:], in_=gfin_d.ap().partition_broadcast(128)), writes=[gfin])

        def p3_load(tile):
            xm3, pTf = xm3s[tile % 2], pTfs[tile % 2]
            r0 = tile * 128
            S.dma("sp", (lambda r0, xm3: lambda g: g.dma_start(out=xm3[:], in_=xmid_d.ap()[r0:r0 + 128, :]))(r0, xm3), writes=[xm3])
            S.dma("sp", (lambda r0, pTf: lambda g: g.dma_start(out=pTf[:], in_=pT_d.ap()[:, r0:r0 + 128].rearrange("(c p) t -> p c t", p=128)))(r0, pTf),
                  writes=[pTf])

        def p3_gather(tile):
            for k in range(2):
                S.dma("pool", (lambda k, tile: lambda g: g.indirect_dma_start(
                    out=y12[k][:], out_offset=None, in_=ys_d.ap(),
                    in_offset=bass.IndirectOffsetOnAxis(ap=dest_i[:, k, tile:tile + 1], axis=0)))(k, tile), reads=[dest_i], writes=[y12[k]])

        p3_load(0)
        p3_gather(0)
        for tile in range(NT):
            r0 = tile * 128
            xm3, pTf, pTb, s3 = xm3s[tile % 2], pTfs[tile % 2], pTbs[tile % 2], s3s[tile % 2]
            if tile + 1 < NT:
                p3_load(tile + 1)
            S.op("act", (lambda pTb, pTf: lambda g: g.copy(out=pTb[:], in_=pTf[:]))(pTb, pTf), reads=[pTf], writes=[pTb])
            S.op("dve", (lambda tile, xm3: lambda g: g.scalar_tensor_tensor(out=xm3[:], in0=y12[0][:], scalar=w12_all[:, 0, tile:tile + 1], in1=xm3[:],
                                                                           op0=ALU.mult, op1=ALU.add))(tile, xm3), reads=[y12[0], w12_all, xm3], writes=[xm3])
            S.op("dve", (lambda tile, xm3: lambda g: g.scalar_tensor_tensor(out=xm3[:], in0=y12[1][:], scalar=w12_all[:, 1, tile:tile + 1], in1=xm3[:],
                                                                           op0=ALU.mult, op1=ALU.add))(tile, xm3), reads=[y12[1], w12_all, xm3], writes=[xm3])
            if tile + 1 < NT:
                p3_gather(tile + 1)
            S.op("act", (lambda xm3: lambda g: g.copy(out=x2b[:], in_=xm3[:]))(xm3), reads=[xm3], writes=[x2b])
            for hh in range(2):
                bkb, bk = (B6b, banks[6]) if hh == 0 else (B5b, banks[5])
                for cc in range(8):
                    c = hh * 8 + cc
                    S.op("pe", (lambda c, cc, bkb: lambda g: g.transpose(out=bkb[:, cc * 128:(cc + 1) * 128], in_=x2b[:, c * 128:(c + 1) * 128],
                                                                      identity=ident_b[:]))(c, cc, bkb), reads=[x2b, ident_b], writes=[bk])
                if hh == 0:
                    S.op("act", (lambda bkb: lambda g: g.copy(out=x2T[:, 0:8, :], in_=bkb.rearrange("p (a b) -> p a b", a=8)))(bkb), reads=[bk], writes=[x2T])
                else:
                    S.op("dve", (lambda bkb: lambda g: g.tensor_copy(out=x2T[:, 8:16, :], in_=bkb.rearrange("p (a b) -> p a b", a=8)))(bkb), reads=[bk], writes=[x2T])
            for n in range(4):
                bk = banks[n % 2]
                for kc in range(2):
                    S.op("pe", (lambda kc, n, bk, pTb: lambda g: g.matmul(bk[:, :], lhsT=pTb[:, kc, :], rhs=wple[:, kc, n * 512:(n + 1) * 512],
                                                                          start=(kc == 0), stop=(kc == 1)))(kc, n, bk, pTb), reads=[pTb, wple], writes=[bk])
                S.op("act", (lambda n, bk: lambda g: g.copy(out=pler[:, n * 512:(n + 1) * 512], in_=bk[:, :]))(n, bk), reads=[bk], writes=[pler])
            S.op("act", (lambda s3: lambda g: g.activation(out=junk3[:], in_=pler[:], func=AF.Square, accum_out=s3[:, 0:1]))(s3), reads=[pler], writes=[junk3, s3])
            S.op("act", (lambda s3: lambda g: g.activation(out=s3[:, 1:2], in_=s3[:, 0:1], func=AF.Ln, scale=1.0 / D, bias=EPS))(s3), reads=[s3], writes=[s3])
            S.op("act", (lambda s3: lambda g: g.activation(out=s3[:, 2:3], in_=s3[:, 1:2], func=AF.Exp, scale=-0.5))(s3), reads=[s3], writes=[s3])
            S.op("dve", (lambda s3: lambda g: g.scalar_tensor_tensor(out=pler[:], in0=pler[:], scalar=s3[:, 2:3], in1=gple[:], op0=ALU.mult, op1=ALU.mult))(s3),
                 reads=[pler, s3, gple], writes=[pler])
            for n in range(4):
                bk = banks[2 + n % 2]
                for c in range(16):
                    S.op("pe", (lambda c, n, bk: lambda g: g.matmul(bk[:, :], lhsT=x2T[:, c, :], rhs=wpg[:, c, n * 512:(n + 1) * 512],
                                                                    start=(c == 0), stop=(c == 15)))(c, n, bk), reads=[x2T, wpg], writes=[bk])
                S.op("act", (lambda bk: lambda g: g.activation(out=eg[:], in_=bk[:, :], func=AF.Exp, scale=-1.0))(bk), reads=[bk], writes=[eg])
                S.op("act", lambda g: g.activation(out=eg[:], in_=eg[:], func=AF.Ln, bias=1.0), reads=[eg], writes=[eg])
                S.op("act", lambda g: g.activation(out=eg[:], in_=eg[:], func=AF.Exp, scale=-1.0), reads=[eg], writes=[eg])
                S.op("dve", (lambda n: lambda g: g.tensor_tensor(out=eg[:], in0=eg[:], in1=pler[:, n * 512:(n + 1) * 512], op=ALU.mult))(n),
                     reads=[eg, pler], writes=[eg])
                S.op("dve", (lambda n, xm3: lambda g: g.tensor_tensor(out=xm3[:, n * 512:(n + 1) * 512], in0=eg[:], in1=xm3[:, n * 512:(n + 1) * 512], op=ALU.add))(n, xm3),
                     reads=[eg, xm3], writes=[xm3])
            S.op("act", (lambda s3, xm3: lambda g: g.activation(out=junk3[:], in_=xm3[:], func=AF.Square, accum_out=s3[:, 4:5]))(s3, xm3), reads=[xm3], writes=[junk3, s3])
            S.op("act", (lambda s3: lambda g: g.activation(out=s3[:, 5:6], in_=s3[:, 4:5], func=AF.Ln, scale=1.0 / D, bias=EPS))(s3), reads=[s3], writes=[s3])
            S.op("act", (lambda s3: lambda g: g.activation(out=s3[:, 6:7], in_=s3[:, 5:6], func=AF.Exp, scale=-0.5))(s3), reads=[s3], writes=[s3])
            S.op("dve", (lambda s3, xm3: lambda g: g.scalar_tensor_tensor(out=xm3[:], in0=xm3[:], scalar=s3[:, 6:7], in1=gfin[:], op0=ALU.mult, op1=ALU.mult))(s3, xm3),
                 reads=[xm3, s3, gfin], writes=[xm3])
            S.dma("act", (lambda r0, xm3: lambda g: g.dma_start(out=out_d.ap()[r0:r0 + 128, :], in_=xm3[:]))(r0, xm3), reads=[xm3])
        S.emit()
    return nc


def prep_weights(g_mix, w_in, lb_logits, hg_norm, conv_w, sc_norm, w_out, g_ffn, w_router_group, w_router_expert,
                 w_gate, w_up, w_down, w_ple, g_ple, w_ple_gate, g_final):
    f = np.float32
    w_in = np.asarray(w_in[0], f)
    q, fz, iz, gz = (w_in[:, k * 1024:(k + 1) * 1024].reshape(D, 8, 128) for k in range(4))
    whg = np.ascontiguousarray(np.stack([q, gz, fz, iz], axis=2).transpose(1, 0, 2, 3).reshape(8, D, 512))
    Bw, Cw, Hw = (w_in[:, 4096 + k * 1024:4096 + (k + 1) * 1024].reshape(D, 8, 128) for k in range(3))
    wsc = np.ascontiguousarray(np.stack([Bw, Cw, Hw], axis=2).transpose(1, 0, 2, 3).reshape(8, D, 384))
    wr = np.concatenate([np.asarray(w_router_group[0], f), np.asarray(w_router_expert[0], f)], axis=1)
    wr = np.ascontiguousarray(wr.reshape(16, 128, 36).transpose(1, 0, 2))
    wg = np.asarray(w_gate[0], f).reshape(NEXP, 16, 128, 512).transpose(0, 2, 1, 3)
    wgt = np.ascontiguousarray(wg).reshape(NEXP * 128 * 4, 2048)
    wu = np.asarray(w_up[0], f).reshape(NEXP, 16, 128, 512).transpose(0, 2, 1, 3)
    wut = np.ascontiguousarray(wu).reshape(NEXP * 128 * 4, 2048)
    wd = np.asarray(w_down[0], f).reshape(NEXP, 4, 128, 2048).transpose(0, 2, 1, 3)
    wdt = np.ascontiguousarray(wd).reshape(NEXP * 128 * 4, 2048)
    cols = np.zeros((128, 64), f)
    cols[:, 0:16] = np.asarray(g_mix[0], f).reshape(16, 128).T
    cols[:, 16:32] = np.asarray(g_ffn[0], f).reshape(16, 128).T
    cols[:, 32:40] = np.asarray(sc_norm[0], f).reshape(8, 128).T
    cw = np.asarray(conv_w[0], f).reshape(3, 8, 128)
    cols[:, 40:64] = cw.transpose(2, 1, 0).reshape(128, 24)
    return {
        "whg": whg, "wsc": wsc, "wout": np.ascontiguousarray(np.asarray(w_out[0], f)), "wr": wr,
        "wgt": wgt, "wut": wut, "wdt": wdt,
        "wple": np.ascontiguousarray(np.asarray(w_ple[0], f)), "wpg": np.ascontiguousarray(np.asarray(w_ple_gate[0], f)),
        "lb": np.ascontiguousarray(np.asarray(lb_logits, f)), "hgn": np.asarray(hg_norm, f).reshape(1, 128),
        "gffn": np.asarray(g_ffn, f).reshape(1, D), "gple": np.asarray(g_ple, f).reshape(1, D),
        "gfin": np.asarray(g_final, f).reshape(1, D), "cols": cols,
        "zeros": np.zeros((256, D), ml_dtypes.bfloat16),
    }


_NC_CACHE = {}


def kernel(x, p, g_mix, w_in, lb_logits, hg_norm, conv_w, sc_norm, w_out, g_ffn, w_router_group, w_router_expert,
           w_gate, w_up, w_down, w_ple, g_ple, w_ple_gate, g_final):
    x = np.asarray(x, np.float32)
    p = np.asarray(p, np.float32)
    Bn, T, _ = x.shape
    half = T // 2
    wts = prep_weights(g_mix, w_in, lb_logits, hg_norm, conv_w, sc_norm, w_out, g_ffn, w_router_group, w_router_expert,
                       w_gate, w_up, w_down, w_ple, g_ple, w_ple_gate, g_final)
    if "nc" not in _NC_CACHE:
        _NC_CACHE["nc"] = build(NG=half // 512, NWG=half // 512)
    nc = _NC_CACHE["nc"]
    in_maps = []
    for c in range(8):
        b, hf = c // 2, c % 2
        rows = slice(hf * half, (hf + 1) * half)
        m = dict(wts)
        m["xT"] = np.ascontiguousarray(x[b, rows].T)
        m["xTp"] = np.ascontiguousarray(x[b, 0:half].T) if hf == 1 else np.zeros((D, half), np.float32)
        m["xtok"] = np.ascontiguousarray(x[b, rows])
        m["pT"] = np.ascontiguousarray(p[0, b, rows].T)
        in_maps.append(m)
    res = run_bass_kernel_spmd(nc, in_maps, core_ids=list(range(8)))
    out = np.empty((Bn, T, D), np.float32)
    for c in range(8):
        b, hf = c // 2, c % 2
        out[b, hf * half:(hf + 1) * half] = res.results[c]["out"]
    return out
```

```python
import numpy as np
import ml_dtypes
from contextlib import ExitStack
import concourse.bass as bass
import concourse.mybir as mybir
from concourse.bass_utils import run_bass_kernel_spmd

F32 = mybir.dt.float32
BF16 = mybir.dt.bfloat16
I32 = mybir.dt.int32
AF = mybir.ActivationFunctionType
ALU = mybir.AluOpType
AX = mybir.AxisListType

D = 2048
EPS = 1e-6
EPS_HG = 1e-6 * 128.0
NEXP = 32


class Buf:
    __slots__ = ("name", "t", "last_w", "readers", "aliases", "off", "excl")

    def __init__(self, name, t):
        self.name = name
        self.t = t
        self.last_w = None
        self.readers = {}
        self.aliases = []
        self.off = None
        self.excl = False

    def __getitem__(self, idx):
        return self.t[idx]


class Instr:
    __slots__ = ("eng", "fn", "deps", "is_dma", "sem", "val", "signal", "prewait")

    def __init__(self, eng, fn, deps, is_dma):
        self.eng = eng
        self.fn = fn
        self.deps = deps
        self.is_dma = is_dma
        self.sem = None
        self.val = None
        self.signal = False
        self.prewait = None


class Sched:
    ENGS = ("pe", "act", "dve", "pool", "sp")

    def __init__(self, nc, es, n_dma_sems=8):
        self.nc = nc
        self.streams = {e: [] for e in self.ENGS}
        self.esem = {e: es.enter_context(nc.semaphore("s_" + e)) for e in self.ENGS}
        self.dsems = {}
        for q in ("sp", "pool", "act"):
            self.dsems[q] = [es.enter_context(nc.semaphore("d_%s%d" % (q, i))) for i in range(n_dma_sems)]
        self.dcount = {q: 0 for q in self.dsems}
        self.duse = {q: [0] * n_dma_sems for q in self.dsems}
        self.dlast = {q: [None] * n_dma_sems for q in self.dsems}
        self.n_instr = 0

    @staticmethod
    def _expand(bufs):
        out = {}
        for b in bufs:
            out[id(b)] = b
            for a in b.aliases:
                out[id(a)] = a
        return list(out.values())

    def _deps(self, eng, reads, writes, is_dma):
        deps = {}
        for b in reads:
            if b.last_w is not None:
                deps[id(b.last_w)] = b.last_w
        for b in writes:
            if b.last_w is not None:
                deps[id(b.last_w)] = b.last_w
            for r in b.readers.values():
                deps[id(r)] = r
        out = []
        for d in deps.values():
            if (not is_dma) and (not d.is_dma) and d.eng == "pe" and eng == "pe":
                continue
            if not d.is_dma:
                d.signal = True
            out.append(d)
        return out

    def _commit(self, ins, reads, writes):
        wset = set(id(b) for b in writes)
        for b in reads:
            if id(b) in wset:
                continue
            key = ("dma", id(ins)) if ins.is_dma else ins.eng
            b.readers[key] = ins
        for b in writes:
            b.last_w = ins
            b.readers = {}
        self.n_instr += 1

    def op(self, eng, fn, reads=(), writes=()):
        reads = self._expand(reads)
        writes = self._expand(writes)
        ex = [b for b in reads if b.excl]
        if ex:
            writes = writes + [b for b in ex if all(b is not w for w in writes)]
        ins = Instr(eng, fn, self._deps(eng, reads, writes, False), False)
        self.streams[eng].append(ins)
        self._commit(ins, reads, writes)
        return ins

    def dma(self, q, fn, reads=(), writes=()):
        reads = self._expand(reads)
        writes = self._expand(writes)
        ins = Instr(q, fn, self._deps(q, reads, writes, True), True)
        n = self.dcount[q]
        k = n % len(self.dsems[q])
        self.dcount[q] += 1
        ins.prewait = self.dlast[q][k]
        self.duse[q][k] += 1
        ins.sem = self.dsems[q][k]
        ins.val = 16 * self.duse[q][k]
        self.dlast[q][k] = ins
        self.streams[q].append(ins)
        self._commit(ins, reads, writes)
        return ins

    def barrier(self):
        lasts = []
        for e in self.ENGS:
            for ins in reversed(self.streams[e]):
                if not ins.is_dma:
                    ins.signal = True
                    lasts.append(ins)
                    break
        for q in self.dsems:
            for last in self.dlast[q]:
                if last is not None:
                    lasts.append(last)
        for e in self.ENGS:
            ins = Instr(e, lambda h: h.nop(), list(lasts), False)
            self.streams[e].append(ins)

    def emit(self):
        nc = self.nc
        for e in self.ENGS:
            c = 0
            for ins in self.streams[e]:
                if not ins.is_dma and ins.signal:
                    c += 1
                    ins.sem = self.esem[e]
                    ins.val = c
        with nc.Block() as block:
            def run(e, h):
                waited = {}

                def wait(d):
                    k = id(d.sem)
                    if waited.get(k, 0) >= d.val:
                        return
                    waited[k] = d.val
                    h.wait_ge(d.sem, d.val)

                for ins in self.streams[e]:
                    if ins.is_dma and ins.prewait is not None:
                        wait(ins.prewait)
                    for d in ins.deps:
                        wait(d)
                    r = ins.fn(h)
                    if ins.is_dma:
                        r.then_inc(ins.sem, 16)
                    elif ins.signal:
                        r.then_inc(ins.sem, 1)
                if e in self.dsems:
                    for last in self.dlast[e]:
                        if last is not None:
                            wait(last)

            @block.tensor
            def _(h):
                run("pe", h)

            @block.scalar
            def _(h):
                run("act", h)

            @block.vector
            def _(h):
                run("dve", h)

            @block.gpsimd
            def _(h):
                run("pool", h)

            @block.sync
            def _(h):
                run("sp", h)


class Arena:
    def __init__(self, t, words):
        self.t = t
        self.words = words
        self.off = 0
        self.n = 0

    def alloc(self, name, shape, dt=F32, at=None):
        n = 1
        for s in shape:
            n *= s
        esz = 4 if dt in (F32, I32) else 2
        w = (n * esz + 3) // 4
        w = (w + 7) // 8 * 8
        off = self.off if at is None else at
        assert off + w <= self.words, "arena overflow at %s: %d + %d > %d" % (name, off, w, self.words)
        ap = self.t[:, off:off + w]
        if dt != F32:
            ap = ap.bitcast(dt)
        ap = ap[:, 0:n]
        if len(shape) == 2:
            ap = ap.rearrange("p (a b) -> p a b", a=shape[0])
        elif len(shape) == 3:
            ap = ap.rearrange("p (a b c) -> p a b c", a=shape[0], b=shape[1])
        if at is None:
            self.off += w
        self.n += 1
        b = Buf(name, ap)
        b.off = off
        return b


def build(NG=8, NWG=8, debug=False, stop=99):
    nc = bass.Bass("TRN2", target_bir_lowering=False)
    TOK = NG * 512
    WTOK = NWG * 512
    NT = TOK // 128
    NB = -(-(2 * TOK + NEXP * 255) // 256)
    PR = NB * 256
    okind = "ExternalOutput" if debug else "Internal"

    def din(name, shape, dt=F32):
        return nc.dram_tensor(name, shape, dt, kind="ExternalInput")

    xT_d = din("xT", [D, TOK])
    xTp_d = din("xTp", [D, WTOK])
    xtok_d = din("xtok", [TOK, D])
    pT_d = din("pT", [256, TOK])
    whg_d = din("whg", [8, D, 512])
    wsc_d = din("wsc", [8, D, 384])
    wout_d = din("wout", [D, D])
    wr_d = din("wr", [128, 16, 36])
    wgt_d = din("wgt", [NEXP * 128 * 4, 2048])
    wut_d = din("wut", [NEXP * 128 * 4, 2048])
    wdt_d = din("wdt", [NEXP * 128 * 4, 2048])
    wple_d = din("wple", [256, D])
    wpg_d = din("wpg", [D, D])
    lb_d = din("lb", [2, 1024])
    hgn_d = din("hgn", [1, 128])
    gffn_d = din("gffn", [1, D])
    gple_d = din("gple", [1, D])
    gfin_d = din("gfin", [1, D])
    cols_d = din("cols", [128, 64])
    zeros_d = din("zeros", [256, D], BF16)
    out_d = nc.dram_tensor("out", [TOK, D], F32, kind="ExternalOutput")
    xmid_d = nc.dram_tensor("xmid", [TOK, D], F32, kind=okind)
    hn_d = nc.dram_tensor("hn", [TOK, D], BF16, kind="Internal")
    xs_d = nc.dram_tensor("xs", [PR, D], BF16, kind="Internal")
    ys_d = nc.dram_tensor("ys", [PR, D], F32, kind="Internal")
    if debug:
        dbg_d = nc.dram_tensor("dbg", [128, 8, NT], F32, kind="ExternalOutput")
        dbgb_d = nc.dram_tensor("dbgb", [128, NB], F32, kind="ExternalOutput")

    with ExitStack() as es:
        S = Sched(nc, es)
        AW = 51200
        arena_t = es.enter_context(nc.sbuf_tensor("arena", [128, AW], F32))
        A = Arena(arena_t, AW)
        banks = [Buf("bank%d" % i, es.enter_context(nc.psum_tensor("bank%d" % i, [128, 512], F32))) for i in range(8)]
        for bk_ in banks:
            bk_.excl = True
        B6b = banks[6].t[:, :].bitcast(BF16)
        B5b = banks[5].t[:, :].bitcast(BF16)

        ident_f = A.alloc("ident_f", [128])
        ident_b = A.alloc("ident_b", [128], BF16)
        tri2 = A.alloc("tri2", [128])
        stri = A.alloc("stri", [128])
        ones_f = A.alloc("ones_f", [128])
        ones_b = A.alloc("ones_b", [128], BF16)
        w12_all = A.alloc("w12_all", [2, NT])
        dest_f = A.alloc("dest_f", [2, NT])
        dest_i = A.alloc("dest_i", [2, NT], I32)
        idxw = A.alloc("idxw", [4, NB], I32)
        p4 = A.alloc("p4", [4])
        true_persist_mark = A.off
        ind2 = A.alloc("ind2", [2])
        oml = A.alloc("oml", [1024])
        hgn = A.alloc("hgn", [128])
        gffn = A.alloc("gffn", [D])
        cols = A.alloc("cols", [64])
        wr = A.alloc("wr", [16, 36])
        Sst = [A.alloc("S%d" % h, [128]) for h in range(8)]
        Sbf = [A.alloc("Sb%d" % h, [128], BF16) for h in range(8)]
        tails = A.alloc("tails", [8, 2])
        oh1_all = A.alloc("oh1_all", [NT, 32])
        oh2_all = A.alloc("oh2_all", [NT, 32])
        rank_all = A.alloc("rank_all", [NT, 32])
        Csum = A.alloc("Csum", [32])
        gmix_c = lambda c: cols[:, c:c + 1]
        gffn_c = lambda c: cols[:, 16 + c:17 + c]
        scn_c = lambda j: cols[:, 32 + j:33 + j]
        cw_c = lambda j, k: cols[:, 40 + 3 * j + k:41 + 3 * j + k]

        def memset(eng, buf, ap, val):
            S.op(eng, lambda g: g.memset(ap, val), writes=[buf])

        memset("pool", ident_f, ident_f[:], 1.0)
        S.op("pool", lambda g: g.affine_select(out=ident_f[:], in_=ident_f[:], pattern=[[-1, 128]], compare_op=ALU.is_equal,
                                                fill=0.0, base=0, channel_multiplier=1), reads=[ident_f], writes=[ident_f])
        S.op("dve", lambda g: g.tensor_copy(out=ident_b[:], in_=ident_f[:]), reads=[ident_f], writes=[ident_b])
        memset("pool", tri2, tri2[:], 1.0)
        S.op("pool", lambda g: g.affine_select(out=tri2[:], in_=tri2[:], pattern=[[1, 128]], compare_op=ALU.is_ge,
                                                fill=0.0, base=0, channel_multiplier=-1), reads=[tri2], writes=[tri2])
        memset("pool", tri2, tri2[0:64, 64:128], 0.0)
        memset("pool", stri, stri[:], 1.0)
        S.op("pool", lambda g: g.affine_select(out=stri[:], in_=stri[:], pattern=[[1, 128]], compare_op=ALU.is_ge,
                                                fill=0.0, base=-1, channel_multiplier=-1), reads=[stri], writes=[stri])
        memset("pool", ones_f, ones_f[:], 1.0)
        memset("pool", ones_b, ones_b[:], 1.0)
        memset("pool", ind2, ind2[:], 0.0)
        memset("pool", ind2, ind2[0:64, 0:1], 1.0)
        memset("pool", ind2, ind2[64:128, 1:2], 1.0)
        memset("pool", tails, tails[:], 0.0)
        memset("pool", Csum, Csum[:], 0.0)
        for h in range(8):
            memset("pool", Sst[h], Sst[h][:], 0.0)
            memset("pool", Sbf[h], Sbf[h][:], 0.0)
        S.op("pool", lambda g: g.iota(p4[:], pattern=[[1, 4]], base=0, channel_multiplier=4,
                                      allow_small_or_imprecise_dtypes=True), writes=[p4])
        tmp_lb = A.alloc("tmp_lb", [2, 1024])
        S.dma("sp", lambda g: g.dma_start(out=tmp_lb[:, 0, :], in_=lb_d.ap()[0:1, :].partition_broadcast(128)), writes=[tmp_lb])
        S.dma("sp", lambda g: g.dma_start(out=tmp_lb[:, 1, :], in_=lb_d.ap()[1:2, :].partition_broadcast(128)), writes=[tmp_lb])
        S.dma("sp", lambda g: g.dma_start(out=hgn[:], in_=hgn_d.ap().partition_broadcast(128)), writes=[hgn])
        S.dma("sp", lambda g: g.dma_start(out=gffn[:], in_=gffn_d.ap().partition_broadcast(128)), writes=[gffn])
        S.dma("sp", lambda g: g.dma_start(out=cols[:], in_=cols_d.ap()), writes=[cols])
        S.dma("sp", lambda g: g.dma_start(out=wr[:], in_=wr_d.ap()), writes=[wr])
        S.op("dve", lambda g: g.tensor_tensor(out=oml[:], in0=tmp_lb[:, 0, :], in1=tmp_lb[:, 1, :], op=ALU.subtract),
             reads=[tmp_lb], writes=[oml])
        S.op("act", lambda g: g.activation(out=oml[:], in_=oml[:], func=AF.Exp), reads=[oml], writes=[oml])
        S.op("dve", lambda g: g.tensor_scalar(out=oml[:], in0=oml[:], scalar1=1.0, scalar2=None, op0=ALU.add), reads=[oml], writes=[oml])
        S.op("dve", lambda g: g.reciprocal(out=oml[:], in_=oml[:]), reads=[oml], writes=[oml])
        for c in range(16):
            S.op("pool", (lambda c: lambda g: g.tensor_scalar(out=wr[:, c, :], in0=wr[:, c, :], scalar1=gffn_c(c), scalar2=None,
                                                              op0=ALU.mult))(c), reads=[wr, cols], writes=[wr])
        A.off -= 2048 + 0
        S.barrier()
        persist_mark = A.off

        wslab = [A.alloc("wslab%d" % i, [16, 512], BF16) for i in range(2)]
        xb = A.alloc("xb", [16, 512], BF16)
        xp = [A.alloc("xp%d" % i, [2, 512]) for i in range(4)]
        xmid_off = None
        sq = [A.alloc("sq%d" % i, [2, 512], BF16) for i in range(2)]
        omixT = A.alloc("omixT", [16, 512], BF16)
        byb = A.alloc("byb", [8, 512], BF16)
        ubuf = A.alloc("ubuf", [514])
        t1 = A.alloc("t1", [512])
        yv = A.alloc("yv", [512])
        yv2 = A.alloc("yv2", [512])
        sqy = A.alloc("sqy", [512], BF16)
        rstd_bc = A.alloc("rstd_bc", [512])
        rstd2_bc = A.alloc("rstd2_bc", [512])
        rstd_sc = A.alloc("rstd_sc", [512])
        lnbc = A.alloc("lnbc", [512])
        rcol = A.alloc("rcol", [8])
        xmid = [A.alloc("xmid0", [D], at=xb.off), A.alloc("xmid1", [D], at=xb.off + 2048),
                A.alloc("xmid2", [D], at=xp[0].off), A.alloc("xmid3", [D], at=xp[2].off)]
        assert xp[1].off == xp[0].off + 1024 and xp[3].off == xp[2].off + 1024
        for xm_, al_ in ((xmid[0], [xb]), (xmid[1], [xb]), (xmid[2], [xp[0], xp[1]]), (xmid[3], [xp[2], xp[3]])):
            xm_.aliases = list(al_)
            for a_ in al_:
                a_.aliases.append(xm_)
        xmT = A.alloc("xmT", [16, 128])
        hnb = A.alloc("hnb", [D], BF16)
        junk = A.alloc("junk", [D], BF16)
        etmp = [A.alloc("etmp%d" % t, [256]) for t in range(2)]
        qg = [[A.alloc("qg%d_%d" % (p_, t), [256]) for t in range(4)] for p_ in range(2)]
        kk = [[A.alloc("kk%d_%d" % (p_, t), [128]) for t in range(4)] for p_ in range(2)]
        lf = [[A.alloc("lf%d_%d" % (p_, t), [128]) for t in range(4)] for p_ in range(2)]
        vb = [[A.alloc("vb%d_%d" % (p_, t), [128], BF16) for t in range(4)] for p_ in range(2)]
        NTMP = 3
        eA = [A.alloc("eA%d" % i, [128]) for i in range(NTMP)]
        enA = [A.alloc("enA%d" % i, [128]) for i in range(NTMP)]
        Ecol = [A.alloc("Ecol%d" % i, [2]) for i in range(NTMP)]
        qt = [A.alloc("qt%d" % i, [128], BF16) for i in range(NTMP)]
        kt = [A.alloc("kt%d" % i, [128], BF16) for i in range(NTMP)]
        qtT = [A.alloc("qtT%d" % i, [128], BF16) for i in range(NTMP)]
        ktT = [A.alloc("ktT%d" % i, [128], BF16) for i in range(NTMP)]
        scTm = [A.alloc("scTm%d" % i, [128], BF16) for i in range(NTMP)]
        U0 = [A.alloc("U0%d" % i, [128]) for i in range(NTMP)]
        S1 = [A.alloc("S1%d" % i, [128]) for i in range(NTMP)]
        S1b = [A.alloc("S1b%d" % i, [128], BF16) for i in range(NTMP)]
        gsn = [A.alloc("gsn%d" % i, [128]) for i in range(NTMP)]
        ogb = [A.alloc("ogb%d" % i, [128], BF16) for i in range(NTMP)]
        sm = [A.alloc("sm%d" % i, [8]) for i in range(NTMP)]
        lg = A.alloc("lg", [36])
        rt = A.alloc("rt", [64])
        Ct = A.alloc("Ct", [32])

        import os
        _dbg2 = os.environ.get("K_DBG2", "")
        _dbg3 = os.environ.get("K_DBG3", "")

        def load_x_group(src_d, g0):
            for i in range(8):
                xpi = xp[i % 4]
                sqi = sq[i % 2]
                S.dma("sp", (lambda i, xpi: lambda g: g.dma_start(
                    out=xpi[:], in_=src_d.ap()[2 * i * 128:(2 * i + 2) * 128, g0 * 512:(g0 + 1) * 512]
                    .rearrange("(c p) t -> p c t", p=128)))(i, xpi), writes=[xpi])
                S.op("act", (lambda xpi, sqi: lambda g: g.activation(out=sqi[:], in_=xpi[:], func=AF.Square))(xpi, sqi),
                     reads=[xpi], writes=[sqi])
                for cc in range(2):
                    c = 2 * i + cc
                    S.op("act", (lambda c, cc, xpi: lambda g: g.activation(out=xb[:, c, :], in_=xpi[:, cc, :], func=AF.Copy, scale=gmix_c(c)))(c, cc, xpi),
                         reads=[xpi, cols], writes=[xb])
                    S.op("pe", (lambda c, cc, sqi: lambda g: g.matmul(banks[7][:, :], lhsT=ones_b[:], rhs=sqi[:, cc, :],
                                                                      start=(c == 0), stop=(c == 15)))(c, cc, sqi),
                         reads=[sqi, ones_b], writes=[banks[7]])
            S.op("act", lambda g: g.activation(out=lnbc[:], in_=banks[7][:, :], func=AF.Ln, scale=1.0 / D, bias=EPS),
                 reads=[banks[7]], writes=[lnbc])
            S.op("act", lambda g: g.activation(out=rstd_bc[:], in_=lnbc[:], func=AF.Exp, scale=-0.5), reads=[lnbc], writes=[rstd_bc])
            S.op("act", lambda g: g.activation(out=rstd2_bc[:], in_=lnbc[:], func=AF.Exp, scale=-1.0), reads=[lnbc], writes=[rstd2_bc])
            if _dbg2 == "nok1":
                return
            for t in range(4):
                S.op("pe", (lambda t: lambda g: g.transpose(out=banks[3][:, 384 + 32 * t:384 + 32 * (t + 1)], in_=rstd_bc[0:32, t * 128:(t + 1) * 128],
                                                            identity=ident_f[0:32, 0:32]))(t),
                     reads=[rstd_bc, ident_f], writes=[banks[3]])
            S.op("dve", lambda g: g.tensor_copy(out=rcol[:, 0:4], in_=banks[3][:, 384:512].rearrange("p (a b) -> p a b", a=4)[:, :, 0]),
                 reads=[banks[3]], writes=[rcol])
            S.op("dve", lambda g: g.tensor_scalar(out=rcol[:, 4:8], in0=rcol[:, 0:4], scalar1=-1.0, scalar2=None, op0=ALU.mult),
                 reads=[rcol], writes=[rcol])


        slab_plan = []
        slab_state = {"i": 0, "issued": 0}

        def plan_slabs():
            for wg in range(NWG):
                for h in range(8):
                    slab_plan.append((whg_d.ap()[h, :, 256:512].rearrange("(c p) n -> p c n", p=128), 256))
                if wg == NWG - 1:
                    for j in range(8):
                        slab_plan.append((wsc_d.ap()[j, :, :].rearrange("(c p) n -> p c n", p=128), 384))
            for g0 in range(NG):
                for h in range(8):
                    slab_plan.append((whg_d.ap()[h, :, :].rearrange("(c p) n -> p c n", p=128), 512))
                for j in range(8):
                    slab_plan.append((wsc_d.ap()[j, :, :].rearrange("(c p) n -> p c n", p=128), 384))
                for n in range(4):
                    slab_plan.append((wout_d.ap()[:, n * 512:(n + 1) * 512].rearrange("(c p) n -> p c n", p=128), 512))

        def issue_slab(i):
            src_ap, ncols = slab_plan[i]
            ws = wslab[i % 2]
            S.dma("pool", (lambda ws, src_ap, ncols: lambda g: g.dma_start(out=ws[:, :, 0:ncols], in_=src_ap))(ws, src_ap, ncols), writes=[ws])

        def next_slab(ncols_expected):
            i = slab_state["i"]
            slab_state["i"] += 1
            while slab_state["issued"] <= min(i + 1, len(slab_plan) - 1):
                issue_slab(slab_state["issued"])
                slab_state["issued"] += 1
            assert slab_plan[i][1] == ncols_expected, (i, slab_plan[i][1], ncols_expected)
            return wslab[i % 2]

        tmp_ctr = {"n": 0, "e": 0}

        def gen_inproj(h, full, par):
            c0 = 0 if full else 256
            ncols = 512 if full else 256
            ws = next_slab(ncols)
            oml_h = oml[:, h * 128:(h + 1) * 128]
            fo = 256 - c0
            for t in range(4):
                bk = banks[t % 2]
                for c in range(16):
                    S.op("pe", (lambda c, t, bk: lambda g: g.matmul(bk[:, 0:ncols], lhsT=xb[:, c, t * 128:(t + 1) * 128],
                                                                     rhs=ws[:, c, 0:ncols], start=(c == 0), stop=(c == 15)))(c, t, bk),
                         reads=[xb, ws], writes=[bk])
                    if c % 2 == 1 and c < 15:
                        yield
                rs = rcol[:, t:t + 1]
                nrs = rcol[:, 4 + t:5 + t]
                qg_t, kk_t, vb_t, lf_t = qg[par][t], kk[par][t], vb[par][t], lf[par][t]
                et = etmp[tmp_ctr["e"] % 2]
                tmp_ctr["e"] += 1
                if full:
                    S.op("act", (lambda et, bk, nrs: lambda g: g.activation(out=et[:], in_=bk[:, 0:256], func=AF.Exp, scale=nrs))(et, bk, nrs),
                         reads=[bk, rcol], writes=[et])
                S.op("act", (lambda kk_t, bk, rs: lambda g: g.activation(out=kk_t[:], in_=bk[:, fo:fo + 128], func=AF.Exp, scale=rs))(kk_t, bk, rs),
                     reads=[bk, rcol], writes=[kk_t])
                S.op("dve", (lambda vb_t, bk, rs: lambda g: g.tensor_scalar(out=vb_t[:], in0=bk[:, fo + 128:fo + 256], scalar1=rs, scalar2=None,
                                                                        op0=ALU.mult))(vb_t, bk, rs), reads=[bk, rcol], writes=[vb_t])
                if full:
                    S.op("act", (lambda et: lambda g: g.activation(out=et[:], in_=et[:], func=AF.Ln, bias=1.0))(et), reads=[et], writes=[et])
                    S.op("act", (lambda et: lambda g: g.activation(out=et[:], in_=et[:], func=AF.Exp, scale=-1.0))(et), reads=[et], writes=[et])
                    S.op("dve", (lambda qg_t, bk, rs, et: lambda g: g.scalar_tensor_tensor(out=qg_t[:], in0=bk[:, 0:256], scalar=rs, in1=et[:],
                                                                                       op0=ALU.mult, op1=ALU.mult))(qg_t, bk, rs, et),
                         reads=[bk, rcol, et], writes=[qg_t])
                S.op("act", (lambda kk_t: lambda g: g.activation(out=kk_t[:], in_=kk_t[:], func=AF.Ln, bias=1.0))(kk_t), reads=[kk_t], writes=[kk_t])
                S.op("act", (lambda kk_t: lambda g: g.activation(out=kk_t[:], in_=kk_t[:], func=AF.Exp, scale=-1.0))(kk_t), reads=[kk_t], writes=[kk_t])
                S.op("dve", (lambda kk_t: lambda g: g.tensor_tensor(out=kk_t[:], in0=kk_t[:], in1=oml_h, op=ALU.mult))(kk_t),
                     reads=[kk_t, oml], writes=[kk_t])
                S.op("act", (lambda kk_t, lf_t: lambda g: g.activation(out=lf_t[:], in_=kk_t[:], func=AF.Ln, scale=-1.0, bias=1.0))(kk_t, lf_t),
                     reads=[kk_t], writes=[lf_t])
                yield

        B2b = banks[2].t[:, :].bitcast(BF16)

        def gen_pre(h, t, full, par, i):
            b3 = banks[3]
            qg_t, kk_t, lf_t = qg[par][t], kk[par][t], lf[par][t]
            S.op("pe", lambda g: g.matmul(b3[:, 0:128], lhsT=tri2[:], rhs=lf_t[:], start=True, stop=True), reads=[tri2, lf_t], writes=[b3])
            S.op("pe", lambda g: g.matmul(b3[:, 128:130], lhsT=lf_t[:], rhs=ind2[:], start=True, stop=True), reads=[lf_t, ind2], writes=[b3])
            yield
            S.op("act", lambda g: g.activation(out=enA[i][:], in_=b3[:, 0:128], func=AF.Exp, scale=-1.0), reads=[b3], writes=[enA[i]])
            if full:
                S.op("act", lambda g: g.activation(out=eA[i][:], in_=b3[:, 0:128], func=AF.Exp), reads=[b3], writes=[eA[i]])
            S.op("act", lambda g: g.activation(out=Ecol[i][:], in_=b3[:, 128:130], func=AF.Exp), reads=[b3], writes=[Ecol[i]])
            S.op("dve", lambda g: g.tensor_tensor(out=kt[i][:], in0=kk_t[:], in1=enA[i][:], op=ALU.mult), reads=[kk_t, enA[i]], writes=[kt[i]])
            if not full:
                return
            S.op("dve", lambda g: g.tensor_tensor(out=qt[i][:], in0=qg_t[:, 0:128], in1=eA[i][:], op=ALU.mult), reads=[qg_t, eA[i]], writes=[qt[i]])
            S.op("pool", lambda g: g.tensor_tensor(out=gsn[i][:], in0=qg_t[:, 128:256], in1=hgn[:], op=ALU.mult), reads=[qg_t, hgn], writes=[gsn[i]])
            S.op("pe", lambda g: g.transpose(out=B6b[:, 0:128], in_=qt[i][:], identity=ident_b[:]), reads=[qt[i], ident_b], writes=[banks[6]])
            S.op("pe", lambda g: g.transpose(out=B6b[:, 128:256], in_=kt[i][:], identity=ident_b[:]), reads=[kt[i], ident_b], writes=[banks[6]])
            yield
            S.op("act", lambda g: g.copy(out=qtT[i][:], in_=B6b[:, 0:128]), reads=[banks[6]], writes=[qtT[i]])
            S.op("act", lambda g: g.copy(out=ktT[i][:], in_=B6b[:, 128:256]), reads=[banks[6]], writes=[ktT[i]])
            S.op("pe", lambda g: g.matmul(b3[:, 256:384], lhsT=ktT[i][:], rhs=qtT[i][:], start=True, stop=True), reads=[ktT[i], qtT[i]], writes=[b3])
            yield
            S.op("dve", lambda g: g.tensor_tensor(out=scTm[i][:], in0=b3[:, 256:384], in1=tri2[:], op=ALU.mult), reads=[b3, tri2], writes=[scTm[i]])

        def gen_state(h, t, full, par, i):
            Sh, Sb = Sst[h], Sbf[h]
            b4, b5 = banks[4], banks[5]
            vb_t = vb[par][t]
            S.op("dve", lambda g: g.tensor_scalar(out=U0[i][:], in0=Sh[:], scalar1=Ecol[i][:, 0:1], scalar2=None, op0=ALU.mult),
                 reads=[Sh, Ecol[i]], writes=[U0[i]])
            S.op("pe", lambda g: g.matmul(b5[:, 0:128], lhsT=kt[i][0:64, :], rhs=vb_t[0:64, :], start=True, stop=True), reads=[kt[i], vb_t], writes=[b5])
            yield
            S.op("dve", lambda g: g.scalar_tensor_tensor(out=S1[i][:], in0=b5[:, 0:128], scalar=Ecol[i][:, 0:1], in1=U0[i][:], op0=ALU.mult, op1=ALU.add),
                 reads=[b5, Ecol[i], U0[i]], writes=[S1[i]])
            S.op("dve", lambda g: g.tensor_scalar(out=U0[i][:], in0=S1[i][:], scalar1=Ecol[i][:, 1:2], scalar2=None, op0=ALU.mult),
                 reads=[S1[i], Ecol[i]], writes=[U0[i]])
            if full:
                S.op("act", lambda g: g.copy(out=S1b[i][:], in_=S1[i][:]), reads=[S1[i]], writes=[S1b[i]])
                S.op("pe", lambda g: g.matmul(b4[:, 0:128], lhsT=scTm[i][:], rhs=vb_t[:], start=True, stop=False), reads=[scTm[i], vb_t], writes=[b4])
                S.op("pe", lambda g: g.matmul(b4[0:64, 0:128], lhsT=qtT[i][:, 0:64], rhs=Sb[:], start=False, stop=True), reads=[qtT[i], Sb], writes=[b4])
                S.op("pe", lambda g: g.matmul(b4[64:128, 0:128], lhsT=qtT[i][:, 64:128], rhs=S1b[i][:], start=False, stop=True),
                     reads=[qtT[i], S1b[i]], writes=[b4])
            S.op("pe", lambda g: g.matmul(b5[:, 128:256], lhsT=kt[i][64:128, :], rhs=vb_t[64:128, :], start=True, stop=True), reads=[kt[i], vb_t], writes=[b5])
            yield
            S.op("dve", lambda g: g.scalar_tensor_tensor(out=Sh[:], in0=b5[:, 128:256], scalar=Ecol[i][:, 1:2], in1=U0[i][:], op0=ALU.mult, op1=ALU.add),
                 reads=[b5, Ecol[i], U0[i]], writes=[Sh])
            if not full:
                return
            S.op("act", lambda g: g.copy(out=Sb[:], in_=Sh[:]), reads=[Sh], writes=[Sb])
            S.op("act", lambda g: g.activation(out=junk[:, 0:128], in_=b4[:, 0:128], func=AF.Square, accum_out=sm[i][:, 0:1]), reads=[b4], writes=[junk, sm[i]])
            S.op("act", lambda g: g.activation(out=sm[i][:, 1:2], in_=sm[i][:, 0:1], func=AF.Ln, scale=1.0 / 128, bias=EPS_HG), reads=[sm[i]], writes=[sm[i]])
            S.op("act", lambda g: g.activation(out=sm[i][:, 2:3], in_=sm[i][:, 1:2], func=AF.Exp, scale=-0.5), reads=[sm[i]], writes=[sm[i]])
            S.op("dve", lambda g: g.scalar_tensor_tensor(out=ogb[i][:], in0=b4[:, 0:128], scalar=sm[i][:, 2:3], in1=gsn[i][:], op0=ALU.mult, op1=ALU.mult),
                 reads=[b4, sm[i], gsn[i]], writes=[ogb[i]])
            S.op("pe", lambda g: g.transpose(out=B2b[:, 0:128], in_=ogb[i][:], identity=ident_b[:]), reads=[ogb[i], ident_b], writes=[banks[2]])
            yield
            S.op("act", lambda g: g.copy(out=omixT[:, h, t * 128:(t + 1) * 128], in_=B2b[:, 0:128]), reads=[banks[2]], writes=[omixT])

        def exhaust(gen):
            if gen is None:
                return
            for _ in gen:
                pass

        def roundrobin(gens, steps):
            live = [g is not None for g in gens]
            while any(live[:-1]):
                for k, g in enumerate(gens):
                    if not live[k]:
                        continue
                    for _ in range(steps[k]):
                        try:
                            next(g)
                        except StopIteration:
                            live[k] = False
                            break

        def run_heads(full):
            exhaust(gen_inproj(0, full, 0))
            units = [(h, t) for h in range(8) for t in range(4)]
            bg = None
            prev = None
            for s_i in range(len(units) + 1):
                cur = units[s_i] if s_i < len(units) else None
                if cur is not None and cur[1] == 1 and cur[0] + 1 < 8:
                    bg = gen_inproj(cur[0] + 1, full, (cur[0] + 1) % 2)
                if cur is not None and cur[1] == 0 and bg is not None:
                    exhaust(bg)
                    bg = None
                gs = []
                if prev is not None:
                    u = s_i - 1
                    gs.append(gen_state(prev[0], prev[1], full, prev[0] % 2, u % NTMP))
                if cur is not None:
                    gs.append(gen_pre(cur[0], cur[1], full, cur[0] % 2, s_i % NTMP))
                gs.append(bg)
                roundrobin(gs, [1] * (len(gs) - 1) + [3])
                prev = cur
            exhaust(bg)

        sc_ctr = {"n": 0}

        def sc_chunk(j, tail_only=False):
            ws = next_slab(384)
            bset = [banks[0], banks[1], banks[2]] if sc_ctr["n"] % 2 == 0 else [banks[3], banks[4], banks[5]]
            sc_ctr["n"] += 1
            for part in range(3):
                if tail_only and part == 0:
                    continue
                bk = bset[part]
                for c in range(16):
                    S.op("pe", (lambda c, part, bk: lambda g: g.matmul(bk[:, :], lhsT=ws[:, c, part * 128:(part + 1) * 128], rhs=xb[:, c, :],
                                                                        start=(c == 0), stop=(c == 15)))(c, part, bk),
                         reads=[ws, xb], writes=[bk])
            S.op("pool", lambda g: g.tensor_copy(out=ubuf[:, 0:2], in_=tails[:, j, :]), reads=[tails], writes=[ubuf])
            S.op("dve", lambda g: g.tensor_tensor(out=t1[:], in0=bset[1][:, :], in1=rstd2_bc[:], op=ALU.mult),
                 reads=[bset[1], rstd2_bc], writes=[t1])
            S.op("dve", lambda g: g.tensor_tensor(out=ubuf[:, 2:514], in0=t1[:], in1=bset[2][:, :], op=ALU.mult),
                 reads=[bset[2], t1], writes=[ubuf])
            S.op("pool", lambda g: g.tensor_copy(out=tails[:, j, :], in_=ubuf[:, 512:514]), reads=[ubuf], writes=[tails])
            if tail_only:
                return
            S.op("act", lambda g: g.activation(out=yv[:], in_=ubuf[:, 0:512], func=AF.Copy, scale=cw_c(j, 0)), reads=[ubuf, cols], writes=[yv])
            S.op("dve", lambda g: g.scalar_tensor_tensor(out=yv[:], in0=ubuf[:, 1:513], scalar=cw_c(j, 1), in1=yv[:], op0=ALU.mult, op1=ALU.add),
                 reads=[ubuf, cols, yv], writes=[yv])
            S.op("dve", lambda g: g.scalar_tensor_tensor(out=yv[:], in0=ubuf[:, 2:514], scalar=cw_c(j, 2), in1=yv[:], op0=ALU.mult, op1=ALU.add),
                 reads=[ubuf, cols, yv], writes=[yv])
            S.op("dve", lambda g: g.tensor_tensor(out=yv[:], in0=yv[:], in1=rstd_bc[:], op=ALU.mult), reads=[yv, rstd_bc], writes=[yv])
            S.op("dve", lambda g: g.tensor_tensor(out=byb[:, j, :], in0=bset[0][:, :], in1=yv[:], op=ALU.mult),
                 reads=[bset[0], yv], writes=[byb])
            S.op("act", lambda g: g.activation(out=sqy[:], in_=byb[:, j, :], func=AF.Square), reads=[byb], writes=[sqy])
            S.op("pe", lambda g: g.matmul(banks[7][:, :], lhsT=ones_b[:], rhs=sqy[:], start=(j == 0), stop=(j == 7)),
                 reads=[ones_b, sqy], writes=[banks[7]])

        def sc_finish():
            S.op("act", lambda g: g.activation(out=lnbc[:], in_=banks[7][:, :], func=AF.Ln, scale=1.0 / 1024, bias=EPS),
                 reads=[banks[7]], writes=[lnbc])
            S.op("act", lambda g: g.activation(out=rstd_sc[:], in_=lnbc[:], func=AF.Exp, scale=-0.5), reads=[lnbc], writes=[rstd_sc])
            for j in range(8):
                S.op("dve", (lambda j: lambda g: g.scalar_tensor_tensor(out=omixT[:, 8 + j, :], in0=byb[:, j, :], scalar=scn_c(j), in1=rstd_sc[:],
                                                                        op0=ALU.mult, op1=ALU.mult))(j),
                     reads=[byb, cols, rstd_sc], writes=[omixT])

        def outproj_and_route(g0):
            for t in range(4):
                r0 = g0 * 512 + t * 128
                S.dma("sp", (lambda t, r0: lambda g: g.dma_start(out=xmid[t][:], in_=xtok_d.ap()[r0:r0 + 128, :]))(t, r0), writes=[xmid[t]])
            k = 0
            for n in range(4):
                ws = next_slab(512)
                for t in range(4):
                    bk = banks[k % 3]
                    k += 1
                    for c in range(16):
                        S.op("pe", (lambda c, t, bk, ws: lambda g: g.matmul(bk[:, :], lhsT=omixT[:, c, t * 128:(t + 1) * 128], rhs=ws[:, c, :],
                                                                            start=(c == 0), stop=(c == 15)))(c, t, bk, ws),
                             reads=[omixT, ws], writes=[bk])
                    S.op("dve", (lambda t, n, bk: lambda g: g.tensor_tensor(out=xmid[t][:, n * 512:(n + 1) * 512], in0=bk[:, :],
                                                                           in1=xmid[t][:, n * 512:(n + 1) * 512], op=ALU.add))(t, n, bk),
                         reads=[bk, xmid[t]], writes=[xmid[t]])
            for t in range(4):
                tile = g0 * 4 + t
                r0 = tile * 128
                xm = xmid[t]
                S.dma("act", (lambda xm, r0: lambda g: g.dma_start(out=xmid_d.ap()[r0:r0 + 128, :], in_=xm[:]))(xm, r0), reads=[xm])
                S.op("act", (lambda xm: lambda g: g.activation(out=junk[:], in_=xm[:], func=AF.Square, accum_out=rt[:, 0:1]))(xm),
                     reads=[xm], writes=[junk, rt])
                S.op("act", lambda g: g.activation(out=rt[:, 1:2], in_=rt[:, 0:1], func=AF.Ln, scale=1.0 / D, bias=EPS), reads=[rt], writes=[rt])
                S.op("act", lambda g: g.activation(out=rt[:, 2:3], in_=rt[:, 1:2], func=AF.Exp, scale=-0.5), reads=[rt], writes=[rt])
                S.op("dve", (lambda xm: lambda g: g.scalar_tensor_tensor(out=hnb[:], in0=xm[:], scalar=rt[:, 2:3], in1=gffn[:],
                                                                        op0=ALU.mult, op1=ALU.mult))(xm), reads=[xm, rt, gffn], writes=[hnb])
                S.dma("act", (lambda r0: lambda g: g.dma_start(out=hn_d.ap()[r0:r0 + 128, :], in_=hnb[:]))(r0), reads=[hnb])
                for q4 in range(4):
                    bk = banks[q4 % 3]
                    for cc in range(4):
                        c = q4 * 4 + cc
                        S.op("pe", (lambda c, cc, bk, xm: lambda g: g.transpose(out=bk[:, cc * 128:(cc + 1) * 128], in_=xm[:, c * 128:(c + 1) * 128],
                                                                              identity=ident_f[:]))(c, cc, bk, xm),
                             reads=[xm, ident_f], writes=[bk])
                    eng = "act" if q4 % 2 == 0 else "dve"
                    if eng == "act":
                        S.op("act", (lambda q4, bk: lambda g: g.copy(out=xmT[:, q4 * 4:(q4 + 1) * 4, :], in_=bk[:, :].rearrange("p (a b) -> p a b", a=4)))(q4, bk),
                             reads=[bk], writes=[xmT])
                    else:
                        S.op("dve", (lambda q4, bk: lambda g: g.tensor_copy(out=xmT[:, q4 * 4:(q4 + 1) * 4, :], in_=bk[:, :].rearrange("p (a b) -> p a b", a=4)))(q4, bk),
                             reads=[bk], writes=[xmT])
                for c in range(16):
                    S.op("pe", (lambda c: lambda g: g.matmul(banks[7][:, 0:36], lhsT=xmT[:, c, :], rhs=wr[:, c, :], start=(c == 0), stop=(c == 15)))(c),
                         reads=[xmT, wr], writes=[banks[7]])
                S.op("dve", lambda g: g.tensor_scalar(out=lg[:], in0=banks[7][:, 0:36], scalar1=rt[:, 2:3], scalar2=None, op0=ALU.mult),
                     reads=[banks[7], rt], writes=[lg])
                route_tile(tile)

        def route_tile(tile):
            P = "dve"
            R = [lg, rt]
            S.op("dve", lambda g: g.tensor_reduce(out=rt[:, 3:4], in_=lg[:, 0:4], axis=AX.X, op=ALU.max), reads=R, writes=[rt])
            S.op("dve", lambda g: g.tensor_scalar(out=rt[:, 12:16], in0=lg[:, 0:4], scalar1=rt[:, 3:4], scalar2=None, op0=ALU.is_equal), reads=R, writes=[rt])
            S.op("dve", lambda g: g.tensor_scalar(out=rt[:, 48:52], in0=lg[:, 0:4], scalar1=rt[:, 3:4], scalar2=None, op0=ALU.subtract), reads=R, writes=[rt])
            S.op("act", lambda g: g.activation(out=rt[:, 48:52], in_=rt[:, 48:52], func=AF.Exp, accum_out=rt[:, 4:5]), reads=R, writes=[rt])
            S.op("dve", lambda g: g.reciprocal(out=rt[:, 5:6], in_=rt[:, 4:5]), reads=R, writes=[rt])
            S.op("dve", lambda g: g.tensor_scalar(out=rt[:, 16:24], in0=lg[:, 4:12], scalar1=rt[:, 12:13], scalar2=None, op0=ALU.mult), reads=R, writes=[rt])
            for gi in range(1, 4):
                S.op("dve", (lambda gi: lambda g: g.scalar_tensor_tensor(out=rt[:, 16:24], in0=lg[:, 4 + 8 * gi:12 + 8 * gi], scalar=rt[:, 12 + gi:13 + gi],
                                                                         in1=rt[:, 16:24], op0=ALU.mult, op1=ALU.add))(gi), reads=R, writes=[rt])
            S.op("dve", lambda g: g.tensor_reduce(out=rt[:, 6:7], in_=rt[:, 16:24], axis=AX.X, op=ALU.max), reads=R, writes=[rt])
            S.op("dve", lambda g: g.tensor_scalar(out=rt[:, 24:32], in0=rt[:, 16:24], scalar1=rt[:, 6:7], scalar2=None, op0=ALU.is_equal), reads=R, writes=[rt])
            S.op("dve", lambda g: g.scalar_tensor_tensor(out=rt[:, 32:40], in0=rt[:, 24:32], scalar=-1e30, in1=rt[:, 16:24], op0=ALU.mult, op1=ALU.add),
                 reads=R, writes=[rt])
            S.op("dve", lambda g: g.tensor_reduce(out=rt[:, 7:8], in_=rt[:, 32:40], axis=AX.X, op=ALU.max), reads=R, writes=[rt])
            S.op("dve", lambda g: g.tensor_scalar(out=rt[:, 40:48], in0=rt[:, 32:40], scalar1=rt[:, 7:8], scalar2=None, op0=ALU.is_equal), reads=R, writes=[rt])
            S.op("dve", lambda g: g.tensor_tensor(out=rt[:, 8:9], in0=rt[:, 7:8], in1=rt[:, 6:7], op=ALU.subtract), reads=R, writes=[rt])
            S.op("act", lambda g: g.activation(out=rt[:, 8:9], in_=rt[:, 8:9], func=AF.Exp), reads=R, writes=[rt])
            S.op("dve", lambda g: g.tensor_scalar(out=rt[:, 8:9], in0=rt[:, 8:9], scalar1=1.0, scalar2=None, op0=ALU.add), reads=R, writes=[rt])
            S.op("dve", lambda g: g.reciprocal(out=rt[:, 8:9], in_=rt[:, 8:9]), reads=R, writes=[rt])
            S.op("dve", lambda g: g.tensor_tensor(out=w12_all[:, 0, tile:tile + 1], in0=rt[:, 8:9], in1=rt[:, 5:6], op=ALU.mult), reads=R, writes=[w12_all])
            S.op("dve", lambda g: g.tensor_tensor(out=w12_all[:, 1, tile:tile + 1], in0=rt[:, 5:6], in1=w12_all[:, 0, tile:tile + 1], op=ALU.subtract),
                 reads=R + [w12_all], writes=[w12_all])
            for gi in range(4):
                S.op(P, (lambda gi: lambda g: g.tensor_scalar(out=oh1_all[:, tile, gi * 8:(gi + 1) * 8], in0=rt[:, 24:32], scalar1=rt[:, 12 + gi:13 + gi],
                                                               scalar2=None, op0=ALU.mult))(gi), reads=R, writes=[oh1_all])
                S.op(P, (lambda gi: lambda g: g.tensor_scalar(out=oh2_all[:, tile, gi * 8:(gi + 1) * 8], in0=rt[:, 40:48], scalar1=rt[:, 12 + gi:13 + gi],
                                                               scalar2=None, op0=ALU.mult))(gi), reads=R, writes=[oh2_all])
            S.op(P, lambda g: g.tensor_tensor(out=Ct[:], in0=oh1_all[:, tile, :], in1=oh2_all[:, tile, :], op=ALU.add), reads=[oh1_all, oh2_all], writes=[Ct])
            S.op("pe", lambda g: g.matmul(banks[3][:, 400:432], lhsT=stri[:], rhs=Ct[:], start=True, stop=False), reads=[stri, Ct], writes=[banks[3]])
            S.op("pe", lambda g: g.matmul(banks[3][:, 400:432], lhsT=ones_f[:], rhs=Csum[:], start=False, stop=True), reads=[ones_f, Csum], writes=[banks[3]])
            S.op("dve", lambda g: g.tensor_copy(out=rank_all[:, tile, :], in_=banks[3][:, 400:432]), reads=[banks[3]], writes=[rank_all])
            S.op(P, lambda g: g.tensor_tensor(out=Csum[:], in0=Csum[:], in1=Ct[:], op=ALU.add), reads=[Csum, Ct], writes=[Csum])

        if stop <= 0:
            S.emit()
            return nc
        import os
        _dbg = os.environ.get("K_DBG", "")
        plan_slabs()
        for wg in range(NWG):
            load_x_group(xTp_d, wg)
            if _dbg == "lx":
                S.emit()
                return nc
            run_heads(False)
            if _dbg == "h8":
                S.emit()
                return nc
            if wg == NWG - 1:
                for j in range(8):
                    sc_chunk(j, tail_only=True)
        if stop <= 1:
            S.emit()
            return nc
        for h in range(8):
            S.op("act", (lambda h: lambda g: g.copy(out=Sbf[h][:], in_=Sst[h][:]))(h), reads=[Sst[h]], writes=[Sbf[h]])
        for zb in range(NB):
            S.dma("act", (lambda zb: lambda g: g.dma_start(out=xs_d.ap()[zb * 256:(zb + 1) * 256, :], in_=zeros_d.ap()))(zb))
        for g0 in range(NG):
            load_x_group(xT_d, g0)
            run_heads(True)
            for j in range(8):
                sc_chunk(j)
            sc_finish()
            outproj_and_route(g0)

        if stop <= 2:
            S.emit()
            return nc
        S.barrier()
        A.off = persist_mark
        fin = A.alloc("fin", [512])
        fin_i = A.alloc("fin_i", [128], I32)
        b3 = banks[3]
        S.op("pe", lambda g: g.matmul(b3[0:32, 0:128], lhsT=Csum[:], rhs=ones_f[:], start=True, stop=True), reads=[Csum, ones_f], writes=[b3])
        S.op("dve", lambda g: g.tensor_scalar(out=fin[0:32, 0:128], in0=b3[0:32, 0:128], scalar1=255.0, scalar2=None, op0=ALU.add), reads=[b3], writes=[fin])
        S.op("dve", lambda g: g.tensor_copy(out=fin_i[0:32, :], in_=fin[0:32, 0:128]), reads=[fin], writes=[fin_i])
        S.op("dve", lambda g: g.tensor_scalar(out=fin_i[0:32, :], in0=fin_i[0:32, :], scalar1=8, scalar2=8, op0=ALU.arith_shift_right,
                                              op1=ALU.logical_shift_left), reads=[fin_i], writes=[fin_i])
        S.op("dve", lambda g: g.tensor_copy(out=fin[0:32, 0:128], in_=fin_i[0:32, :]), reads=[fin_i], writes=[fin])
        S.op("pe", lambda g: g.matmul(b3[:, 128:160], lhsT=fin[0:32, 0:128], rhs=tri2[0:32, 0:32], start=True, stop=True), reads=[fin, tri2], writes=[b3])
        S.op("pe", lambda g: g.matmul(b3[:, 160:192], lhsT=fin[0:32, 0:128], rhs=stri[0:32, 0:32], start=True, stop=True), reads=[fin, stri], writes=[b3])
        S.op("dve", lambda g: g.tensor_copy(out=fin[:, 128:192], in_=b3[:, 128:192]), reads=[b3], writes=[fin])
        pend = fin[:, 128:160]
        pstart = fin[:, 160:192]
        tmp32 = fin[:, 192:224]
        for tile in range(NT):
            for k, oh in ((0, oh1_all), (1, oh2_all)):
                S.op("dve", (lambda tile: lambda g: g.tensor_tensor(out=tmp32, in0=rank_all[:, tile, :], in1=pstart, op=ALU.add))(tile),
                     reads=[rank_all, fin], writes=[fin])
                S.op("dve", (lambda tile, oh: lambda g: g.tensor_tensor(out=tmp32, in0=tmp32, in1=oh[:, tile, :], op=ALU.mult))(tile, oh),
                     reads=[oh, fin], writes=[fin])
                S.op("dve", (lambda tile, k: lambda g: g.tensor_reduce(out=dest_f[:, k, tile:tile + 1], in_=tmp32, axis=AX.X, op=ALU.add))(tile, k),
                     reads=[fin], writes=[dest_f])
        S.op("dve", lambda g: g.tensor_copy(out=dest_i[:], in_=dest_f[:]), reads=[dest_f], writes=[dest_i])
        bthr = A.alloc("bthr", [NB])
        bacc = A.alloc("bacc", [NB])
        idxf = A.alloc("idxf", [4, NB])
        S.op("pool", lambda g: g.iota(bthr[:], pattern=[[256, NB]], base=0, channel_multiplier=0, allow_small_or_imprecise_dtypes=True), writes=[bthr])
        memset("pool", bacc, bacc[:], 0.0)
        for e in range(NEXP):
            S.op("dve", (lambda e: lambda g: g.scalar_tensor_tensor(out=bacc[:], in0=bthr[:], scalar=fin[:, 128 + e:129 + e], in1=bacc[:],
                                                                   op0=ALU.is_ge, op1=ALU.add))(e), reads=[bthr, fin, bacc], writes=[bacc])
        S.op("dve", lambda g: g.tensor_scalar(out=bacc[:], in0=bacc[:], scalar1=float(NEXP - 1), scalar2=None, op0=ALU.min), reads=[bacc], writes=[bacc])
        for cc in range(4):
            S.op("dve", (lambda cc: lambda g: g.tensor_scalar(out=idxf[:, cc, :], in0=bacc[:], scalar1=512.0, scalar2=p4[:, cc:cc + 1],
                                                             op0=ALU.mult, op1=ALU.add))(cc), reads=[bacc, p4], writes=[idxf])
        S.op("dve", lambda g: g.tensor_copy(out=idxw[:], in_=idxf[:]), reads=[idxf], writes=[idxw])
        if debug:
            dbgt = A.alloc("dbgt", [8, NT])
            S.op("dve", lambda g: g.tensor_copy(out=dbgt[:, 0:2, :], in_=dest_f[:]), reads=[dest_f], writes=[dbgt])
            S.op("dve", lambda g: g.tensor_copy(out=dbgt[:, 2:4, :], in_=w12_all[:]), reads=[w12_all], writes=[dbgt])
            S.op("dve", lambda g: g.memset(dbgt[:, 4:8, :], 0.0), writes=[dbgt])
            S.dma("sp", lambda g: g.dma_start(out=dbg_d.ap(), in_=dbgt[:]), reads=[dbgt])
            S.dma("sp", lambda g: g.dma_start(out=dbgb_d.ap(), in_=bacc[:]), reads=[bacc])
        S.barrier()

        if stop <= 3:
            S.emit()
            return nc
        A.off = true_persist_mark
        hnt = [A.alloc("hnt%d" % i, [D], BF16) for i in range(2)]
        for tile in range(NT):
            ht = hnt[tile % 2]
            r0 = tile * 128
            S.dma("sp", (lambda ht, r0: lambda g: g.dma_start(out=ht[:], in_=hn_d.ap()[r0:r0 + 128, :]))(ht, r0), writes=[ht])
            for k in range(2):
                S.dma("pool", (lambda ht, k, tile: lambda g: g.indirect_dma_start(
                    out=xs_d.ap(), out_offset=bass.IndirectOffsetOnAxis(ap=dest_i[:, k, tile:tile + 1], axis=0),
                    in_=ht[:], in_offset=None))(ht, k, tile), reads=[ht, dest_i])
        S.barrier()
        if stop <= 4:
            S.emit()
            return nc
        A.off = true_persist_mark
        wgb = [A.alloc("wgb%d" % i, [16, 512], BF16) for i in range(2)]
        wub = [A.alloc("wub%d" % i, [16, 512], BF16) for i in range(2)]
        wdb = [A.alloc("wdb%d" % i, [4, 2048], BF16) for i in range(2)]
        xt = [A.alloc("xt%d" % i, [D], BF16) for i in range(2)]
        xsT2 = [A.alloc("xsT", [16, 256], BF16)]
        actT2 = [A.alloc("actT%d" % i, [4, 256], BF16) for i in range(2)]
        ee = A.alloc("ee", [256])
        ga = A.alloc("ga", [256])
        yrow = [A.alloc("yrow0", [D])]
        yrow.append(yrow[0])
        NSTG = min(8, (A.words - A.off) // 2048)
        assert NSTG >= 3, NSTG
        stage = [A.alloc("stage%d" % i, [2048]) for i in range(NSTG)]
        stg = {"n": 0}
        cast_engs = ["act", "dve"]

        assert NSTG == 8, NSTG

        def w_piece(b, i):
            k = b % 2
            if i < 4:
                return wgt_d, wgb[k], i, True
            if i < 8:
                return wut_d, wub[k], i - 4, True
            return wdt_d, wdb[k], i - 8, False

        def gather_w(b, i):
            tab, dst, cc, gu = w_piece(b, i)
            st = stage[i % NSTG]
            S.dma("pool", (lambda st, tab, cc, b: lambda g: g.indirect_dma_start(
                out=st[:], out_offset=None, in_=tab.ap(),
                in_offset=bass.IndirectOffsetOnAxis(ap=idxw[:, cc, b:b + 1], axis=0)))(st, tab, cc, b), reads=[idxw], writes=[st])

        def cast_w(b, i):
            tab, dst, cc, gu = w_piece(b, i)
            st = stage[i % NSTG]
            if gu:
                dview = dst[:, 4 * cc:4 * cc + 4, :]
                sview = st[:].rearrange("p (a b) -> p a b", a=4)
            else:
                dview = dst[:, cc, :]
                sview = st[:]
            if i % 2 == 0:
                S.op("act", (lambda dview, sview: lambda g: g.copy(out=dview, in_=sview))(dview, sview), reads=[st], writes=[dst])
            else:
                S.op("dve", (lambda dview, sview: lambda g: g.tensor_copy(out=dview, in_=sview))(dview, sview), reads=[st], writes=[dst])
            if i + NSTG < 12:
                gather_w(b, i + NSTG)

        for i in range(NSTG):
            gather_w(0, i)
        for i in range(12):
            cast_w(0, i)
        for b in range(NB):
            k = b % 2
            xsT = xsT2[b % len(xsT2)]
            actT = actT2[b % 2]
            if b + 1 < NB:
                for i in range(NSTG):
                    gather_w(b + 1, i)
            for r in range(2):
                r0 = b * 256 + r * 128
                S.dma("sp", (lambda r, r0: lambda g: g.dma_start(out=xt[r][:], in_=xs_d.ap()[r0:r0 + 128, :]))(r, r0), writes=[xt[r]])
                for hh in range(2):
                    bkb, bk = (B6b, banks[6]) if hh == 0 else (B5b, banks[5])
                    for cc in range(8):
                        c = hh * 8 + cc
                        S.op("pe", (lambda c, cc, r, bkb: lambda g: g.transpose(out=bkb[:, cc * 128:(cc + 1) * 128], in_=xt[r][:, c * 128:(c + 1) * 128],
                                                                             identity=ident_b[:]))(c, cc, r, bkb), reads=[xt[r], ident_b], writes=[bk])
                    if hh == 0:
                        S.op("act", (lambda r, bkb, xsT: lambda g: g.copy(out=xsT[:, 0:8, r * 128:(r + 1) * 128], in_=bkb.rearrange("p (a b) -> p a b", a=8)))(r, bkb, xsT),
                             reads=[bk], writes=[xsT])
                    else:
                        S.op("dve", (lambda r, bkb, xsT: lambda g: g.tensor_copy(out=xsT[:, 8:16, r * 128:(r + 1) * 128], in_=bkb.rearrange("p (a b) -> p a b", a=8)))(r, bkb, xsT),
                             reads=[bk], writes=[xsT])
            for fc in range(4):
                bg, bu = banks[0 + 2 * (fc % 2)], banks[1 + 2 * (fc % 2)]
                for c in range(16):
                    S.op("pe", (lambda c, fc, bg, k, xsT: lambda g: g.matmul(bg[:, 0:256], lhsT=wgb[k][:, c, fc * 128:(fc + 1) * 128], rhs=xsT[:, c, :],
                                                                     start=(c == 0), stop=(c == 15)))(c, fc, bg, k, xsT), reads=[wgb[k], xsT], writes=[bg])
                for c in range(16):
                    S.op("pe", (lambda c, fc, bu, k, xsT: lambda g: g.matmul(bu[:, 0:256], lhsT=wub[k][:, c, fc * 128:(fc + 1) * 128], rhs=xsT[:, c, :],
                                                                     start=(c == 0), stop=(c == 15)))(c, fc, bu, k, xsT), reads=[wub[k], xsT], writes=[bu])
                S.op("act", (lambda bg: lambda g: g.activation(out=ee[:], in_=bg[:, 0:256], func=AF.Exp, scale=-1.0))(bg), reads=[bg], writes=[ee])
                S.op("act", lambda g: g.activation(out=ee[:], in_=ee[:], func=AF.Ln, bias=1.0), reads=[ee], writes=[ee])
                S.op("act", lambda g: g.activation(out=ee[:], in_=ee[:], func=AF.Exp, scale=-1.0), reads=[ee], writes=[ee])
                S.op("dve", (lambda bg: lambda g: g.tensor_tensor(out=ga[:], in0=bg[:, 0:256], in1=ee[:], op=ALU.mult))(bg), reads=[bg, ee], writes=[ga])
                S.op("dve", (lambda bu, fc, actT: lambda g: g.tensor_tensor(out=actT[:, fc, :], in0=bu[:, 0:256], in1=ga[:], op=ALU.mult))(bu, fc, actT),
                     reads=[bu, ga], writes=[actT])
                if b + 1 < NB:
                    for i in (3 * fc, 3 * fc + 1, 3 * fc + 2):
                        cast_w(b + 1, i)
            kk2 = 0
            for r in range(2):
                for n in range(4):
                    bk = banks[4 + (kk2 % 2) * 3]
                    kk2 += 1
                    for fc in range(4):
                        S.op("pe", (lambda fc, r, n, bk, k, actT: lambda g: g.matmul(bk[:, :], lhsT=actT[:, fc, r * 128:(r + 1) * 128],
                                                                            rhs=wdb[k][:, fc, n * 512:(n + 1) * 512], start=(fc == 0), stop=(fc == 3)))(fc, r, n, bk, k, actT),
                             reads=[actT, wdb[k]], writes=[bk])
                    S.op("act", (lambda r, n, bk: lambda g: g.copy(out=yrow[r][:, n * 512:(n + 1) * 512], in_=bk[:, :]))(r, n, bk), reads=[bk], writes=[yrow[r]])
                r0 = b * 256 + r * 128
                S.dma("act", (lambda r, r0: lambda g: g.dma_start(out=ys_d.ap()[r0:r0 + 128, :], in_=yrow[r][:]))(r, r0), reads=[yrow[r]])
        S.barrier()

        if stop <= 5:
            S.emit()
            return nc
        A.off = true_persist_mark
        wpg = A.alloc("wpg", [16, D], BF16)
        wple = A.alloc("wple", [2, D], BF16)
        gple = A.alloc("gple", [D])
        gfin = A.alloc("gfin", [D])
        xm3s = [A.alloc("xm3_%d" % i, [D]) for i in range(2)]
        y12 = [A.alloc("y12_%d" % i, [D]) for i in range(2)]
        pTfs = [A.alloc("pTf%d" % i, [2, 128]) for i in range(2)]
        pTbs = [A.alloc("pTb%d" % i, [2, 128], BF16) for i in range(2)]
        x2bs = [A.alloc("x2b%d" % i, [D], BF16) for i in range(2)]
        x2Ts = [A.alloc("x2T%d" % i, [16, 128], BF16) for i in range(2)]
        plers = [A.alloc("pler%d" % i, [D]) for i in range(2)]
        egs = [A.alloc("eg%d" % i, [512]) for i in range(2)]
        junk3 = A.alloc("junk3", [D], BF16)
        s3s = [A.alloc("s3_%d" % i, [8]) for i in range(2)]
        for c4 in range(4):
            S.dma("pool", (lambda c4: lambda g: g.dma_start(out=wpg[:, 4 * c4:4 * c4 + 4, :],
                                                           in_=wpg_d.ap()[c4 * 512:(c4 + 1) * 512, :].rearrange("(c p) n -> p c n", p=128)))(c4), writes=[wpg])
        S.dma("pool", lambda g: g.dma_start(out=wple[:], in_=wple_d.ap().rearrange("(c p) n -> p c n", p=128)), writes=[wple])
        S.dma("sp", lambda g: g.dma_start(out=gple[:], in_=gple_d.ap().partition_broadcast(128)), writes=[gple])
        S.dma("sp", lambda g: g.dma_start(out=gfin[:], in_=gfin_d.ap().partition_broadcast(128)), writes=[gfin])

        def p3_load(tile):
            xm3, pTf = xm3s[tile % 2], pTfs[tile % 2]
            r0 = tile * 128
            S.dma("sp", (lambda r0, xm3: lambda g: g.dma_start(out=xm3[:], in_=xmid_d.ap()[r0:r0 + 128, :]))(r0, xm3), writes=[xm3])
            S.dma("sp", (lambda r0, pTf: lambda g: g.dma_start(out=pTf[:], in_=pT_d.ap()[:, r0:r0 + 128].rearrange("(c p) t -> p c t", p=128)))(r0, pTf),
                  writes=[pTf])

        def p3_gather(tile):
            for k in range(2):
                S.dma("pool", (lambda k, tile: lambda g: g.indirect_dma_start(
                    out=y12[k][:], out_offset=None, in_=ys_d.ap(),
                    in_offset=bass.IndirectOffsetOnAxis(ap=dest_i[:, k, tile:tile + 1], axis=0)))(k, tile), reads=[dest_i], writes=[y12[k]])

        p3_load(0)
        p3_gather(0)
        for tile in range(NT):
            r0 = tile * 128
            xm3, pTf, pTb, s3 = xm3s[tile % 2], pTfs[tile % 2], pTbs[tile % 2], s3s[tile % 2]
            x2b, x2T, pler = x2bs[tile % 2], x2Ts[tile % 2], plers[tile % 2]
            if tile + 1 < NT:
                p3_load(tile + 1)
            S.op("act", (lambda pTb, pTf: lambda g: g.copy(out=pTb[:], in_=pTf[:]))(pTb, pTf), reads=[pTf], writes=[pTb])
            S.op("dve", (lambda tile, xm3: lambda g: g.scalar_tensor_tensor(out=xm3[:], in0=y12[0][:], scalar=w12_all[:, 0, tile:tile + 1], in1=xm3[:],
                                                                           op0=ALU.mult, op1=ALU.add))(tile, xm3), reads=[y12[0], w12_all, xm3], writes=[xm3])
            S.op("dve", (lambda tile, xm3: lambda g: g.scalar_tensor_tensor(out=xm3[:], in0=y12[1][:], scalar=w12_all[:, 1, tile:tile + 1], in1=xm3[:],
                                                                           op0=ALU.mult, op1=ALU.add))(tile, xm3), reads=[y12[1], w12_all, xm3], writes=[xm3])
            if tile + 1 < NT:
                p3_gather(tile + 1)
            S.op("act", (lambda xm3, x2b: lambda g: g.copy(out=x2b[:], in_=xm3[:]))(xm3, x2b), reads=[xm3], writes=[x2b])
            for hh in range(2):
                bkb, bk = (B6b, banks[6]) if hh == 0 else (B5b, banks[5])
                for cc in range(8):
                    c = hh * 8 + cc
                    S.op("pe", (lambda c, cc, bkb, x2b: lambda g: g.transpose(out=bkb[:, cc * 128:(cc + 1) * 128], in_=x2b[:, c * 128:(c + 1) * 128],
                                                                      identity=ident_b[:]))(c, cc, bkb, x2b), reads=[x2b, ident_b], writes=[bk])
                if hh == 0:
                    S.op("act", (lambda bkb, x2T: lambda g: g.copy(out=x2T[:, 0:8, :], in_=bkb.rearrange("p (a b) -> p a b", a=8)))(bkb, x2T), reads=[bk], writes=[x2T])
                else:
                    S.op("dve", (lambda bkb, x2T: lambda g: g.tensor_copy(out=x2T[:, 8:16, :], in_=bkb.rearrange("p (a b) -> p a b", a=8)))(bkb, x2T), reads=[bk], writes=[x2T])
            for n in range(4):
                bk = banks[n % 2]
                for kc in range(2):
                    S.op("pe", (lambda kc, n, bk, pTb: lambda g: g.matmul(bk[:, :], lhsT=pTb[:, kc, :], rhs=wple[:, kc, n * 512:(n + 1) * 512],
                                                                          start=(kc == 0), stop=(kc == 1)))(kc, n, bk, pTb), reads=[pTb, wple], writes=[bk])
                S.op("act", (lambda n, bk, pler: lambda g: g.copy(out=pler[:, n * 512:(n + 1) * 512], in_=bk[:, :]))(n, bk, pler), reads=[bk], writes=[pler])
            S.op("act", (lambda s3, pler: lambda g: g.activation(out=junk3[:], in_=pler[:], func=AF.Square, accum_out=s3[:, 0:1]))(s3, pler), reads=[pler], writes=[junk3, s3])
            S.op("act", (lambda s3: lambda g: g.activation(out=s3[:, 1:2], in_=s3[:, 0:1], func=AF.Ln, scale=1.0 / D, bias=EPS))(s3), reads=[s3], writes=[s3])
            S.op("act", (lambda s3: lambda g: g.activation(out=s3[:, 2:3], in_=s3[:, 1:2], func=AF.Exp, scale=-0.5))(s3), reads=[s3], writes=[s3])
            S.op("dve", (lambda s3, pler: lambda g: g.scalar_tensor_tensor(out=pler[:], in0=pler[:], scalar=s3[:, 2:3], in1=gple[:], op0=ALU.mult, op1=ALU.mult))(s3, pler),
                 reads=[pler, s3, gple], writes=[pler])
            for n in range(4):
                bk = banks[2 + n % 2]
                eg = egs[n % 2]
                for c in range(16):
                    S.op("pe", (lambda c, n, bk, x2T: lambda g: g.matmul(bk[:, :], lhsT=x2T[:, c, :], rhs=wpg[:, c, n * 512:(n + 1) * 512],
                                                                    start=(c == 0), stop=(c == 15)))(c, n, bk, x2T), reads=[x2T, wpg], writes=[bk])
                S.op("act", (lambda bk, eg: lambda g: g.activation(out=eg[:], in_=bk[:, :], func=AF.Exp, scale=-1.0))(bk, eg), reads=[bk], writes=[eg])
                S.op("act", (lambda eg: lambda g: g.activation(out=eg[:], in_=eg[:], func=AF.Ln, bias=1.0))(eg), reads=[eg], writes=[eg])
                S.op("act", (lambda eg: lambda g: g.activation(out=eg[:], in_=eg[:], func=AF.Exp, scale=-1.0))(eg), reads=[eg], writes=[eg])
                S.op("dve", (lambda n, eg, pler: lambda g: g.tensor_tensor(out=eg[:], in0=eg[:], in1=pler[:, n * 512:(n + 1) * 512], op=ALU.mult))(n, eg, pler),
                     reads=[eg, pler], writes=[eg])
                S.op("dve", (lambda n, xm3, eg: lambda g: g.tensor_tensor(out=xm3[:, n * 512:(n + 1) * 512], in0=eg[:], in1=xm3[:, n * 512:(n + 1) * 512], op=ALU.add))(n, xm3, eg),
                     reads=[eg, xm3], writes=[xm3])
            S.op("act", (lambda s3, xm3: lambda g: g.activation(out=junk3[:], in_=xm3[:], func=AF.Square, accum_out=s3[:, 4:5]))(s3, xm3), reads=[xm3], writes=[junk3, s3])
            S.op("act", (lambda s3: lambda g: g.activation(out=s3[:, 5:6], in_=s3[:, 4:5], func=AF.Ln, scale=1.0 / D, bias=EPS))(s3), reads=[s3], writes=[s3])
            S.op("act", (lambda s3: lambda g: g.activation(out=s3[:, 6:7], in_=s3[:, 5:6], func=AF.Exp, scale=-0.5))(s3), reads=[s3], writes=[s3])
            S.op("dve", (lambda s3, xm3: lambda g: g.scalar_tensor_tensor(out=xm3[:], in0=xm3[:], scalar=s3[:, 6:7], in1=gfin[:], op0=ALU.mult, op1=ALU.mult))(s3, xm3),
                 reads=[xm3, s3, gfin], writes=[xm3])
            S.dma("act", (lambda r0, xm3: lambda g: g.dma_start(out=out_d.ap()[r0:r0 + 128, :], in_=xm3[:]))(r0, xm3), reads=[xm3])
        S.emit()
    return nc


def prep_weights(g_mix, w_in, lb_logits, hg_norm, conv_w, sc_norm, w_out, g_ffn, w_router_group, w_router_expert,
                 w_gate, w_up, w_down, w_ple, g_ple, w_ple_gate, g_final):
    f = np.float32
    w_in = np.asarray(w_in[0], f)
    q, fz, iz, gz = (w_in[:, k * 1024:(k + 1) * 1024].reshape(D, 8, 128) for k in range(4))
    whg = np.ascontiguousarray(np.stack([q, gz, fz, iz], axis=2).transpose(1, 0, 2, 3).reshape(8, D, 512))
    Bw, Cw, Hw = (w_in[:, 4096 + k * 1024:4096 + (k + 1) * 1024].reshape(D, 8, 128) for k in range(3))
    wsc = np.ascontiguousarray(np.stack([Bw, Cw, Hw], axis=2).transpose(1, 0, 2, 3).reshape(8, D, 384))
    wr = np.concatenate([np.asarray(w_router_group[0], f), np.asarray(w_router_expert[0], f)], axis=1)
    wr = np.ascontiguousarray(wr.reshape(16, 128, 36).transpose(1, 0, 2))
    wg = np.asarray(w_gate[0], f).reshape(NEXP, 16, 128, 512).transpose(0, 2, 1, 3)
    wgt = np.ascontiguousarray(wg).reshape(NEXP * 128 * 4, 2048)
    wu = np.asarray(w_up[0], f).reshape(NEXP, 16, 128, 512).transpose(0, 2, 1, 3)
    wut = np.ascontiguousarray(wu).reshape(NEXP * 128 * 4, 2048)
    wd = np.asarray(w_down[0], f).reshape(NEXP, 4, 128, 2048).transpose(0, 2, 1, 3)
    wdt = np.ascontiguousarray(wd).reshape(NEXP * 128 * 4, 2048)
    cols = np.zeros((128, 64), f)
    cols[:, 0:16] = np.asarray(g_mix[0], f).reshape(16, 128).T
    cols[:, 16:32] = np.asarray(g_ffn[0], f).reshape(16, 128).T
    cols[:, 32:40] = np.asarray(sc_norm[0], f).reshape(8, 128).T
    cw = np.asarray(conv_w[0], f).reshape(3, 8, 128)
    cols[:, 40:64] = cw.transpose(2, 1, 0).reshape(128, 24)
    return {
        "whg": whg, "wsc": wsc, "wout": np.ascontiguousarray(np.asarray(w_out[0], f)), "wr": wr,
        "wgt": wgt, "wut": wut, "wdt": wdt,
        "wple": np.ascontiguousarray(np.asarray(w_ple[0], f)), "wpg": np.ascontiguousarray(np.asarray(w_ple_gate[0], f)),
        "lb": np.ascontiguousarray(np.asarray(lb_logits, f)), "hgn": np.asarray(hg_norm, f).reshape(1, 128),
        "gffn": np.asarray(g_ffn, f).reshape(1, D), "gple": np.asarray(g_ple, f).reshape(1, D),
        "gfin": np.asarray(g_final, f).reshape(1, D), "cols": cols,
        "zeros": np.zeros((256, D), ml_dtypes.bfloat16),
    }


_NC_CACHE = {}


def kernel(x, p, g_mix, w_in, lb_logits, hg_norm, conv_w, sc_norm, w_out, g_ffn, w_router_group, w_router_expert,
           w_gate, w_up, w_down, w_ple, g_ple, w_ple_gate, g_final):
    x = np.asarray(x, np.float32)
    p = np.asarray(p, np.float32)
    Bn, T, _ = x.shape
    half = T // 2
    wts = prep_weights(g_mix, w_in, lb_logits, hg_norm, conv_w, sc_norm, w_out, g_ffn, w_router_group, w_router_expert,
                       w_gate, w_up, w_down, w_ple, g_ple, w_ple_gate, g_final)
    if "nc" not in _NC_CACHE:
        _NC_CACHE["nc"] = build(NG=half // 512, NWG=half // 512)
    nc = _NC_CACHE["nc"]
    in_maps = []
    for c in range(8):
        b, hf = c // 2, c % 2
        rows = slice(hf * half, (hf + 1) * half)
        m = dict(wts)
        m["xT"] = np.ascontiguousarray(x[b, rows].T)
        m["xTp"] = np.ascontiguousarray(x[b, 0:half].T) if hf == 1 else np.zeros((D, half), np.float32)
        m["xtok"] = np.ascontiguousarray(x[b, rows])
        m["pT"] = np.ascontiguousarray(p[0, b, rows].T)
        in_maps.append(m)
    res = run_bass_kernel_spmd(nc, in_maps, core_ids=list(range(8)))
    out = np.empty((Bn, T, D), np.float32)
    for c in range(8):
        b, hf = c // 2, c % 2
        out[b, hf * half:(hf + 1) * half] = res.results[c]["out"]
    return out
```

```python
import numpy as np
import ml_dtypes
from contextlib import ExitStack
import concourse.bass as bass
import concourse.mybir as mybir
from concourse.bass_utils import run_bass_kernel_spmd

F32 = mybir.dt.float32
BF16 = mybir.dt.bfloat16
I32 = mybir.dt.int32
AF = mybir.ActivationFunctionType
ALU = mybir.AluOpType
AX = mybir.AxisListType

D = 2048
EPS = 1e-6
EPS_HG = 1e-6 * 128.0
NEXP = 32


class Buf:
    __slots__ = ("name", "t", "last_w", "readers", "aliases", "off", "excl")

    def __init__(self, name, t):
        self.name = name
        self.t = t
        self.last_w = None
        self.readers = {}
        self.aliases = []
        self.off = None
        self.excl = False

    def __getitem__(self, idx):
        return self.t[idx]


class Instr:
    __slots__ = ("eng", "fn", "deps", "is_dma", "sem", "val", "signal", "prewait")

    def __init__(self, eng, fn, deps, is_dma):
        self.eng = eng
        self.fn = fn
        self.deps = deps
        self.is_dma = is_dma
        self.sem = None
        self.val = None
        self.signal = False
        self.prewait = None


class Sched:
    ENGS = ("pe", "act", "dve", "pool", "sp")

    def __init__(self, nc, es, n_dma_sems=8):
        self.nc = nc
        self.streams = {e: [] for e in self.ENGS}
        self.esem = {e: es.enter_context(nc.semaphore("s_" + e)) for e in self.ENGS}
        self.dsems = {}
        for q in ("sp", "pool", "act"):
            self.dsems[q] = [es.enter_context(nc.semaphore("d_%s%d" % (q, i))) for i in range(n_dma_sems)]
        self.dcount = {q: 0 for q in self.dsems}
        self.duse = {q: [0] * n_dma_sems for q in self.dsems}
        self.dlast = {q: [None] * n_dma_sems for q in self.dsems}
        self.n_instr = 0

    @staticmethod
    def _expand(bufs):
        out = {}
        for b in bufs:
            out[id(b)] = b
            for a in b.aliases:
                out[id(a)] = a
        return list(out.values())

    def _deps(self, eng, reads, writes, is_dma):
        deps = {}
        for b in reads:
            if b.last_w is not None:
                deps[id(b.last_w)] = b.last_w
        for b in writes:
            if b.last_w is not None:
                deps[id(b.last_w)] = b.last_w
            for r in b.readers.values():
                deps[id(r)] = r
        out = []
        for d in deps.values():
            if (not is_dma) and (not d.is_dma) and d.eng == "pe" and eng == "pe":
                continue
            if not d.is_dma:
                d.signal = True
            out.append(d)
        return out

    def _commit(self, ins, reads, writes):
        wset = set(id(b) for b in writes)
        for b in reads:
            if id(b) in wset:
                continue
            key = ("dma", id(ins)) if ins.is_dma else ins.eng
            b.readers[key] = ins
        for b in writes:
            b.last_w = ins
            b.readers = {}
        self.n_instr += 1

    def op(self, eng, fn, reads=(), writes=()):
        reads = self._expand(reads)
        writes = self._expand(writes)
        ex = [b for b in reads if b.excl]
        if ex:
            writes = writes + [b for b in ex if all(b is not w for w in writes)]
        ins = Instr(eng, fn, self._deps(eng, reads, writes, False), False)
        self.streams[eng].append(ins)
        self._commit(ins, reads, writes)
        return ins

    def dma(self, q, fn, reads=(), writes=()):
        reads = self._expand(reads)
        writes = self._expand(writes)
        ins = Instr(q, fn, self._deps(q, reads, writes, True), True)
        n = self.dcount[q]
        k = n % len(self.dsems[q])
        self.dcount[q] += 1
        ins.prewait = self.dlast[q][k]
        self.duse[q][k] += 1
        ins.sem = self.dsems[q][k]
        ins.val = 16 * self.duse[q][k]
        self.dlast[q][k] = ins
        self.streams[q].append(ins)
        self._commit(ins, reads, writes)
        return ins

    def barrier(self):
        lasts = []
        for e in self.ENGS:
            for ins in reversed(self.streams[e]):
                if not ins.is_dma:
                    ins.signal = True
                    lasts.append(ins)
                    break
        for q in self.dsems:
            for last in self.dlast[q]:
                if last is not None:
                    lasts.append(last)
        for e in self.ENGS:
            ins = Instr(e, lambda h: h.nop(), list(lasts), False)
            self.streams[e].append(ins)

    def emit(self):
        nc = self.nc
        for e in self.ENGS:
            c = 0
            for ins in self.streams[e]:
                if not ins.is_dma and ins.signal:
                    c += 1
                    ins.sem = self.esem[e]
                    ins.val = c
        with nc.Block() as block:
            def run(e, h):
                waited = {}

                def wait(d):
                    k = id(d.sem)
                    if waited.get(k, 0) >= d.val:
                        return
                    waited[k] = d.val
                    h.wait_ge(d.sem, d.val)

                for ins in self.streams[e]:
                    if ins.is_dma and ins.prewait is not None:
                        wait(ins.prewait)
                    for d in ins.deps:
                        wait(d)
                    r = ins.fn(h)
                    if ins.is_dma:
                        r.then_inc(ins.sem, 16)
                    elif ins.signal:
                        r.then_inc(ins.sem, 1)
                if e in self.dsems:
                    for last in self.dlast[e]:
                        if last is not None:
                            wait(last)

            @block.tensor
            def _(h):
                run("pe", h)

            @block.scalar
            def _(h):
                run("act", h)

            @block.vector
            def _(h):
                run("dve", h)

            @block.gpsimd
            def _(h):
                run("pool", h)

            @block.sync
            def _(h):
                run("sp", h)


class Arena:
    def __init__(self, t, words):
        self.t = t
        self.words = words
        self.off = 0
        self.n = 0

    def alloc(self, name, shape, dt=F32, at=None):
        n = 1
        for s in shape:
            n *= s
        esz = 4 if dt in (F32, I32) else 2
        w = (n * esz + 3) // 4
        w = (w + 7) // 8 * 8
        off = self.off if at is None else at
        assert off + w <= self.words, "arena overflow at %s: %d + %d > %d" % (name, off, w, self.words)
        ap = self.t[:, off:off + w]
        if dt != F32:
            ap = ap.bitcast(dt)
        ap = ap[:, 0:n]
        if len(shape) == 2:
            ap = ap.rearrange("p (a b) -> p a b", a=shape[0])
        elif len(shape) == 3:
            ap = ap.rearrange("p (a b c) -> p a b c", a=shape[0], b=shape[1])
        if at is None:
            self.off += w
        self.n += 1
        b = Buf(name, ap)
        b.off = off
        return b


def build(NG=8, NWG=8, debug=False, stop=99):
    nc = bass.Bass("TRN2", target_bir_lowering=False)
    TOK = NG * 512
    WTOK = NWG * 512
    NT = TOK // 128
    NB = -(-(2 * TOK + NEXP * 255) // 256)
    PR = NB * 256
    okind = "ExternalOutput" if debug else "Internal"

    def din(name, shape, dt=F32):
        return nc.dram_tensor(name, shape, dt, kind="ExternalInput")

    xT_d = din("xT", [D, TOK])
    xTp_d = din("xTp", [D, WTOK])
    xtok_d = din("xtok", [TOK, D])
    pT_d = din("pT", [256, TOK])
    whg_d = din("whg", [8, D, 512])
    wsc_d = din("wsc", [8, D, 384])
    wout_d = din("wout", [D, D])
    wr_d = din("wr", [128, 16, 36])
    wgt_d = din("wgt", [NEXP * 128 * 4, 2048])
    wut_d = din("wut", [NEXP * 128 * 4, 2048])
    wdt_d = din("wdt", [NEXP * 128 * 4, 2048])
    wple_d = din("wple", [256, D])
    wpg_d = din("wpg", [D, D])
    lb_d = din("lb", [2, 1024])
    hgn_d = din("hgn", [1, 128])
    gffn_d = din("gffn", [1, D])
    gple_d = din("gple", [1, D])
    gfin_d = din("gfin", [1, D])
    cols_d = din("cols", [128, 64])
    zeros_d = din("zeros", [256, D], BF16)
    out_d = nc.dram_tensor("out", [TOK, D], F32, kind="ExternalOutput")
    xmid_d = nc.dram_tensor("xmid", [TOK, D], F32, kind=okind)
    hn_d = nc.dram_tensor("hn", [TOK, D], BF16, kind="Internal")
    xs_d = nc.dram_tensor("xs", [PR, D], BF16, kind="Internal")
    ys_d = nc.dram_tensor("ys", [PR, D], F32, kind="Internal")
    if debug:
        dbg_d = nc.dram_tensor("dbg", [128, 8, NT], F32, kind="ExternalOutput")
        dbgb_d = nc.dram_tensor("dbgb", [128, NB], F32, kind="ExternalOutput")

    with ExitStack() as es:
        S = Sched(nc, es)
        AW = 51200
        arena_t = es.enter_context(nc.sbuf_tensor("arena", [128, AW], F32))
        A = Arena(arena_t, AW)
        banks = [Buf("bank%d" % i, es.enter_context(nc.psum_tensor("bank%d" % i, [128, 512], F32))) for i in range(8)]
        for bk_ in banks:
            bk_.excl = True
        B6b = banks[6].t[:, :].bitcast(BF16)
        B5b = banks[5].t[:, :].bitcast(BF16)

        ident_f = A.alloc("ident_f", [128])
        ident_b = A.alloc("ident_b", [128], BF16)
        tri2 = A.alloc("tri2", [128])
        stri = A.alloc("stri", [128])
        ones_f = A.alloc("ones_f", [128])
        ones_b = A.alloc("ones_b", [128], BF16)
        w12_all = A.alloc("w12_all", [2, NT])
        dest_f = A.alloc("dest_f", [2, NT])
        dest_i = A.alloc("dest_i", [2, NT], I32)
        idxw = A.alloc("idxw", [4, NB], I32)
        p4 = A.alloc("p4", [4])
        true_persist_mark = A.off
        ind2 = A.alloc("ind2", [2])
        oml = A.alloc("oml", [1024])
        hgn = A.alloc("hgn", [128])
        gffn = A.alloc("gffn", [D])
        cols = A.alloc("cols", [64])
        wr = A.alloc("wr", [16, 36])
        Sst = [A.alloc("S%d" % h, [128]) for h in range(8)]
        Sbf = [A.alloc("Sb%d" % h, [128], BF16) for h in range(8)]
        tails = A.alloc("tails", [8, 2])
        oh1_all = A.alloc("oh1_all", [NT, 32])
        oh2_all = A.alloc("oh2_all", [NT, 32])
        rank_all = A.alloc("rank_all", [NT, 32])
        Csum = A.alloc("Csum", [32])
        gmix_c = lambda c: cols[:, c:c + 1]
        gffn_c = lambda c: cols[:, 16 + c:17 + c]
        scn_c = lambda j: cols[:, 32 + j:33 + j]
        cw_c = lambda j, k: cols[:, 40 + 3 * j + k:41 + 3 * j + k]

        def memset(eng, buf, ap, val):
            S.op(eng, lambda g: g.memset(ap, val), writes=[buf])

        memset("pool", ident_f, ident_f[:], 1.0)
        S.op("pool", lambda g: g.affine_select(out=ident_f[:], in_=ident_f[:], pattern=[[-1, 128]], compare_op=ALU.is_equal,
                                                fill=0.0, base=0, channel_multiplier=1), reads=[ident_f], writes=[ident_f])
        S.op("dve", lambda g: g.tensor_copy(out=ident_b[:], in_=ident_f[:]), reads=[ident_f], writes=[ident_b])
        memset("pool", tri2, tri2[:], 1.0)
        S.op("pool", lambda g: g.affine_select(out=tri2[:], in_=tri2[:], pattern=[[1, 128]], compare_op=ALU.is_ge,
                                                fill=0.0, base=0, channel_multiplier=-1), reads=[tri2], writes=[tri2])
        memset("pool", tri2, tri2[0:64, 64:128], 0.0)
        memset("pool", stri, stri[:], 1.0)
        S.op("pool", lambda g: g.affine_select(out=stri[:], in_=stri[:], pattern=[[1, 128]], compare_op=ALU.is_ge,
                                                fill=0.0, base=-1, channel_multiplier=-1), reads=[stri], writes=[stri])
        memset("pool", ones_f, ones_f[:], 1.0)
        memset("pool", ones_b, ones_b[:], 1.0)
        memset("pool", ind2, ind2[:], 0.0)
        memset("pool", ind2, ind2[0:64, 0:1], 1.0)
        memset("pool", ind2, ind2[64:128, 1:2], 1.0)
        memset("pool", tails, tails[:], 0.0)
        memset("pool", Csum, Csum[:], 0.0)
        for h in range(8):
            memset("pool", Sst[h], Sst[h][:], 0.0)
            memset("pool", Sbf[h], Sbf[h][:], 0.0)
        S.op("pool", lambda g: g.iota(p4[:], pattern=[[1, 4]], base=0, channel_multiplier=4,
                                      allow_small_or_imprecise_dtypes=True), writes=[p4])
        tmp_lb = A.alloc("tmp_lb", [2, 1024])
        S.dma("sp", lambda g: g.dma_start(out=tmp_lb[:, 0, :], in_=lb_d.ap()[0:1, :].partition_broadcast(128)), writes=[tmp_lb])
        S.dma("sp", lambda g: g.dma_start(out=tmp_lb[:, 1, :], in_=lb_d.ap()[1:2, :].partition_broadcast(128)), writes=[tmp_lb])
        S.dma("sp", lambda g: g.dma_start(out=hgn[:], in_=hgn_d.ap().partition_broadcast(128)), writes=[hgn])
        S.dma("sp", lambda g: g.dma_start(out=gffn[:], in_=gffn_d.ap().partition_broadcast(128)), writes=[gffn])
        S.dma("sp", lambda g: g.dma_start(out=cols[:], in_=cols_d.ap()), writes=[cols])
        S.dma("sp", lambda g: g.dma_start(out=wr[:], in_=wr_d.ap()), writes=[wr])
        S.op("dve", lambda g: g.tensor_tensor(out=oml[:], in0=tmp_lb[:, 0, :], in1=tmp_lb[:, 1, :], op=ALU.subtract),
             reads=[tmp_lb], writes=[oml])
        S.op("act", lambda g: g.activation(out=oml[:], in_=oml[:], func=AF.Exp), reads=[oml], writes=[oml])
        S.op("dve", lambda g: g.tensor_scalar(out=oml[:], in0=oml[:], scalar1=1.0, scalar2=None, op0=ALU.add), reads=[oml], writes=[oml])
        S.op("dve", lambda g: g.reciprocal(out=oml[:], in_=oml[:]), reads=[oml], writes=[oml])
        for c in range(16):
            S.op("pool", (lambda c: lambda g: g.tensor_scalar(out=wr[:, c, :], in0=wr[:, c, :], scalar1=gffn_c(c), scalar2=None,
                                                              op0=ALU.mult))(c), reads=[wr, cols], writes=[wr])
        A.off -= 2048 + 0
        S.barrier()
        persist_mark = A.off

        wslab = [A.alloc("wslab%d" % i, [16, 512], BF16) for i in range(2)]
        xb = A.alloc("xb", [16, 512], BF16)
        xp = [A.alloc("xp%d" % i, [2, 512]) for i in range(4)]
        xmid_off = None
        sq = [A.alloc("sq%d" % i, [2, 512], BF16) for i in range(2)]
        omixT = A.alloc("omixT", [16, 512], BF16)
        byb = A.alloc("byb", [8, 512], BF16)
        ubuf = A.alloc("ubuf", [514])
        t1 = A.alloc("t1", [512])
        yv = A.alloc("yv", [512])
        yv2 = A.alloc("yv2", [512])
        sqy = A.alloc("sqy", [512], BF16)
        rstd_bc = A.alloc("rstd_bc", [512])
        rstd2_bc = A.alloc("rstd2_bc", [512])
        rstd_sc = A.alloc("rstd_sc", [512])
        lnbc = A.alloc("lnbc", [512])
        rcol = A.alloc("rcol", [8])
        xmid = [A.alloc("xmid0", [D], at=xb.off), A.alloc("xmid1", [D], at=xb.off + 2048),
                A.alloc("xmid2", [D], at=xp[0].off), A.alloc("xmid3", [D], at=xp[2].off)]
        assert xp[1].off == xp[0].off + 1024 and xp[3].off == xp[2].off + 1024
        for xm_, al_ in ((xmid[0], [xb]), (xmid[1], [xb]), (xmid[2], [xp[0], xp[1]]), (xmid[3], [xp[2], xp[3]])):
            xm_.aliases = list(al_)
            for a_ in al_:
                a_.aliases.append(xm_)
        xmT = A.alloc("xmT", [16, 128])
        hnb = A.alloc("hnb", [D], BF16)
        junk = A.alloc("junk", [D], BF16)
        etmp = [A.alloc("etmp%d" % t, [256]) for t in range(2)]
        qg = [[A.alloc("qg%d_%d" % (p_, t), [256]) for t in range(4)] for p_ in range(2)]
        kk = [[A.alloc("kk%d_%d" % (p_, t), [128]) for t in range(4)] for p_ in range(2)]
        lf = [[A.alloc("lf%d_%d" % (p_, t), [128]) for t in range(4)] for p_ in range(2)]
        vb = [[A.alloc("vb%d_%d" % (p_, t), [128], BF16) for t in range(4)] for p_ in range(2)]
        NTMP = 3
        eA = [A.alloc("eA%d" % i, [128]) for i in range(NTMP)]
        enA = [A.alloc("enA%d" % i, [128]) for i in range(NTMP)]
        Ecol = [A.alloc("Ecol%d" % i, [2]) for i in range(NTMP)]
        qt = [A.alloc("qt%d" % i, [128], BF16) for i in range(NTMP)]
        kt = [A.alloc("kt%d" % i, [128], BF16) for i in range(NTMP)]
        qtT = [A.alloc("qtT%d" % i, [128], BF16) for i in range(NTMP)]
        ktT = [A.alloc("ktT%d" % i, [128], BF16) for i in range(NTMP)]
        scTm = [A.alloc("scTm%d" % i, [128], BF16) for i in range(NTMP)]
        U0 = [A.alloc("U0%d" % i, [128]) for i in range(NTMP)]
        S1 = [A.alloc("S1%d" % i, [128]) for i in range(NTMP)]
        S1b = [A.alloc("S1b%d" % i, [128], BF16) for i in range(NTMP)]
        gsn = [A.alloc("gsn%d" % i, [128]) for i in range(NTMP)]
        ogb = [A.alloc("ogb%d" % i, [128], BF16) for i in range(NTMP)]
        sm = [A.alloc("sm%d" % i, [8]) for i in range(NTMP)]
        lg = A.alloc("lg", [36])
        rt = A.alloc("rt", [64])
        Ct = A.alloc("Ct", [32])

        import os
        _dbg2 = os.environ.get("K_DBG2", "")
        _dbg3 = os.environ.get("K_DBG3", "")

        def load_x_group(src_d, g0):
            for i in range(8):
                xpi = xp[i % 4]
                sqi = sq[i % 2]
                S.dma("sp", (lambda i, xpi: lambda g: g.dma_start(
                    out=xpi[:], in_=src_d.ap()[2 * i * 128:(2 * i + 2) * 128, g0 * 512:(g0 + 1) * 512]
                    .rearrange("(c p) t -> p c t", p=128)))(i, xpi), writes=[xpi])
                S.op("act", (lambda xpi, sqi: lambda g: g.activation(out=sqi[:], in_=xpi[:], func=AF.Square))(xpi, sqi),
                     reads=[xpi], writes=[sqi])
                for cc in range(2):
                    c = 2 * i + cc
                    S.op("act", (lambda c, cc, xpi: lambda g: g.activation(out=xb[:, c, :], in_=xpi[:, cc, :], func=AF.Copy, scale=gmix_c(c)))(c, cc, xpi),
                         reads=[xpi, cols], writes=[xb])
                    S.op("pe", (lambda c, cc, sqi: lambda g: g.matmul(banks[7][:, :], lhsT=ones_b[:], rhs=sqi[:, cc, :],
                                                                      start=(c == 0), stop=(c == 15)))(c, cc, sqi),
                         reads=[sqi, ones_b], writes=[banks[7]])
            S.op("act", lambda g: g.activation(out=lnbc[:], in_=banks[7][:, :], func=AF.Ln, scale=1.0 / D, bias=EPS),
                 reads=[banks[7]], writes=[lnbc])
            S.op("act", lambda g: g.activation(out=rstd_bc[:], in_=lnbc[:], func=AF.Exp, scale=-0.5), reads=[lnbc], writes=[rstd_bc])
            S.op("act", lambda g: g.activation(out=rstd2_bc[:], in_=lnbc[:], func=AF.Exp, scale=-1.0), reads=[lnbc], writes=[rstd2_bc])
            if _dbg2 == "nok1":
                return
            for t in range(4):
                S.op("pe", (lambda t: lambda g: g.transpose(out=banks[3][:, 384 + 32 * t:384 + 32 * (t + 1)], in_=rstd_bc[0:32, t * 128:(t + 1) * 128],
                                                            identity=ident_f[0:32, 0:32]))(t),
                     reads=[rstd_bc, ident_f], writes=[banks[3]])
            S.op("dve", lambda g: g.tensor_copy(out=rcol[:, 0:4], in_=banks[3][:, 384:512].rearrange("p (a b) -> p a b", a=4)[:, :, 0]),
                 reads=[banks[3]], writes=[rcol])
            S.op("dve", lambda g: g.tensor_scalar(out=rcol[:, 4:8], in0=rcol[:, 0:4], scalar1=-1.0, scalar2=None, op0=ALU.mult),
                 reads=[rcol], writes=[rcol])


        slab_plan = []
        slab_state = {"i": 0, "issued": 0}

        def plan_slabs():
            for wg in range(NWG):
                for h in range(8):
                    slab_plan.append((whg_d.ap()[h, :, 256:512].rearrange("(c p) n -> p c n", p=128), 256))
                if wg == NWG - 1:
                    for j in range(8):
                        slab_plan.append((wsc_d.ap()[j, :, :].rearrange("(c p) n -> p c n", p=128), 384))
            for g0 in range(NG):
                for h in range(8):
                    slab_plan.append((whg_d.ap()[h, :, :].rearrange("(c p) n -> p c n", p=128), 512))
                for j in range(8):
                    slab_plan.append((wsc_d.ap()[j, :, :].rearrange("(c p) n -> p c n", p=128), 384))
                for n in range(4):
                    slab_plan.append((wout_d.ap()[:, n * 512:(n + 1) * 512].rearrange("(c p) n -> p c n", p=128), 512))

        def issue_slab(i):
            src_ap, ncols = slab_plan[i]
            ws = wslab[i % 2]
            S.dma("pool", (lambda ws, src_ap, ncols: lambda g: g.dma_start(out=ws[:, :, 0:ncols], in_=src_ap))(ws, src_ap, ncols), writes=[ws])

        def next_slab(ncols_expected):
            i = slab_state["i"]
            slab_state["i"] += 1
            while slab_state["issued"] <= min(i + 1, len(slab_plan) - 1):
                issue_slab(slab_state["issued"])
                slab_state["issued"] += 1
            assert slab_plan[i][1] == ncols_expected, (i, slab_plan[i][1], ncols_expected)
            return wslab[i % 2]

        tmp_ctr = {"n": 0, "e": 0}

        def gen_inproj(h, full, par):
            c0 = 0 if full else 256
            ncols = 512 if full else 256
            ws = next_slab(ncols)
            oml_h = oml[:, h * 128:(h + 1) * 128]
            fo = 256 - c0
            for t in range(4):
                bk = banks[t % 2]
                for c in range(16):
                    S.op("pe", (lambda c, t, bk: lambda g: g.matmul(bk[:, 0:ncols], lhsT=xb[:, c, t * 128:(t + 1) * 128],
                                                                     rhs=ws[:, c, 0:ncols], start=(c == 0), stop=(c == 15)))(c, t, bk),
                         reads=[xb, ws], writes=[bk])
                    if c % 2 == 1 and c < 15:
                        yield
                rs = rcol[:, t:t + 1]
                nrs = rcol[:, 4 + t:5 + t]
                qg_t, kk_t, vb_t, lf_t = qg[par][t], kk[par][t], vb[par][t], lf[par][t]
                et = etmp[tmp_ctr["e"] % 2]
                tmp_ctr["e"] += 1
                if full:
                    S.op("act", (lambda et, bk, nrs: lambda g: g.activation(out=et[:], in_=bk[:, 0:256], func=AF.Exp, scale=nrs))(et, bk, nrs),
                         reads=[bk, rcol], writes=[et])
                S.op("act", (lambda kk_t, bk, rs: lambda g: g.activation(out=kk_t[:], in_=bk[:, fo:fo + 128], func=AF.Exp, scale=rs))(kk_t, bk, rs),
                     reads=[bk, rcol], writes=[kk_t])
                S.op("dve", (lambda vb_t, bk, rs: lambda g: g.tensor_scalar(out=vb_t[:], in0=bk[:, fo + 128:fo + 256], scalar1=rs, scalar2=None,
                                                                        op0=ALU.mult))(vb_t, bk, rs), reads=[bk, rcol], writes=[vb_t])
                if full:
                    S.op("act", (lambda et: lambda g: g.activation(out=et[:], in_=et[:], func=AF.Ln, bias=1.0))(et), reads=[et], writes=[et])
                    S.op("act", (lambda et: lambda g: g.activation(out=et[:], in_=et[:], func=AF.Exp, scale=-1.0))(et), reads=[et], writes=[et])
                    S.op("dve", (lambda qg_t, bk, rs, et: lambda g: g.scalar_tensor_tensor(out=qg_t[:], in0=bk[:, 0:256], scalar=rs, in1=et[:],
                                                                                       op0=ALU.mult, op1=ALU.mult))(qg_t, bk, rs, et),
                         reads=[bk, rcol, et], writes=[qg_t])
                S.op("act", (lambda kk_t: lambda g: g.activation(out=kk_t[:], in_=kk_t[:], func=AF.Ln, bias=1.0))(kk_t), reads=[kk_t], writes=[kk_t])
                S.op("act", (lambda kk_t: lambda g: g.activation(out=kk_t[:], in_=kk_t[:], func=AF.Exp, scale=-1.0))(kk_t), reads=[kk_t], writes=[kk_t])
                S.op("dve", (lambda kk_t: lambda g: g.tensor_tensor(out=kk_t[:], in0=kk_t[:], in1=oml_h, op=ALU.mult))(kk_t),
                     reads=[kk_t, oml], writes=[kk_t])
                S.op("act", (lambda kk_t, lf_t: lambda g: g.activation(out=lf_t[:], in_=kk_t[:], func=AF.Ln, scale=-1.0, bias=1.0))(kk_t, lf_t),
                     reads=[kk_t], writes=[lf_t])
                yield

        B2b = banks[2].t[:, :].bitcast(BF16)

        def gen_pre(h, t, full, par, i):
            b3 = banks[3]
            qg_t, kk_t, lf_t = qg[par][t], kk[par][t], lf[par][t]
            S.op("pe", lambda g: g.matmul(b3[:, 0:128], lhsT=tri2[:], rhs=lf_t[:], start=True, stop=True), reads=[tri2, lf_t], writes=[b3])
            S.op("pe", lambda g: g.matmul(b3[:, 128:130], lhsT=lf_t[:], rhs=ind2[:], start=True, stop=True), reads=[lf_t, ind2], writes=[b3])
            yield
            S.op("act", lambda g: g.activation(out=enA[i][:], in_=b3[:, 0:128], func=AF.Exp, scale=-1.0), reads=[b3], writes=[enA[i]])
            if full:
                S.op("act", lambda g: g.activation(out=eA[i][:], in_=b3[:, 0:128], func=AF.Exp), reads=[b3], writes=[eA[i]])
            S.op("act", lambda g: g.activation(out=Ecol[i][:], in_=b3[:, 128:130], func=AF.Exp), reads=[b3], writes=[Ecol[i]])
            S.op("dve", lambda g: g.tensor_tensor(out=kt[i][:], in0=kk_t[:], in1=enA[i][:], op=ALU.mult), reads=[kk_t, enA[i]], writes=[kt[i]])
            if not full:
                return
            S.op("dve", lambda g: g.tensor_tensor(out=qt[i][:], in0=qg_t[:, 0:128], in1=eA[i][:], op=ALU.mult), reads=[qg_t, eA[i]], writes=[qt[i]])
            S.op("pool", lambda g: g.tensor_tensor(out=gsn[i][:], in0=qg_t[:, 128:256], in1=hgn[:], op=ALU.mult), reads=[qg_t, hgn], writes=[gsn[i]])
            S.op("pe", lambda g: g.transpose(out=B6b[:, 0:128], in_=qt[i][:], identity=ident_b[:]), reads=[qt[i], ident_b], writes=[banks[6]])
            S.op("pe", lambda g: g.transpose(out=B6b[:, 128:256], in_=kt[i][:], identity=ident_b[:]), reads=[kt[i], ident_b], writes=[banks[6]])
            yield
            S.op("act", lambda g: g.copy(out=qtT[i][:], in_=B6b[:, 0:128]), reads=[banks[6]], writes=[qtT[i]])
            S.op("act", lambda g: g.copy(out=ktT[i][:], in_=B6b[:, 128:256]), reads=[banks[6]], writes=[ktT[i]])
            S.op("pe", lambda g: g.matmul(b3[:, 256:384], lhsT=ktT[i][:], rhs=qtT[i][:], start=True, stop=True), reads=[ktT[i], qtT[i]], writes=[b3])
            yield
            S.op("dve", lambda g: g.tensor_tensor(out=scTm[i][:], in0=b3[:, 256:384], in1=tri2[:], op=ALU.mult), reads=[b3, tri2], writes=[scTm[i]])

        def gen_state(h, t, full, par, i):
            Sh, Sb = Sst[h], Sbf[h]
            b4, b5 = banks[4], banks[5]
            vb_t = vb[par][t]
            S.op("dve", lambda g: g.tensor_scalar(out=U0[i][:], in0=Sh[:], scalar1=Ecol[i][:, 0:1], scalar2=None, op0=ALU.mult),
                 reads=[Sh, Ecol[i]], writes=[U0[i]])
            S.op("pe", lambda g: g.matmul(b5[:, 0:128], lhsT=kt[i][0:64, :], rhs=vb_t[0:64, :], start=True, stop=True), reads=[kt[i], vb_t], writes=[b5])
            yield
            S.op("dve", lambda g: g.scalar_tensor_tensor(out=S1[i][:], in0=b5[:, 0:128], scalar=Ecol[i][:, 0:1], in1=U0[i][:], op0=ALU.mult, op1=ALU.add),
                 reads=[b5, Ecol[i], U0[i]], writes=[S1[i]])
            S.op("dve", lambda g: g.tensor_scalar(out=U0[i][:], in0=S1[i][:], scalar1=Ecol[i][:, 1:2], scalar2=None, op0=ALU.mult),
                 reads=[S1[i], Ecol[i]], writes=[U0[i]])
            if full:
                S.op("act", lambda g: g.copy(out=S1b[i][:], in_=S1[i][:]), reads=[S1[i]], writes=[S1b[i]])
                S.op("pe", lambda g: g.matmul(b4[:, 0:128], lhsT=scTm[i][:], rhs=vb_t[:], start=True, stop=False), reads=[scTm[i], vb_t], writes=[b4])
                S.op("pe", lambda g: g.matmul(b4[0:64, 0:128], lhsT=qtT[i][:, 0:64], rhs=Sb[:], start=False, stop=True), reads=[qtT[i], Sb], writes=[b4])
                S.op("pe", lambda g: g.matmul(b4[64:128, 0:128], lhsT=qtT[i][:, 64:128], rhs=S1b[i][:], start=False, stop=True),
                     reads=[qtT[i], S1b[i]], writes=[b4])
            S.op("pe", lambda g: g.matmul(b5[:, 128:256], lhsT=kt[i][64:128, :], rhs=vb_t[64:128, :], start=True, stop=True), reads=[kt[i], vb_t], writes=[b5])
            yield
            S.op("dve", lambda g: g.scalar_tensor_tensor(out=Sh[:], in0=b5[:, 128:256], scalar=Ecol[i][:, 1:2], in1=U0[i][:], op0=ALU.mult, op1=ALU.add),
                 reads=[b5, Ecol[i], U0[i]], writes=[Sh])
            if not full:
                return
            S.op("act", lambda g: g.copy(out=Sb[:], in_=Sh[:]), reads=[Sh], writes=[Sb])
            S.op("act", lambda g: g.activation(out=junk[:, 0:128], in_=b4[:, 0:128], func=AF.Square, accum_out=sm[i][:, 0:1]), reads=[b4], writes=[junk, sm[i]])
            S.op("act", lambda g: g.activation(out=sm[i][:, 1:2], in_=sm[i][:, 0:1], func=AF.Ln, scale=1.0 / 128, bias=EPS_HG), reads=[sm[i]], writes=[sm[i]])
            S.op("act", lambda g: g.activation(out=sm[i][:, 2:3], in_=sm[i][:, 1:2], func=AF.Exp, scale=-0.5), reads=[sm[i]], writes=[sm[i]])
            S.op("dve", lambda g: g.scalar_tensor_tensor(out=ogb[i][:], in0=b4[:, 0:128], scalar=sm[i][:, 2:3], in1=gsn[i][:], op0=ALU.mult, op1=ALU.mult),
                 reads=[b4, sm[i], gsn[i]], writes=[ogb[i]])
            S.op("pe", lambda g: g.transpose(out=B2b[:, 0:128], in_=ogb[i][:], identity=ident_b[:]), reads=[ogb[i], ident_b], writes=[banks[2]])
            yield
            S.op("act", lambda g: g.copy(out=omixT[:, h, t * 128:(t + 1) * 128], in_=B2b[:, 0:128]), reads=[banks[2]], writes=[omixT])

        def exhaust(gen):
            if gen is None:
                return
            for _ in gen:
                pass

        def roundrobin(gens, steps):
            live = [g is not None for g in gens]
            while any(live[:-1]):
                for k, g in enumerate(gens):
                    if not live[k]:
                        continue
                    for _ in range(steps[k]):
                        try:
                            next(g)
                        except StopIteration:
                            live[k] = False
                            break

        def run_heads(full):
            exhaust(gen_inproj(0, full, 0))
            units = [(h, t) for h in range(8) for t in range(4)]
            bg = None
            prev = None
            for s_i in range(len(units) + 1):
                cur = units[s_i] if s_i < len(units) else None
                if cur is not None and cur[1] == 1 and cur[0] + 1 < 8:
                    bg = gen_inproj(cur[0] + 1, full, (cur[0] + 1) % 2)
                if cur is not None and cur[1] == 0 and bg is not None:
                    exhaust(bg)
                    bg = None
                gs = []
                if prev is not None:
                    u = s_i - 1
                    gs.append(gen_state(prev[0], prev[1], full, prev[0] % 2, u % NTMP))
                if cur is not None:
                    gs.append(gen_pre(cur[0], cur[1], full, cur[0] % 2, s_i % NTMP))
                gs.append(bg)
                roundrobin(gs, [1] * (len(gs) - 1) + [3])
                prev = cur
            exhaust(bg)

        sc_ctr = {"n": 0}

        def sc_chunk(j, tail_only=False):
            ws = next_slab(384)
            bset = [banks[0], banks[1], banks[2]] if sc_ctr["n"] % 2 == 0 else [banks[3], banks[4], banks[5]]
            sc_ctr["n"] += 1
            for part in range(3):
                if tail_only and part == 0:
                    continue
                bk = bset[part]
                for c in range(16):
                    S.op("pe", (lambda c, part, bk: lambda g: g.matmul(bk[:, :], lhsT=ws[:, c, part * 128:(part + 1) * 128], rhs=xb[:, c, :],
                                                                        start=(c == 0), stop=(c == 15)))(c, part, bk),
                         reads=[ws, xb], writes=[bk])
            S.op("pool", lambda g: g.tensor_copy(out=ubuf[:, 0:2], in_=tails[:, j, :]), reads=[tails], writes=[ubuf])
            S.op("dve", lambda g: g.tensor_tensor(out=t1[:], in0=bset[1][:, :], in1=rstd2_bc[:], op=ALU.mult),
                 reads=[bset[1], rstd2_bc], writes=[t1])
            S.op("dve", lambda g: g.tensor_tensor(out=ubuf[:, 2:514], in0=t1[:], in1=bset[2][:, :], op=ALU.mult),
                 reads=[bset[2], t1], writes=[ubuf])
            S.op("pool", lambda g: g.tensor_copy(out=tails[:, j, :], in_=ubuf[:, 512:514]), reads=[ubuf], writes=[tails])
            if tail_only:
                return
            S.op("act", lambda g: g.activation(out=yv[:], in_=ubuf[:, 0:512], func=AF.Copy, scale=cw_c(j, 0)), reads=[ubuf, cols], writes=[yv])
            S.op("dve", lambda g: g.scalar_tensor_tensor(out=yv[:], in0=ubuf[:, 1:513], scalar=cw_c(j, 1), in1=yv[:], op0=ALU.mult, op1=ALU.add),
                 reads=[ubuf, cols, yv], writes=[yv])
            S.op("dve", lambda g: g.scalar_tensor_tensor(out=yv[:], in0=ubuf[:, 2:514], scalar=cw_c(j, 2), in1=yv[:], op0=ALU.mult, op1=ALU.add),
                 reads=[ubuf, cols, yv], writes=[yv])
            S.op("dve", lambda g: g.tensor_tensor(out=yv[:], in0=yv[:], in1=rstd_bc[:], op=ALU.mult), reads=[yv, rstd_bc], writes=[yv])
            S.op("dve", lambda g: g.tensor_tensor(out=byb[:, j, :], in0=bset[0][:, :], in1=yv[:], op=ALU.mult),
                 reads=[bset[0], yv], writes=[byb])
            S.op("act", lambda g: g.activation(out=sqy[:], in_=byb[:, j, :], func=AF.Square), reads=[byb], writes=[sqy])
            S.op("pe", lambda g: g.matmul(banks[7][:, :], lhsT=ones_b[:], rhs=sqy[:], start=(j == 0), stop=(j == 7)),
                 reads=[ones_b, sqy], writes=[banks[7]])

        def sc_finish():
            S.op("act", lambda g: g.activation(out=lnbc[:], in_=banks[7][:, :], func=AF.Ln, scale=1.0 / 1024, bias=EPS),
                 reads=[banks[7]], writes=[lnbc])
            S.op("act", lambda g: g.activation(out=rstd_sc[:], in_=lnbc[:], func=AF.Exp, scale=-0.5), reads=[lnbc], writes=[rstd_sc])
            for j in range(8):
                S.op("dve", (lambda j: lambda g: g.scalar_tensor_tensor(out=omixT[:, 8 + j, :], in0=byb[:, j, :], scalar=scn_c(j), in1=rstd_sc[:],
                                                                        op0=ALU.mult, op1=ALU.mult))(j),
                     reads=[byb, cols, rstd_sc], writes=[omixT])

        def outproj_and_route(g0):
            for t in range(4):
                r0 = g0 * 512 + t * 128
                S.dma("sp", (lambda t, r0: lambda g: g.dma_start(out=xmid[t][:], in_=xtok_d.ap()[r0:r0 + 128, :]))(t, r0), writes=[xmid[t]])
            k = 0
            for n in range(4):
                ws = next_slab(512)
                for t in range(4):
                    bk = banks[k % 3]
                    k += 1
                    for c in range(16):
                        S.op("pe", (lambda c, t, bk, ws: lambda g: g.matmul(bk[:, :], lhsT=omixT[:, c, t * 128:(t + 1) * 128], rhs=ws[:, c, :],
                                                                            start=(c == 0), stop=(c == 15)))(c, t, bk, ws),
                             reads=[omixT, ws], writes=[bk])
                    S.op("dve", (lambda t, n, bk: lambda g: g.tensor_tensor(out=xmid[t][:, n * 512:(n + 1) * 512], in0=bk[:, :],
                                                                           in1=xmid[t][:, n * 512:(n + 1) * 512], op=ALU.add))(t, n, bk),
                         reads=[bk, xmid[t]], writes=[xmid[t]])
            for t in range(4):
                tile = g0 * 4 + t
                r0 = tile * 128
                xm = xmid[t]
                S.dma("act", (lambda xm, r0: lambda g: g.dma_start(out=xmid_d.ap()[r0:r0 + 128, :], in_=xm[:]))(xm, r0), reads=[xm])
                S.op("act", (lambda xm: lambda g: g.activation(out=junk[:], in_=xm[:], func=AF.Square, accum_out=rt[:, 0:1]))(xm),
                     reads=[xm], writes=[junk, rt])
                S.op("act", lambda g: g.activation(out=rt[:, 1:2], in_=rt[:, 0:1], func=AF.Ln, scale=1.0 / D, bias=EPS), reads=[rt], writes=[rt])
                S.op("act", lambda g: g.activation(out=rt[:, 2:3], in_=rt[:, 1:2], func=AF.Exp, scale=-0.5), reads=[rt], writes=[rt])
                S.op("dve", (lambda xm: lambda g: g.scalar_tensor_tensor(out=hnb[:], in0=xm[:], scalar=rt[:, 2:3], in1=gffn[:],
                                                                        op0=ALU.mult, op1=ALU.mult))(xm), reads=[xm, rt, gffn], writes=[hnb])
                S.dma("act", (lambda r0: lambda g: g.dma_start(out=hn_d.ap()[r0:r0 + 128, :], in_=hnb[:]))(r0), reads=[hnb])
                for q4 in range(4):
                    bk = banks[q4 % 3]
                    for cc in range(4):
                        c = q4 * 4 + cc
                        S.op("pe", (lambda c, cc, bk, xm: lambda g: g.transpose(out=bk[:, cc * 128:(cc + 1) * 128], in_=xm[:, c * 128:(c + 1) * 128],
                                                                              identity=ident_f[:]))(c, cc, bk, xm),
                             reads=[xm, ident_f], writes=[bk])
                    eng = "act" if q4 % 2 == 0 else "dve"
                    if eng == "act":
                        S.op("act", (lambda q4, bk: lambda g: g.copy(out=xmT[:, q4 * 4:(q4 + 1) * 4, :], in_=bk[:, :].rearrange("p (a b) -> p a b", a=4)))(q4, bk),
                             reads=[bk], writes=[xmT])
                    else:
                        S.op("dve", (lambda q4, bk: lambda g: g.tensor_copy(out=xmT[:, q4 * 4:(q4 + 1) * 4, :], in_=bk[:, :].rearrange("p (a b) -> p a b", a=4)))(q4, bk),
                             reads=[bk], writes=[xmT])
                for c in range(16):
                    S.op("pe", (lambda c: lambda g: g.matmul(banks[7][:, 0:36], lhsT=xmT[:, c, :], rhs=wr[:, c, :], start=(c == 0), stop=(c == 15)))(c),
                         reads=[xmT, wr], writes=[banks[7]])
                S.op("dve", lambda g: g.tensor_scalar(out=lg[:], in0=banks[7][:, 0:36], scalar1=rt[:, 2:3], scalar2=None, op0=ALU.mult),
                     reads=[banks[7], rt], writes=[lg])
                route_tile(tile)

        def route_tile(tile):
            P = "dve"
            R = [lg, rt]
            S.op("dve", lambda g: g.tensor_reduce(out=rt[:, 3:4], in_=lg[:, 0:4], axis=AX.X, op=ALU.max), reads=R, writes=[rt])
            S.op("dve", lambda g: g.tensor_scalar(out=rt[:, 12:16], in0=lg[:, 0:4], scalar1=rt[:, 3:4], scalar2=None, op0=ALU.is_equal), reads=R, writes=[rt])
            S.op("dve", lambda g: g.tensor_scalar(out=rt[:, 48:52], in0=lg[:, 0:4], scalar1=rt[:, 3:4], scalar2=None, op0=ALU.subtract), reads=R, writes=[rt])
            S.op("act", lambda g: g.activation(out=rt[:, 48:52], in_=rt[:, 48:52], func=AF.Exp, accum_out=rt[:, 4:5]), reads=R, writes=[rt])
            S.op("dve", lambda g: g.reciprocal(out=rt[:, 5:6], in_=rt[:, 4:5]), reads=R, writes=[rt])
            S.op("dve", lambda g: g.tensor_scalar(out=rt[:, 16:24], in0=lg[:, 4:12], scalar1=rt[:, 12:13], scalar2=None, op0=ALU.mult), reads=R, writes=[rt])
            for gi in range(1, 4):
                S.op("dve", (lambda gi: lambda g: g.scalar_tensor_tensor(out=rt[:, 16:24], in0=lg[:, 4 + 8 * gi:12 + 8 * gi], scalar=rt[:, 12 + gi:13 + gi],
                                                                         in1=rt[:, 16:24], op0=ALU.mult, op1=ALU.add))(gi), reads=R, writes=[rt])
            S.op("dve", lambda g: g.tensor_reduce(out=rt[:, 6:7], in_=rt[:, 16:24], axis=AX.X, op=ALU.max), reads=R, writes=[rt])
            S.op("dve", lambda g: g.tensor_scalar(out=rt[:, 24:32], in0=rt[:, 16:24], scalar1=rt[:, 6:7], scalar2=None, op0=ALU.is_equal), reads=R, writes=[rt])
            S.op("dve", lambda g: g.scalar_tensor_tensor(out=rt[:, 32:40], in0=rt[:, 24:32], scalar=-1e30, in1=rt[:, 16:24], op0=ALU.mult, op1=ALU.add),
                 reads=R, writes=[rt])
            S.op("dve", lambda g: g.tensor_reduce(out=rt[:, 7:8], in_=rt[:, 32:40], axis=AX.X, op=ALU.max), reads=R, writes=[rt])
            S.op("dve", lambda g: g.tensor_scalar(out=rt[:, 40:48], in0=rt[:, 32:40], scalar1=rt[:, 7:8], scalar2=None, op0=ALU.is_equal), reads=R, writes=[rt])
            S.op("dve", lambda g: g.tensor_tensor(out=rt[:, 8:9], in0=rt[:, 7:8], in1=rt[:, 6:7], op=ALU.subtract), reads=R, writes=[rt])
            S.op("act", lambda g: g.activation(out=rt[:, 8:9], in_=rt[:, 8:9], func=AF.Exp), reads=R, writes=[rt])
            S.op("dve", lambda g: g.tensor_scalar(out=rt[:, 8:9], in0=rt[:, 8:9], scalar1=1.0, scalar2=None, op0=ALU.add), reads=R, writes=[rt])
            S.op("dve", lambda g: g.reciprocal(out=rt[:, 8:9], in_=rt[:, 8:9]), reads=R, writes=[rt])
            S.op("dve", lambda g: g.tensor_tensor(out=w12_all[:, 0, tile:tile + 1], in0=rt[:, 8:9], in1=rt[:, 5:6], op=ALU.mult), reads=R, writes=[w12_all])
            S.op("dve", lambda g: g.tensor_tensor(out=w12_all[:, 1, tile:tile + 1], in0=rt[:, 5:6], in1=w12_all[:, 0, tile:tile + 1], op=ALU.subtract),
                 reads=R + [w12_all], writes=[w12_all])
            for gi in range(4):
                S.op(P, (lambda gi: lambda g: g.tensor_scalar(out=oh1_all[:, tile, gi * 8:(gi + 1) * 8], in0=rt[:, 24:32], scalar1=rt[:, 12 + gi:13 + gi],
                                                               scalar2=None, op0=ALU.mult))(gi), reads=R, writes=[oh1_all])
                S.op(P, (lambda gi: lambda g: g.tensor_scalar(out=oh2_all[:, tile, gi * 8:(gi + 1) * 8], in0=rt[:, 40:48], scalar1=rt[:, 12 + gi:13 + gi],
                                                               scalar2=None, op0=ALU.mult))(gi), reads=R, writes=[oh2_all])
            S.op(P, lambda g: g.tensor_tensor(out=Ct[:], in0=oh1_all[:, tile, :], in1=oh2_all[:, tile, :], op=ALU.add), reads=[oh1_all, oh2_all], writes=[Ct])
            S.op("pe", lambda g: g.matmul(banks[3][:, 400:432], lhsT=stri[:], rhs=Ct[:], start=True, stop=False), reads=[stri, Ct], writes=[banks[3]])
            S.op("pe", lambda g: g.matmul(banks[3][:, 400:432], lhsT=ones_f[:], rhs=Csum[:], start=False, stop=True), reads=[ones_f, Csum], writes=[banks[3]])
            S.op("dve", lambda g: g.tensor_copy(out=rank_all[:, tile, :], in_=banks[3][:, 400:432]), reads=[banks[3]], writes=[rank_all])
            S.op(P, lambda g: g.tensor_tensor(out=Csum[:], in0=Csum[:], in1=Ct[:], op=ALU.add), reads=[Csum, Ct], writes=[Csum])

        if stop <= 0:
            S.emit()
            return nc
        import os
        _dbg = os.environ.get("K_DBG", "")
        plan_slabs()
        for wg in range(NWG):
            load_x_group(xTp_d, wg)
            if _dbg == "lx":
                S.emit()
                return nc
            run_heads(False)
            if _dbg == "h8":
                S.emit()
                return nc
            if wg == NWG - 1:
                for j in range(8):
                    sc_chunk(j, tail_only=True)
        if stop <= 1:
            S.emit()
            return nc
        for h in range(8):
            S.op("act", (lambda h: lambda g: g.copy(out=Sbf[h][:], in_=Sst[h][:]))(h), reads=[Sst[h]], writes=[Sbf[h]])
        for zb in range(NB):
            S.dma("act", (lambda zb: lambda g: g.dma_start(out=xs_d.ap()[zb * 256:(zb + 1) * 256, :], in_=zeros_d.ap()))(zb))
        for g0 in range(NG):
            load_x_group(xT_d, g0)
            run_heads(True)
            for j in range(8):
                sc_chunk(j)
            sc_finish()
            outproj_and_route(g0)

        if stop <= 2:
            S.emit()
            return nc
        S.barrier()
        A.off = persist_mark
        fin = A.alloc("fin", [512])
        fin_i = A.alloc("fin_i", [128], I32)
        b3 = banks[3]
        S.op("pe", lambda g: g.matmul(b3[0:32, 0:128], lhsT=Csum[:], rhs=ones_f[:], start=True, stop=True), reads=[Csum, ones_f], writes=[b3])
        S.op("dve", lambda g: g.tensor_scalar(out=fin[0:32, 0:128], in0=b3[0:32, 0:128], scalar1=255.0, scalar2=None, op0=ALU.add), reads=[b3], writes=[fin])
        S.op("dve", lambda g: g.tensor_copy(out=fin_i[0:32, :], in_=fin[0:32, 0:128]), reads=[fin], writes=[fin_i])
        S.op("dve", lambda g: g.tensor_scalar(out=fin_i[0:32, :], in0=fin_i[0:32, :], scalar1=8, scalar2=8, op0=ALU.arith_shift_right,
                                              op1=ALU.logical_shift_left), reads=[fin_i], writes=[fin_i])
        S.op("dve", lambda g: g.tensor_copy(out=fin[0:32, 0:128], in_=fin_i[0:32, :]), reads=[fin_i], writes=[fin])
        S.op("pe", lambda g: g.matmul(b3[:, 128:160], lhsT=fin[0:32, 0:128], rhs=tri2[0:32, 0:32], start=True, stop=True), reads=[fin, tri2], writes=[b3])
        S.op("pe", lambda g: g.matmul(b3[:, 160:192], lhsT=fin[0:32, 0:128], rhs=stri[0:32, 0:32], start=True, stop=True), reads=[fin, stri], writes=[b3])
        S.op("dve", lambda g: g.tensor_copy(out=fin[:, 128:192], in_=b3[:, 128:192]), reads=[b3], writes=[fin])
        pend = fin[:, 128:160]
        pstart = fin[:, 160:192]
        tmp32 = fin[:, 192:224]
        for tile in range(NT):
            for k, oh in ((0, oh1_all), (1, oh2_all)):
                S.op("dve", (lambda tile: lambda g: g.tensor_tensor(out=tmp32, in0=rank_all[:, tile, :], in1=pstart, op=ALU.add))(tile),
                     reads=[rank_all, fin], writes=[fin])
                S.op("dve", (lambda tile, oh: lambda g: g.tensor_tensor(out=tmp32, in0=tmp32, in1=oh[:, tile, :], op=ALU.mult))(tile, oh),
                     reads=[oh, fin], writes=[fin])
                S.op("dve", (lambda tile, k: lambda g: g.tensor_reduce(out=dest_f[:, k, tile:tile + 1], in_=tmp32, axis=AX.X, op=ALU.add))(tile, k),
                     reads=[fin], writes=[dest_f])
        S.op("dve", lambda g: g.tensor_copy(out=dest_i[:], in_=dest_f[:]), reads=[dest_f], writes=[dest_i])
        bthr = A.alloc("bthr", [NB])
        bacc = A.alloc("bacc", [NB])
        idxf = A.alloc("idxf", [4, NB])
        S.op("pool", lambda g: g.iota(bthr[:], pattern=[[256, NB]], base=0, channel_multiplier=0, allow_small_or_imprecise_dtypes=True), writes=[bthr])
        memset("pool", bacc, bacc[:], 0.0)
        for e in range(NEXP):
            S.op("dve", (lambda e: lambda g: g.scalar_tensor_tensor(out=bacc[:], in0=bthr[:], scalar=fin[:, 128 + e:129 + e], in1=bacc[:],
                                                                   op0=ALU.is_ge, op1=ALU.add))(e), reads=[bthr, fin, bacc], writes=[bacc])
        for cc in range(4):
            S.op("dve", (lambda cc: lambda g: g.tensor_scalar(out=idxf[:, cc, :], in0=bacc[:], scalar1=512.0, scalar2=p4[:, cc:cc + 1],
                                                             op0=ALU.mult, op1=ALU.add))(cc), reads=[bacc, p4], writes=[idxf])
        S.op("dve", lambda g: g.tensor_copy(out=idxw[:], in_=idxf[:]), reads=[idxf], writes=[idxw])
        if debug:
            dbgt = A.alloc("dbgt", [8, NT])
            S.op("dve", lambda g: g.tensor_copy(out=dbgt[:, 0:2, :], in_=dest_f[:]), reads=[dest_f], writes=[dbgt])
            S.op("dve", lambda g: g.tensor_copy(out=dbgt[:, 2:4, :], in_=w12_all[:]), reads=[w12_all], writes=[dbgt])
            S.op("dve", lambda g: g.memset(dbgt[:, 4:8, :], 0.0), writes=[dbgt])
            S.dma("sp", lambda g: g.dma_start(out=dbg_d.ap(), in_=dbgt[:]), reads=[dbgt])
            S.dma("sp", lambda g: g.dma_start(out=dbgb_d.ap(), in_=bacc[:]), reads=[bacc])
        S.barrier()

        if stop <= 3:
            S.emit()
            return nc
        A.off = true_persist_mark
        hnt = [A.alloc("hnt%d" % i, [D], BF16) for i in range(2)]
        for tile in range(NT):
            ht = hnt[tile % 2]
            r0 = tile * 128
            S.dma("sp", (lambda ht, r0: lambda g: g.dma_start(out=ht[:], in_=hn_d.ap()[r0:r0 + 128, :]))(ht, r0), writes=[ht])
            for k in range(2):
                S.dma("pool", (lambda ht, k, tile: lambda g: g.indirect_dma_start(
                    out=xs_d.ap(), out_offset=bass.IndirectOffsetOnAxis(ap=dest_i[:, k, tile:tile + 1], axis=0),
                    in_=ht[:], in_offset=None))(ht, k, tile), reads=[ht, dest_i])
        S.barrier()
        if stop <= 4:
            S.emit()
            return nc
        A.off = true_persist_mark
        wgb = [A.alloc("wgb%d" % i, [16, 512], BF16) for i in range(2)]
        wub = [A.alloc("wub%d" % i, [16, 512], BF16) for i in range(2)]
        wdb = [A.alloc("wdb%d" % i, [4, 2048], BF16) for i in range(2)]
        xt = [A.alloc("xt%d" % i, [D], BF16) for i in range(2)]
        xsT2 = [A.alloc("xsT", [16, 256], BF16)]
        actT2 = [A.alloc("actT%d" % i, [4, 256], BF16) for i in range(2)]
        ee = A.alloc("ee", [256])
        ga = A.alloc("ga", [256])
        yrow = [A.alloc("yrow0", [D])]
        yrow.append(yrow[0])
        NSTG = min(8, (A.words - A.off) // 2048)
        assert NSTG >= 3, NSTG
        stage = [A.alloc("stage%d" % i, [2048]) for i in range(NSTG)]
        stg = {"n": 0}
        cast_engs = ["act", "dve"]

        assert NSTG == 8, NSTG
        bnd_reg = {}
        for st_ in stage:
            S.op("pool", (lambda st_: lambda g: g.memset(st_[:], 0.0))(st_), writes=[st_])

        def w_piece(b, i):
            k = b % 2
            if i < 4:
                return wgt_d, wgb[k], i, True
            if i < 8:
                return wut_d, wub[k], i - 4, True
            return wdt_d, wdb[k], i - 8, False

        def gather_w(b, i):
            tab, dst, cc, gu = w_piece(b, i)
            st = stage[i % NSTG]
            def fn(g, st=st, tab=tab, cc=cc, b=b):
                if "r" not in bnd_reg:
                    bnd_reg["r"] = g.alloc_register("wbound")
                    g.reg_mov(bnd_reg["r"], NEXP * 128 * 4 - 1)
                return g.indirect_dma_start(out=st[:], out_offset=None, in_=tab.ap(),
                                            in_offset=bass.IndirectOffsetOnAxis(ap=idxw[:, cc, b:b + 1], axis=0),
                                            bounds_check=bnd_reg["r"], oob_is_err=False)
            S.dma("pool", fn, reads=[idxw], writes=[st])

        def cast_w(b, i):
            tab, dst, cc, gu = w_piece(b, i)
            st = stage[i % NSTG]
            if gu:
                dview = dst[:, 4 * cc:4 * cc + 4, :]
                sview = st[:].rearrange("p (a b) -> p a b", a=4)
            else:
                dview = dst[:, cc, :]
                sview = st[:]
            if i % 2 == 0:
                S.op("act", (lambda dview, sview: lambda g: g.copy(out=dview, in_=sview))(dview, sview), reads=[st], writes=[dst])
            else:
                S.op("dve", (lambda dview, sview: lambda g: g.tensor_copy(out=dview, in_=sview))(dview, sview), reads=[st], writes=[dst])
            if i + NSTG < 12:
                gather_w(b, i + NSTG)

        for i in range(NSTG):
            gather_w(0, i)
        for i in range(12):
            cast_w(0, i)
        for b in range(NB):
            k = b % 2
            xsT = xsT2[b % len(xsT2)]
            actT = actT2[b % 2]
            if b + 1 < NB:
                for i in range(NSTG):
                    gather_w(b + 1, i)
            for r in range(2):
                r0 = b * 256 + r * 128
                S.dma("sp", (lambda r, r0: lambda g: g.dma_start(out=xt[r][:], in_=xs_d.ap()[r0:r0 + 128, :]))(r, r0), writes=[xt[r]])
                for hh in range(2):
                    bkb, bk = (B6b, banks[6]) if hh == 0 else (B5b, banks[5])
                    for cc in range(8):
                        c = hh * 8 + cc
                        S.op("pe", (lambda c, cc, r, bkb: lambda g: g.transpose(out=bkb[:, cc * 128:(cc + 1) * 128], in_=xt[r][:, c * 128:(c + 1) * 128],
                                                                             identity=ident_b[:]))(c, cc, r, bkb), reads=[xt[r], ident_b], writes=[bk])
                    if hh == 0:
                        S.op("act", (lambda r, bkb, xsT: lambda g: g.copy(out=xsT[:, 0:8, r * 128:(r + 1) * 128], in_=bkb.rearrange("p (a b) -> p a b", a=8)))(r, bkb, xsT),
                             reads=[bk], writes=[xsT])
                    else:
                        S.op("dve", (lambda r, bkb, xsT: lambda g: g.tensor_copy(out=xsT[:, 8:16, r * 128:(r + 1) * 128], in_=bkb.rearrange("p (a b) -> p a b", a=8)))(r, bkb, xsT),
                             reads=[bk], writes=[xsT])
            for fc in range(4):
                bg, bu = banks[0 + 2 * (fc % 2)], banks[1 + 2 * (fc % 2)]
                for c in range(16):
                    S.op("pe", (lambda c, fc, bg, k, xsT: lambda g: g.matmul(bg[:, 0:256], lhsT=wgb[k][:, c, fc * 128:(fc + 1) * 128], rhs=xsT[:, c, :],
                                                                     start=(c == 0), stop=(c == 15)))(c, fc, bg, k, xsT), reads=[wgb[k], xsT], writes=[bg])
                for c in range(16):
                    S.op("pe", (lambda c, fc, bu, k, xsT: lambda g: g.matmul(bu[:, 0:256], lhsT=wub[k][:, c, fc * 128:(fc + 1) * 128], rhs=xsT[:, c, :],
                                                                     start=(c == 0), stop=(c == 15)))(c, fc, bu, k, xsT), reads=[wub[k], xsT], writes=[bu])
                S.op("act", (lambda bg: lambda g: g.activation(out=ee[:], in_=bg[:, 0:256], func=AF.Exp, scale=-1.0))(bg), reads=[bg], writes=[ee])
                S.op("act", lambda g: g.activation(out=ee[:], in_=ee[:], func=AF.Ln, bias=1.0), reads=[ee], writes=[ee])
                S.op("act", lambda g: g.activation(out=ee[:], in_=ee[:], func=AF.Exp, scale=-1.0), reads=[ee], writes=[ee])
                S.op("dve", (lambda bg: lambda g: g.tensor_tensor(out=ga[:], in0=bg[:, 0:256], in1=ee[:], op=ALU.mult))(bg), reads=[bg, ee], writes=[ga])
                S.op("dve", (lambda bu, fc, actT: lambda g: g.tensor_tensor(out=actT[:, fc, :], in0=bu[:, 0:256], in1=ga[:], op=ALU.mult))(bu, fc, actT),
                     reads=[bu, ga], writes=[actT])
                if b + 1 < NB:
                    for i in (3 * fc, 3 * fc + 1, 3 * fc + 2):
                        cast_w(b + 1, i)
            kk2 = 0
            for r in range(2):
                for n in range(4):
                    bk = banks[4 + (kk2 % 2) * 3]
                    kk2 += 1
                    for fc in range(4):
                        S.op("pe", (lambda fc, r, n, bk, k, actT: lambda g: g.matmul(bk[:, :], lhsT=actT[:, fc, r * 128:(r + 1) * 128],
                                                                            rhs=wdb[k][:, fc, n * 512:(n + 1) * 512], start=(fc == 0), stop=(fc == 3)))(fc, r, n, bk, k, actT),
                             reads=[actT, wdb[k]], writes=[bk])
                    S.op("act", (lambda r, n, bk: lambda g: g.copy(out=yrow[r][:, n * 512:(n + 1) * 512], in_=bk[:, :]))(r, n, bk), reads=[bk], writes=[yrow[r]])
                r0 = b * 256 + r * 128
                S.dma("act", (lambda r, r0: lambda g: g.dma_start(out=ys_d.ap()[r0:r0 + 128, :], in_=yrow[r][:]))(r, r0), reads=[yrow[r]])
        S.barrier()

        if stop <= 5:
            S.emit()
            return nc
        A.off = true_persist_mark
        wpg = A.alloc("wpg", [16, D], BF16)
        wple = A.alloc("wple", [2, D], BF16)
        gple = A.alloc("gple", [D])
        gfin = A.alloc("gfin", [D])
        xm3s = [A.alloc("xm3_%d" % i, [D]) for i in range(2)]
        y12 = [A.alloc("y12_%d" % i, [D]) for i in range(2)]
        pTfs = [A.alloc("pTf%d" % i, [2, 128]) for i in range(2)]
        pTbs = [A.alloc("pTb%d" % i, [2, 128], BF16) for i in range(2)]
        x2bs = [A.alloc("x2b%d" % i, [D], BF16) for i in range(2)]
        x2Ts = [A.alloc("x2T%d" % i, [16, 128], BF16) for i in range(2)]
        plers = [A.alloc("pler%d" % i, [D]) for i in range(2)]
        egs = [A.alloc("eg%d" % i, [512]) for i in range(2)]
        junk3 = A.alloc("junk3", [D], BF16)
        s3s = [A.alloc("s3_%d" % i, [8]) for i in range(2)]
        for c4 in range(4):
            S.dma("pool", (lambda c4: lambda g: g.dma_start(out=wpg[:, 4 * c4:4 * c4 + 4, :],
                                                           in_=wpg_d.ap()[c4 * 512:(c4 + 1) * 512, :].rearrange("(c p) n -> p c n", p=128)))(c4), writes=[wpg])
        S.dma("pool", lambda g: g.dma_start(out=wple[:], in_=wple_d.ap().rearrange("(c p) n -> p c n", p=128)), writes=[wple])
        S.dma("sp", lambda g: g.dma_start(out=gple[:], in_=gple_d.ap().partition_broadcast(128)), writes=[gple])
        S.dma("sp", lambda g: g.dma_start(out=gfin[:], in_=gfin_d.ap().partition_broadcast(128)), writes=[gfin])

        def p3_load(tile):
            xm3, pTf = xm3s[tile % 2], pTfs[tile % 2]
            r0 = tile * 128
            S.dma("sp", (lambda r0, xm3: lambda g: g.dma_start(out=xm3[:], in_=xmid_d.ap()[r0:r0 + 128, :]))(r0, xm3), writes=[xm3])
            S.dma("sp", (lambda r0, pTf: lambda g: g.dma_start(out=pTf[:], in_=pT_d.ap()[:, r0:r0 + 128].rearrange("(c p) t -> p c t", p=128)))(r0, pTf),
                  writes=[pTf])

        def p3_gather(tile):
            for k in range(2):
                S.dma("pool", (lambda k, tile: lambda g: g.indirect_dma_start(
                    out=y12[k][:], out_offset=None, in_=ys_d.ap(),
                    in_offset=bass.IndirectOffsetOnAxis(ap=dest_i[:, k, tile:tile + 1], axis=0)))(k, tile), reads=[dest_i], writes=[y12[k]])

        p3_load(0)
        p3_gather(0)
        for tile in range(NT):
            r0 = tile * 128
            xm3, pTf, pTb, s3 = xm3s[tile % 2], pTfs[tile % 2], pTbs[tile % 2], s3s[tile % 2]
            x2b, x2T, pler = x2bs[tile % 2], x2Ts[tile % 2], plers[tile % 2]
            if tile + 1 < NT:
                p3_load(tile + 1)
            S.op("act", (lambda pTb, pTf: lambda g: g.copy(out=pTb[:], in_=pTf[:]))(pTb, pTf), reads=[pTf], writes=[pTb])
            S.op("dve", (lambda tile, xm3: lambda g: g.scalar_tensor_tensor(out=xm3[:], in0=y12[0][:], scalar=w12_all[:, 0, tile:tile + 1], in1=xm3[:],
                                                                           op0=ALU.mult, op1=ALU.add))(tile, xm3), reads=[y12[0], w12_all, xm3], writes=[xm3])
            S.op("dve", (lambda tile, xm3: lambda g: g.scalar_tensor_tensor(out=xm3[:], in0=y12[1][:], scalar=w12_all[:, 1, tile:tile + 1], in1=xm3[:],
                                                                           op0=ALU.mult, op1=ALU.add))(tile, xm3), reads=[y12[1], w12_all, xm3], writes=[xm3])
            if tile + 1 < NT:
                p3_gather(tile + 1)
            S.op("act", (lambda xm3, x2b: lambda g: g.copy(out=x2b[:], in_=xm3[:]))(xm3, x2b), reads=[xm3], writes=[x2b])
            for hh in range(2):
                bkb, bk = (B6b, banks[6]) if hh == 0 else (B5b, banks[5])
                for cc in range(8):
                    c = hh * 8 + cc
                    S.op("pe", (lambda c, cc, bkb, x2b: lambda g: g.transpose(out=bkb[:, cc * 128:(cc + 1) * 128], in_=x2b[:, c * 128:(c + 1) * 128],
                                                                      identity=ident_b[:]))(c, cc, bkb, x2b), reads=[x2b, ident_b], writes=[bk])
                if hh == 0:
                    S.op("act", (lambda bkb, x2T: lambda g: g.copy(out=x2T[:, 0:8, :], in_=bkb.rearrange("p (a b) -> p a b", a=8)))(bkb, x2T), reads=[bk], writes=[x2T])
                else:
                    S.op("dve", (lambda bkb, x2T: lambda g: g.tensor_copy(out=x2T[:, 8:16, :], in_=bkb.rearrange("p (a b) -> p a b", a=8)))(bkb, x2T), reads=[bk], writes=[x2T])
            for n in range(4):
                bk = banks[n % 2]
                for kc in range(2):
                    S.op("pe", (lambda kc, n, bk, pTb: lambda g: g.matmul(bk[:, :], lhsT=pTb[:, kc, :], rhs=wple[:, kc, n * 512:(n + 1) * 512],
                                                                          start=(kc == 0), stop=(kc == 1)))(kc, n, bk, pTb), reads=[pTb, wple], writes=[bk])
                S.op("act", (lambda n, bk, pler: lambda g: g.copy(out=pler[:, n * 512:(n + 1) * 512], in_=bk[:, :]))(n, bk, pler), reads=[bk], writes=[pler])
            S.op("act", (lambda s3, pler: lambda g: g.activation(out=junk3[:], in_=pler[:], func=AF.Square, accum_out=s3[:, 0:1]))(s3, pler), reads=[pler], writes=[junk3, s3])
            S.op("act", (lambda s3: lambda g: g.activation(out=s3[:, 1:2], in_=s3[:, 0:1], func=AF.Ln, scale=1.0 / D, bias=EPS))(s3), reads=[s3], writes=[s3])
            S.op("act", (lambda s3: lambda g: g.activation(out=s3[:, 2:3], in_=s3[:, 1:2], func=AF.Exp, scale=-0.5))(s3), reads=[s3], writes=[s3])
            S.op("dve", (lambda s3, pler: lambda g: g.scalar_tensor_tensor(out=pler[:], in0=pler[:], scalar=s3[:, 2:3], in1=gple[:], op0=ALU.mult, op1=ALU.mult))(s3, pler),
                 reads=[pler, s3, gple], writes=[pler])
            for n in range(4):
                bk = banks[2 + n % 2]
                eg = egs[n % 2]
                for c in range(16):
                    S.op("pe", (lambda c, n, bk, x2T: lambda g: g.matmul(bk[:, :], lhsT=x2T[:, c, :], rhs=wpg[:, c, n * 512:(n + 1) * 512],
                                                                    start=(c == 0), stop=(c == 15)))(c, n, bk, x2T), reads=[x2T, wpg], writes=[bk])
                S.op("act", (lambda bk, eg: lambda g: g.activation(out=eg[:], in_=bk[:, :], func=AF.Exp, scale=-1.0))(bk, eg), reads=[bk], writes=[eg])
                S.op("act", (lambda eg: lambda g: g.activation(out=eg[:], in_=eg[:], func=AF.Ln, bias=1.0))(eg), reads=[eg], writes=[eg])
                S.op("act", (lambda eg: lambda g: g.activation(out=eg[:], in_=eg[:], func=AF.Exp, scale=-1.0))(eg), reads=[eg], writes=[eg])
                S.op("dve", (lambda n, eg, pler: lambda g: g.tensor_tensor(out=eg[:], in0=eg[:], in1=pler[:, n * 512:(n + 1) * 512], op=ALU.mult))(n, eg, pler),
                     reads=[eg, pler], writes=[eg])
                S.op("dve", (lambda n, xm3, eg: lambda g: g.tensor_tensor(out=xm3[:, n * 512:(n + 1) * 512], in0=eg[:], in1=xm3[:, n * 512:(n + 1) * 512], op=ALU.add))(n, xm3, eg),
                     reads=[eg, xm3], writes=[xm3])
            S.op("act", (lambda s3, xm3: lambda g: g.activation(out=junk3[:], in_=xm3[:], func=AF.Square, accum_out=s3[:, 4:5]))(s3, xm3), reads=[xm3], writes=[junk3, s3])
            S.op("act", (lambda s3: lambda g: g.activation(out=s3[:, 5:6], in_=s3[:, 4:5], func=AF.Ln, scale=1.0 / D, bias=EPS))(s3), reads=[s3], writes=[s3])
            S.op("act", (lambda s3: lambda g: g.activation(out=s3[:, 6:7], in_=s3[:, 5:6], func=AF.Exp, scale=-0.5))(s3), reads=[s3], writes=[s3])
            S.op("dve", (lambda s3, xm3: lambda g: g.scalar_tensor_tensor(out=xm3[:], in0=xm3[:], scalar=s3[:, 6:7], in1=gfin[:], op0=ALU.mult, op1=ALU.mult))(s3, xm3),
                 reads=[xm3, s3, gfin], writes=[xm3])
            S.dma("act", (lambda r0, xm3: lambda g: g.dma_start(out=out_d.ap()[r0:r0 + 128, :], in_=xm3[:]))(r0, xm3), reads=[xm3])
        S.emit()
    return nc


def prep_weights(g_mix, w_in, lb_logits, hg_norm, conv_w, sc_norm, w_out, g_ffn, w_router_group, w_router_expert,
                 w_gate, w_up, w_down, w_ple, g_ple, w_ple_gate, g_final):
    f = np.float32
    w_in = np.asarray(w_in[0], f)
    q, fz, iz, gz = (w_in[:, k * 1024:(k + 1) * 1024].reshape(D, 8, 128) for k in range(4))
    whg = np.ascontiguousarray(np.stack([q, gz, fz, iz], axis=2).transpose(1, 0, 2, 3).reshape(8, D, 512))
    Bw, Cw, Hw = (w_in[:, 4096 + k * 1024:4096 + (k + 1) * 1024].reshape(D, 8, 128) for k in range(3))
    wsc = np.ascontiguousarray(np.stack([Bw, Cw, Hw], axis=2).transpose(1, 0, 2, 3).reshape(8, D, 384))
    wr = np.concatenate([np.asarray(w_router_group[0], f), np.asarray(w_router_expert[0], f)], axis=1)
    wr = np.ascontiguousarray(wr.reshape(16, 128, 36).transpose(1, 0, 2))
    wg = np.asarray(w_gate[0], f).reshape(NEXP, 16, 128, 512).transpose(0, 2, 1, 3)
    wgt = np.ascontiguousarray(wg).reshape(NEXP * 128 * 4, 2048)
    wu = np.asarray(w_up[0], f).reshape(NEXP, 16, 128, 512).transpose(0, 2, 1, 3)
    wut = np.ascontiguousarray(wu).reshape(NEXP * 128 * 4, 2048)
    wd = np.asarray(w_down[0], f).reshape(NEXP, 4, 128, 2048).transpose(0, 2, 1, 3)
    wdt = np.ascontiguousarray(wd).reshape(NEXP * 128 * 4, 2048)
    cols = np.zeros((128, 64), f)
    cols[:, 0:16] = np.asarray(g_mix[0], f).reshape(16, 128).T
    cols[:, 16:32] = np.asarray(g_ffn[0], f).reshape(16, 128).T
    cols[:, 32:40] = np.asarray(sc_norm[0], f).reshape(8, 128).T
    cw = np.asarray(conv_w[0], f).reshape(3, 8, 128)
    cols[:, 40:64] = cw.transpose(2, 1, 0).reshape(128, 24)
    return {
        "whg": whg, "wsc": wsc, "wout": np.ascontiguousarray(np.asarray(w_out[0], f)), "wr": wr,
        "wgt": wgt, "wut": wut, "wdt": wdt,
        "wple": np.ascontiguousarray(np.asarray(w_ple[0], f)), "wpg": np.ascontiguousarray(np.asarray(w_ple_gate[0], f)),
        "lb": np.ascontiguousarray(np.asarray(lb_logits, f)), "hgn": np.asarray(hg_norm, f).reshape(1, 128),
        "gffn": np.asarray(g_ffn, f).reshape(1, D), "gple": np.asarray(g_ple, f).reshape(1, D),
        "gfin": np.asarray(g_final, f).reshape(1, D), "cols": cols,
        "zeros": np.zeros((256, D), ml_dtypes.bfloat16),
    }


_NC_CACHE = {}


def kernel(x, p, g_mix, w_in, lb_logits, hg_norm, conv_w, sc_norm, w_out, g_ffn, w_router_group, w_router_expert,
           w_gate, w_up, w_down, w_ple, g_ple, w_ple_gate, g_final):
    x = np.asarray(x, np.float32)
    p = np.asarray(p, np.float32)
    Bn, T, _ = x.shape
    half = T // 2
    wts = prep_weights(g_mix, w_in, lb_logits, hg_norm, conv_w, sc_norm, w_out, g_ffn, w_router_group, w_router_expert,
                       w_gate, w_up, w_down, w_ple, g_ple, w_ple_gate, g_final)
    if "nc" not in _NC_CACHE:
        _NC_CACHE["nc"] = build(NG=half // 512, NWG=half // 512)
    nc = _NC_CACHE["nc"]
    in_maps = []
    for c in range(8):
        b, hf = c // 2, c % 2
        rows = slice(hf * half, (hf + 1) * half)
        m = dict(wts)
        m["xT"] = np.ascontiguousarray(x[b, rows].T)
        m["xTp"] = np.ascontiguousarray(x[b, 0:half].T) if hf == 1 else np.zeros((D, half), np.float32)
        m["xtok"] = np.ascontiguousarray(x[b, rows])
        m["pT"] = np.ascontiguousarray(p[0, b, rows].T)
        in_maps.append(m)
    res = run_bass_kernel_spmd(nc, in_maps, core_ids=list(range(8)))
    out = np.empty((Bn, T, D), np.float32)
    for c in range(8):
        b, hf = c // 2, c % 2
        out[b, hf * half:(hf + 1) * half] = res.results[c]["out"]
    return out
```

```python
import numpy as np
import ml_dtypes
from contextlib import ExitStack
import concourse.bass as bass
import concourse.mybir as mybir
from concourse.bass_utils import run_bass_kernel_spmd

F32 = mybir.dt.float32
BF16 = mybir.dt.bfloat16
I32 = mybir.dt.int32
AF = mybir.ActivationFunctionType
ALU = mybir.AluOpType
AX = mybir.AxisListType

D = 2048
EPS = 1e-6
EPS_HG = 1e-6 * 128.0
NEXP = 32


class Buf:
    __slots__ = ("name", "t", "last_w", "readers", "aliases", "off", "excl")

    def __init__(self, name, t):
        self.name = name
        self.t = t
        self.last_w = None
        self.readers = {}
        self.aliases = []
        self.off = None
        self.excl = False

    def __getitem__(self, idx):
        return self.t[idx]


class Instr:
    __slots__ = ("eng", "fn", "deps", "is_dma", "sem", "val", "signal", "prewait")

    def __init__(self, eng, fn, deps, is_dma):
        self.eng = eng
        self.fn = fn
        self.deps = deps
        self.is_dma = is_dma
        self.sem = None
        self.val = None
        self.signal = False
        self.prewait = None


class Sched:
    ENGS = ("pe", "act", "dve", "pool", "sp")

    def __init__(self, nc, es, n_dma_sems=8):
        self.nc = nc
        self.streams = {e: [] for e in self.ENGS}
        self.esem = {e: es.enter_context(nc.semaphore("s_" + e)) for e in self.ENGS}
        self.dsems = {}
        for q in ("sp", "pool", "act"):
            self.dsems[q] = [es.enter_context(nc.semaphore("d_%s%d" % (q, i))) for i in range(n_dma_sems)]
        self.dcount = {q: 0 for q in self.dsems}
        self.duse = {q: [0] * n_dma_sems for q in self.dsems}
        self.dlast = {q: [None] * n_dma_sems for q in self.dsems}
        self.n_instr = 0

    @staticmethod
    def _expand(bufs):
        out = {}
        for b in bufs:
            out[id(b)] = b
            for a in b.aliases:
                out[id(a)] = a
        return list(out.values())

    def _deps(self, eng, reads, writes, is_dma):
        deps = {}
        for b in reads:
            if b.last_w is not None:
                deps[id(b.last_w)] = b.last_w
        for b in writes:
            if b.last_w is not None:
                deps[id(b.last_w)] = b.last_w
            for r in b.readers.values():
                deps[id(r)] = r
        out = []
        for d in deps.values():
            if (not is_dma) and (not d.is_dma) and d.eng == "pe" and eng == "pe":
                continue
            if not d.is_dma:
                d.signal = True
            out.append(d)
        return out

    def _commit(self, ins, reads, writes):
        wset = set(id(b) for b in writes)
        for b in reads:
            if id(b) in wset:
                continue
            key = ("dma", id(ins)) if ins.is_dma else ins.eng
            b.readers[key] = ins
        for b in writes:
            b.last_w = ins
            b.readers = {}
        self.n_instr += 1

    def op(self, eng, fn, reads=(), writes=()):
        reads = self._expand(reads)
        writes = self._expand(writes)
        ex = [b for b in reads if b.excl]
        if ex:
            writes = writes + [b for b in ex if all(b is not w for w in writes)]
        ins = Instr(eng, fn, self._deps(eng, reads, writes, False), False)
        self.streams[eng].append(ins)
        self._commit(ins, reads, writes)
        return ins

    def dma(self, q, fn, reads=(), writes=()):
        reads = self._expand(reads)
        writes = self._expand(writes)
        ins = Instr(q, fn, self._deps(q, reads, writes, True), True)
        n = self.dcount[q]
        k = n % len(self.dsems[q])
        self.dcount[q] += 1
        ins.prewait = self.dlast[q][k]
        self.duse[q][k] += 1
        ins.sem = self.dsems[q][k]
        ins.val = 16 * self.duse[q][k]
        self.dlast[q][k] = ins
        self.streams[q].append(ins)
        self._commit(ins, reads, writes)
        return ins

    def barrier(self):
        lasts = []
        for e in self.ENGS:
            for ins in reversed(self.streams[e]):
                if not ins.is_dma:
                    ins.signal = True
                    lasts.append(ins)
                    break
        for q in self.dsems:
            for last in self.dlast[q]:
                if last is not None:
                    lasts.append(last)
        for e in self.ENGS:
            ins = Instr(e, lambda h: h.nop(), list(lasts), False)
            self.streams[e].append(ins)

    def emit(self):
        nc = self.nc
        for e in self.ENGS:
            c = 0
            for ins in self.streams[e]:
                if not ins.is_dma and ins.signal:
                    c += 1
                    ins.sem = self.esem[e]
                    ins.val = c
        with nc.Block() as block:
            def run(e, h):
                waited = {}

                def wait(d):
                    k = id(d.sem)
                    if waited.get(k, 0) >= d.val:
                        return
                    waited[k] = d.val
                    h.wait_ge(d.sem, d.val)

                for ins in self.streams[e]:
                    if ins.is_dma and ins.prewait is not None:
                        wait(ins.prewait)
                    for d in ins.deps:
                        wait(d)
                    r = ins.fn(h)
                    if ins.is_dma:
                        r.then_inc(ins.sem, 16)
                    elif ins.signal:
                        r.then_inc(ins.sem, 1)
                if e in self.dsems:
                    for last in self.dlast[e]:
                        if last is not None:
                            wait(last)

            @block.tensor
            def _(h):
                run("pe", h)

            @block.scalar
            def _(h):
                run("act", h)

            @block.vector
            def _(h):
                run("dve", h)

            @block.gpsimd
            def _(h):
                run("pool", h)

            @block.sync
            def _(h):
                run("sp", h)


class Arena:
    def __init__(self, t, words):
        self.t = t
        self.words = words
        self.off = 0
        self.n = 0

    def alloc(self, name, shape, dt=F32, at=None):
        n = 1
        for s in shape:
            n *= s
        esz = 4 if dt in (F32, I32) else 2
        w = (n * esz + 3) // 4
        w = (w + 7) // 8 * 8
        off = self.off if at is None else at
        assert off + w <= self.words, "arena overflow at %s: %d + %d > %d" % (name, off, w, self.words)
        ap = self.t[:, off:off + w]
        if dt != F32:
            ap = ap.bitcast(dt)
        ap = ap[:, 0:n]
        if len(shape) == 2:
            ap = ap.rearrange("p (a b) -> p a b", a=shape[0])
        elif len(shape) == 3:
            ap = ap.rearrange("p (a b c) -> p a b c", a=shape[0], b=shape[1])
        if at is None:
            self.off += w
        self.n += 1
        b = Buf(name, ap)
        b.off = off
        return b


def build(NG=8, NWG=8, debug=False, stop=99):
    nc = bass.Bass("TRN2", target_bir_lowering=False)
    TOK = NG * 512
    WTOK = NWG * 512
    NT = TOK // 128
    NB = -(-(2 * TOK + NEXP * 255) // 256)
    PR = NB * 256
    okind = "ExternalOutput" if debug else "Internal"

    def din(name, shape, dt=F32):
        return nc.dram_tensor(name, shape, dt, kind="ExternalInput")

    xT_d = din("xT", [D, TOK])
    xTp_d = din("xTp", [D, WTOK])
    xtok_d = din("xtok", [TOK, D])
    pT_d = din("pT", [256, TOK])
    whg_d = din("whg", [8, D, 512])
    wsc_d = din("wsc", [8, D, 384])
    wout_d = din("wout", [D, D])
    wr_d = din("wr", [128, 16, 36])
    wgt_d = din("wgt", [NEXP * 128 * 4, 2048])
    wut_d = din("wut", [NEXP * 128 * 4, 2048])
    wdt_d = din("wdt", [NEXP * 128 * 4, 2048])
    wple_d = din("wple", [256, D])
    wpg_d = din("wpg", [D, D])
    lb_d = din("lb", [2, 1024])
    hgn_d = din("hgn", [1, 128])
    gffn_d = din("gffn", [1, D])
    gple_d = din("gple", [1, D])
    gfin_d = din("gfin", [1, D])
    cols_d = din("cols", [128, 64])
    zeros_d = din("zeros", [256, D], BF16)
    out_d = nc.dram_tensor("out", [TOK, D], F32, kind="ExternalOutput")
    xmid_d = nc.dram_tensor("xmid", [TOK, D], F32, kind=okind)
    hn_d = nc.dram_tensor("hn", [TOK, D], BF16, kind="Internal")
    xs_d = nc.dram_tensor("xs", [PR, D], BF16, kind="Internal")
    ys_d = nc.dram_tensor("ys", [PR, D], F32, kind="Internal")
    if debug:
        dbg_d = nc.dram_tensor("dbg", [128, 8, NT], F32, kind="ExternalOutput")
        dbgb_d = nc.dram_tensor("dbgb", [128, NB], F32, kind="ExternalOutput")

    with ExitStack() as es:
        S = Sched(nc, es)
        AW = 52480
        arena_t = es.enter_context(nc.sbuf_tensor("arena", [128, AW], F32))
        A = Arena(arena_t, AW)
        banks = [Buf("bank%d" % i, es.enter_context(nc.psum_tensor("bank%d" % i, [128, 512], F32))) for i in range(8)]
        for bk_ in banks:
            bk_.excl = True
        B6b = banks[6].t[:, :].bitcast(BF16)
        B5b = banks[5].t[:, :].bitcast(BF16)

        ident_f = A.alloc("ident_f", [128])
        ident_b = A.alloc("ident_b", [128], BF16)
        tri2 = A.alloc("tri2", [128])
        stri = A.alloc("stri", [128])
        ones_f = A.alloc("ones_f", [128])
        ones_b = A.alloc("ones_b", [128], BF16)
        w12_all = A.alloc("w12_all", [2, NT])
        dest_f = A.alloc("dest_f", [2, NT])
        dest_i = A.alloc("dest_i", [2, NT], I32)
        idxw = A.alloc("idxw", [4, NB], I32)
        p4 = A.alloc("p4", [4])
        true_persist_mark = A.off
        ind2 = A.alloc("ind2", [2])
        oml = A.alloc("oml", [1024])
        hgn = A.alloc("hgn", [128])
        gffn = A.alloc("gffn", [D])
        cols = A.alloc("cols", [64])
        wr = A.alloc("wr", [16, 36])
        Sst = [A.alloc("S%d" % h, [128]) for h in range(8)]
        Sbf = [A.alloc("Sb%d" % h, [128], BF16) for h in range(8)]
        tails = A.alloc("tails", [8, 2])
        oh1_all = A.alloc("oh1_all", [NT, 32])
        oh2_all = A.alloc("oh2_all", [NT, 32])
        rank_all = A.alloc("rank_all", [NT, 32])
        Csum = A.alloc("Csum", [32])
        gmix_c = lambda c: cols[:, c:c + 1]
        gffn_c = lambda c: cols[:, 16 + c:17 + c]
        scn_c = lambda j: cols[:, 32 + j:33 + j]
        cw_c = lambda j, k: cols[:, 40 + 3 * j + k:41 + 3 * j + k]

        def memset(eng, buf, ap, val):
            S.op(eng, lambda g: g.memset(ap, val), writes=[buf])

        memset("pool", ident_f, ident_f[:], 1.0)
        S.op("pool", lambda g: g.affine_select(out=ident_f[:], in_=ident_f[:], pattern=[[-1, 128]], compare_op=ALU.is_equal,
                                                fill=0.0, base=0, channel_multiplier=1), reads=[ident_f], writes=[ident_f])
        S.op("dve", lambda g: g.tensor_copy(out=ident_b[:], in_=ident_f[:]), reads=[ident_f], writes=[ident_b])
        memset("pool", tri2, tri2[:], 1.0)
        S.op("pool", lambda g: g.affine_select(out=tri2[:], in_=tri2[:], pattern=[[1, 128]], compare_op=ALU.is_ge,
                                                fill=0.0, base=0, channel_multiplier=-1), reads=[tri2], writes=[tri2])
        memset("pool", tri2, tri2[0:64, 64:128], 0.0)
        memset("pool", stri, stri[:], 1.0)
        S.op("pool", lambda g: g.affine_select(out=stri[:], in_=stri[:], pattern=[[1, 128]], compare_op=ALU.is_ge,
                                                fill=0.0, base=-1, channel_multiplier=-1), reads=[stri], writes=[stri])
        memset("pool", ones_f, ones_f[:], 1.0)
        memset("pool", ones_b, ones_b[:], 1.0)
        memset("pool", ind2, ind2[:], 0.0)
        memset("pool", ind2, ind2[0:64, 0:1], 1.0)
        memset("pool", ind2, ind2[64:128, 1:2], 1.0)
        memset("pool", tails, tails[:], 0.0)
        memset("pool", Csum, Csum[:], 0.0)
        for h in range(8):
            memset("pool", Sst[h], Sst[h][:], 0.0)
            memset("pool", Sbf[h], Sbf[h][:], 0.0)
        S.op("pool", lambda g: g.iota(p4[:], pattern=[[1, 4]], base=0, channel_multiplier=4,
                                      allow_small_or_imprecise_dtypes=True), writes=[p4])
        tmp_lb = A.alloc("tmp_lb", [2, 1024])
        S.dma("sp", lambda g: g.dma_start(out=tmp_lb[:, 0, :], in_=lb_d.ap()[0:1, :].partition_broadcast(128)), writes=[tmp_lb])
        S.dma("sp", lambda g: g.dma_start(out=tmp_lb[:, 1, :], in_=lb_d.ap()[1:2, :].partition_broadcast(128)), writes=[tmp_lb])
        S.dma("sp", lambda g: g.dma_start(out=hgn[:], in_=hgn_d.ap().partition_broadcast(128)), writes=[hgn])
        S.dma("sp", lambda g: g.dma_start(out=gffn[:], in_=gffn_d.ap().partition_broadcast(128)), writes=[gffn])
        S.dma("sp", lambda g: g.dma_start(out=cols[:], in_=cols_d.ap()), writes=[cols])
        S.dma("sp", lambda g: g.dma_start(out=wr[:], in_=wr_d.ap()), writes=[wr])
        S.op("dve", lambda g: g.tensor_tensor(out=oml[:], in0=tmp_lb[:, 0, :], in1=tmp_lb[:, 1, :], op=ALU.subtract),
             reads=[tmp_lb], writes=[oml])
        S.op("act", lambda g: g.activation(out=oml[:], in_=oml[:], func=AF.Exp), reads=[oml], writes=[oml])
        S.op("dve", lambda g: g.tensor_scalar(out=oml[:], in0=oml[:], scalar1=1.0, scalar2=None, op0=ALU.add), reads=[oml], writes=[oml])
        S.op("dve", lambda g: g.reciprocal(out=oml[:], in_=oml[:]), reads=[oml], writes=[oml])
        for c in range(16):
            S.op("pool", (lambda c: lambda g: g.tensor_scalar(out=wr[:, c, :], in0=wr[:, c, :], scalar1=gffn_c(c), scalar2=None,
                                                              op0=ALU.mult))(c), reads=[wr, cols], writes=[wr])
        A.off -= 2048 + 0
        S.barrier()
        persist_mark = A.off

        wslab = [A.alloc("wslab%d" % i, [16, 512], BF16) for i in range(2)]
        xb = A.alloc("xb", [16, 512], BF16)
        xp = [A.alloc("xp%d" % i, [2, 512]) for i in range(4)]
        xmid_off = None
        sq = [A.alloc("sq%d" % i, [2, 512], BF16) for i in range(2)]
        omixT = A.alloc("omixT", [16, 512], BF16)
        byb = A.alloc("byb", [8, 512], BF16)
        ubuf = A.alloc("ubuf", [514])
        t1 = A.alloc("t1", [512])
        yv = A.alloc("yv", [512])
        yv2 = A.alloc("yv2", [512])
        sqy = A.alloc("sqy", [512], BF16)
        rstd_bc = A.alloc("rstd_bc", [512])
        rstd2_bc = A.alloc("rstd2_bc", [512])
        rstd_sc = A.alloc("rstd_sc", [512])
        lnbc = A.alloc("lnbc", [512])
        rcol = A.alloc("rcol", [8])
        xmid = [A.alloc("xmid0", [D], at=xb.off), A.alloc("xmid1", [D], at=xb.off + 2048),
                A.alloc("xmid2", [D], at=xp[0].off), A.alloc("xmid3", [D], at=xp[2].off)]
        assert xp[1].off == xp[0].off + 1024 and xp[3].off == xp[2].off + 1024
        for xm_, al_ in ((xmid[0], [xb]), (xmid[1], [xb]), (xmid[2], [xp[0], xp[1]]), (xmid[3], [xp[2], xp[3]])):
            xm_.aliases = list(al_)
            for a_ in al_:
                a_.aliases.append(xm_)
        xmT = A.alloc("xmT", [16, 128])
        hnb = A.alloc("hnb", [D], BF16)
        junk = A.alloc("junk", [D], BF16)
        etmp = [A.alloc("etmp%d" % t, [256]) for t in range(2)]
        qg = [[A.alloc("qg%d_%d" % (p_, t), [256]) for t in range(4)] for p_ in range(2)]
        kk = [[A.alloc("kk%d_%d" % (p_, t), [128]) for t in range(4)] for p_ in range(2)]
        lf = [[A.alloc("lf%d_%d" % (p_, t), [128]) for t in range(4)] for p_ in range(2)]
        vb = [[A.alloc("vb%d_%d" % (p_, t), [128], BF16) for t in range(4)] for p_ in range(2)]
        NTMP = 4
        eA = [A.alloc("eA%d" % i, [128]) for i in range(NTMP)]
        enA = [A.alloc("enA%d" % i, [128]) for i in range(NTMP)]
        Ecol = [A.alloc("Ecol%d" % i, [2]) for i in range(NTMP)]
        qt = [A.alloc("qt%d" % i, [128], BF16) for i in range(NTMP)]
        kt = [A.alloc("kt%d" % i, [128], BF16) for i in range(NTMP)]
        qtT = [A.alloc("qtT%d" % i, [128], BF16) for i in range(NTMP)]
        ktT = [A.alloc("ktT%d" % i, [128], BF16) for i in range(NTMP)]
        scTm = [A.alloc("scTm%d" % i, [128], BF16) for i in range(NTMP)]
        U0 = [A.alloc("U0%d" % i, [128]) for i in range(NTMP)]
        S1 = [A.alloc("S1%d" % i, [128]) for i in range(NTMP)]
        S1b = [A.alloc("S1b%d" % i, [128], BF16) for i in range(NTMP)]
        gsn = [A.alloc("gsn%d" % i, [128]) for i in range(NTMP)]
        ogb = [A.alloc("ogb%d" % i, [128], BF16) for i in range(NTMP)]
        sm = [A.alloc("sm%d" % i, [8]) for i in range(NTMP)]
        lg = A.alloc("lg", [36])
        rt = A.alloc("rt", [64])
        Ct = A.alloc("Ct", [32])

        import os
        _dbg2 = os.environ.get("K_DBG2", "")
        _dbg3 = os.environ.get("K_DBG3", "")

        def load_x_group(src_d, g0):
            for i in range(8):
                xpi = xp[i % 4]
                sqi = sq[i % 2]
                S.dma("sp", (lambda i, xpi: lambda g: g.dma_start(
                    out=xpi[:], in_=src_d.ap()[2 * i * 128:(2 * i + 2) * 128, g0 * 512:(g0 + 1) * 512]
                    .rearrange("(c p) t -> p c t", p=128)))(i, xpi), writes=[xpi])
                S.op("act", (lambda xpi, sqi: lambda g: g.activation(out=sqi[:], in_=xpi[:], func=AF.Square))(xpi, sqi),
                     reads=[xpi], writes=[sqi])
                for cc in range(2):
                    c = 2 * i + cc
                    S.op("act", (lambda c, cc, xpi: lambda g: g.activation(out=xb[:, c, :], in_=xpi[:, cc, :], func=AF.Copy, scale=gmix_c(c)))(c, cc, xpi),
                         reads=[xpi, cols], writes=[xb])
                    S.op("pe", (lambda c, cc, sqi: lambda g: g.matmul(banks[7][:, :], lhsT=ones_b[:], rhs=sqi[:, cc, :],
                                                                      start=(c == 0), stop=(c == 15)))(c, cc, sqi),
                         reads=[sqi, ones_b], writes=[banks[7]])
            S.op("act", lambda g: g.activation(out=lnbc[:], in_=banks[7][:, :], func=AF.Ln, scale=1.0 / D, bias=EPS),
                 reads=[banks[7]], writes=[lnbc])
            S.op("act", lambda g: g.activation(out=rstd_bc[:], in_=lnbc[:], func=AF.Exp, scale=-0.5), reads=[lnbc], writes=[rstd_bc])
            S.op("act", lambda g: g.activation(out=rstd2_bc[:], in_=lnbc[:], func=AF.Exp, scale=-1.0), reads=[lnbc], writes=[rstd2_bc])
            if _dbg2 == "nok1":
                return
            for t in range(4):
                S.op("pe", (lambda t: lambda g: g.transpose(out=banks[3][:, 384 + 32 * t:384 + 32 * (t + 1)], in_=rstd_bc[0:32, t * 128:(t + 1) * 128],
                                                            identity=ident_f[0:32, 0:32]))(t),
                     reads=[rstd_bc, ident_f], writes=[banks[3]])
            S.op("dve", lambda g: g.tensor_copy(out=rcol[:, 0:4], in_=banks[3][:, 384:512].rearrange("p (a b) -> p a b", a=4)[:, :, 0]),
                 reads=[banks[3]], writes=[rcol])
            S.op("dve", lambda g: g.tensor_scalar(out=rcol[:, 4:8], in0=rcol[:, 0:4], scalar1=-1.0, scalar2=None, op0=ALU.mult),
                 reads=[rcol], writes=[rcol])


        slab_plan = []
        slab_state = {"i": 0, "issued": 0}

        def plan_slabs():
            for wg in range(NWG):
                for h in range(8):
                    slab_plan.append((whg_d.ap()[h, :, 256:512].rearrange("(c p) n -> p c n", p=128), 256))
                if wg == NWG - 1:
                    for j in range(8):
                        slab_plan.append((wsc_d.ap()[j, :, :].rearrange("(c p) n -> p c n", p=128), 384))
            for g0 in range(NG):
                for h in range(8):
                    slab_plan.append((whg_d.ap()[h, :, :].rearrange("(c p) n -> p c n", p=128), 512))
                for j in range(8):
                    slab_plan.append((wsc_d.ap()[j, :, :].rearrange("(c p) n -> p c n", p=128), 384))
                for n in range(4):
                    slab_plan.append((wout_d.ap()[:, n * 512:(n + 1) * 512].rearrange("(c p) n -> p c n", p=128), 512))

        def issue_slab(i):
            src_ap, ncols = slab_plan[i]
            ws = wslab[i % 2]
            S.dma("pool", (lambda ws, src_ap, ncols: lambda g: g.dma_start(out=ws[:, :, 0:ncols], in_=src_ap))(ws, src_ap, ncols), writes=[ws])

        def next_slab(ncols_expected):
            i = slab_state["i"]
            slab_state["i"] += 1
            while slab_state["issued"] <= min(i + 1, len(slab_plan) - 1):
                issue_slab(slab_state["issued"])
                slab_state["issued"] += 1
            assert slab_plan[i][1] == ncols_expected, (i, slab_plan[i][1], ncols_expected)
            return wslab[i % 2]

        tmp_ctr = {"n": 0, "e": 0}

        def gen_inproj(h, full, par):
            c0 = 0 if full else 256
            ncols = 512 if full else 256
            ws = next_slab(ncols)
            oml_h = oml[:, h * 128:(h + 1) * 128]
            fo = 256 - c0
            for t in range(4):
                bk = banks[t % 2]
                for c in range(16):
                    S.op("pe", (lambda c, t, bk: lambda g: g.matmul(bk[:, 0:ncols], lhsT=xb[:, c, t * 128:(t + 1) * 128],
                                                                     rhs=ws[:, c, 0:ncols], start=(c == 0), stop=(c == 15)))(c, t, bk),
                         reads=[xb, ws], writes=[bk])
                    if c % 2 == 1 and c < 15:
                        yield
                rs = rcol[:, t:t + 1]
                nrs = rcol[:, 4 + t:5 + t]
                qg_t, kk_t, vb_t, lf_t = qg[par][t], kk[par][t], vb[par][t], lf[par][t]
                et = etmp[tmp_ctr["e"] % 2]
                tmp_ctr["e"] += 1
                if full:
                    S.op("act", (lambda et, bk, nrs: lambda g: g.activation(out=et[:], in_=bk[:, 0:256], func=AF.Exp, scale=nrs))(et, bk, nrs),
                         reads=[bk, rcol], writes=[et])
                S.op("act", (lambda kk_t, bk, rs: lambda g: g.activation(out=kk_t[:], in_=bk[:, fo:fo + 128], func=AF.Exp, scale=rs))(kk_t, bk, rs),
                     reads=[bk, rcol], writes=[kk_t])
                S.op("dve", (lambda vb_t, bk, rs: lambda g: g.tensor_scalar(out=vb_t[:], in0=bk[:, fo + 128:fo + 256], scalar1=rs, scalar2=None,
                                                                        op0=ALU.mult))(vb_t, bk, rs), reads=[bk, rcol], writes=[vb_t])
                if full:
                    S.op("act", (lambda et: lambda g: g.activation(out=et[:], in_=et[:], func=AF.Ln, bias=1.0))(et), reads=[et], writes=[et])
                    S.op("act", (lambda et: lambda g: g.activation(out=et[:], in_=et[:], func=AF.Exp, scale=-1.0))(et), reads=[et], writes=[et])
                    S.op("dve", (lambda qg_t, bk, rs, et: lambda g: g.scalar_tensor_tensor(out=qg_t[:], in0=bk[:, 0:256], scalar=rs, in1=et[:],
                                                                                       op0=ALU.mult, op1=ALU.mult))(qg_t, bk, rs, et),
                         reads=[bk, rcol, et], writes=[qg_t])
                S.op("act", (lambda kk_t: lambda g: g.activation(out=kk_t[:], in_=kk_t[:], func=AF.Ln, bias=1.0))(kk_t), reads=[kk_t], writes=[kk_t])
                S.op("act", (lambda kk_t: lambda g: g.activation(out=kk_t[:], in_=kk_t[:], func=AF.Exp, scale=-1.0))(kk_t), reads=[kk_t], writes=[kk_t])
                S.op("dve", (lambda kk_t: lambda g: g.tensor_tensor(out=kk_t[:], in0=kk_t[:], in1=oml_h, op=ALU.mult))(kk_t),
                     reads=[kk_t, oml], writes=[kk_t])
                S.op("act", (lambda kk_t, lf_t: lambda g: g.activation(out=lf_t[:], in_=kk_t[:], func=AF.Ln, scale=-1.0, bias=1.0))(kk_t, lf_t),
                     reads=[kk_t], writes=[lf_t])
                yield

        B2b = banks[2].t[:, :].bitcast(BF16)

        def gen_pre(h, t, full, par, i):
            b3 = banks[3] if i % 2 == 0 else banks[7]
            tq = 0 if i % 2 == 0 else 256
            qg_t, kk_t, lf_t = qg[par][t], kk[par][t], lf[par][t]
            S.op("pe", lambda g: g.matmul(b3[:, 0:128], lhsT=tri2[:], rhs=lf_t[:], start=True, stop=True), reads=[tri2, lf_t], writes=[b3])
            S.op("pe", lambda g: g.matmul(b3[:, 128:130], lhsT=lf_t[:], rhs=ind2[:], start=True, stop=True), reads=[lf_t, ind2], writes=[b3])
            yield
            S.op("act", lambda g: g.activation(out=enA[i][:], in_=b3[:, 0:128], func=AF.Exp, scale=-1.0), reads=[b3], writes=[enA[i]])
            if full:
                S.op("act", lambda g: g.activation(out=eA[i][:], in_=b3[:, 0:128], func=AF.Exp), reads=[b3], writes=[eA[i]])
            S.op("act", lambda g: g.activation(out=Ecol[i][:], in_=b3[:, 128:130], func=AF.Exp), reads=[b3], writes=[Ecol[i]])
            S.op("dve", lambda g: g.tensor_tensor(out=kt[i][:], in0=kk_t[:], in1=enA[i][:], op=ALU.mult), reads=[kk_t, enA[i]], writes=[kt[i]])
            if not full:
                return
            S.op("dve", lambda g: g.tensor_tensor(out=qt[i][:], in0=qg_t[:, 0:128], in1=eA[i][:], op=ALU.mult), reads=[qg_t, eA[i]], writes=[qt[i]])
            S.op("pool", lambda g: g.tensor_tensor(out=gsn[i][:], in0=qg_t[:, 128:256], in1=hgn[:], op=ALU.mult), reads=[qg_t, hgn], writes=[gsn[i]])
            S.op("pe", lambda g: g.transpose(out=B6b[:, tq:tq + 128], in_=qt[i][:], identity=ident_b[:]), reads=[qt[i], ident_b], writes=[banks[6]])
            S.op("pe", lambda g: g.transpose(out=B6b[:, tq + 128:tq + 256], in_=kt[i][:], identity=ident_b[:]), reads=[kt[i], ident_b], writes=[banks[6]])
            yield
            S.op("act", lambda g: g.copy(out=qtT[i][:], in_=B6b[:, tq:tq + 128]), reads=[banks[6]], writes=[qtT[i]])
            S.op("act", lambda g: g.copy(out=ktT[i][:], in_=B6b[:, tq + 128:tq + 256]), reads=[banks[6]], writes=[ktT[i]])
            S.op("pe", lambda g: g.matmul(b3[:, 256:384], lhsT=ktT[i][:], rhs=qtT[i][:], start=True, stop=True), reads=[ktT[i], qtT[i]], writes=[b3])
            yield
            S.op("dve", lambda g: g.tensor_tensor(out=scTm[i][:], in0=b3[:, 256:384], in1=tri2[:], op=ALU.mult), reads=[b3, tri2], writes=[scTm[i]])

        def gen_state(h, t, full, par, i):
            Sh, Sb = Sst[h], Sbf[h]
            b4, b5 = banks[4], banks[5]
            vb_t = vb[par][t]
            S.op("dve", lambda g: g.tensor_scalar(out=U0[i][:], in0=Sh[:], scalar1=Ecol[i][:, 0:1], scalar2=None, op0=ALU.mult),
                 reads=[Sh, Ecol[i]], writes=[U0[i]])
            S.op("pe", lambda g: g.matmul(b5[:, 0:128], lhsT=kt[i][0:64, :], rhs=vb_t[0:64, :], start=True, stop=True), reads=[kt[i], vb_t], writes=[b5])
            yield
            S.op("dve", lambda g: g.scalar_tensor_tensor(out=S1[i][:], in0=b5[:, 0:128], scalar=Ecol[i][:, 0:1], in1=U0[i][:], op0=ALU.mult, op1=ALU.add),
                 reads=[b5, Ecol[i], U0[i]], writes=[S1[i]])
            S.op("dve", lambda g: g.tensor_scalar(out=U0[i][:], in0=S1[i][:], scalar1=Ecol[i][:, 1:2], scalar2=None, op0=ALU.mult),
                 reads=[S1[i], Ecol[i]], writes=[U0[i]])
            if full:
                S.op("act", lambda g: g.copy(out=S1b[i][:], in_=S1[i][:]), reads=[S1[i]], writes=[S1b[i]])
                S.op("pe", lambda g: g.matmul(b4[:, 0:128], lhsT=scTm[i][:], rhs=vb_t[:], start=True, stop=False), reads=[scTm[i], vb_t], writes=[b4])
                S.op("pe", lambda g: g.matmul(b4[0:64, 0:128], lhsT=qtT[i][:, 0:64], rhs=Sb[:], start=False, stop=True), reads=[qtT[i], Sb], writes=[b4])
                S.op("pe", lambda g: g.matmul(b4[64:128, 0:128], lhsT=qtT[i][:, 64:128], rhs=S1b[i][:], start=False, stop=True),
                     reads=[qtT[i], S1b[i]], writes=[b4])
            S.op("pe", lambda g: g.matmul(b5[:, 128:256], lhsT=kt[i][64:128, :], rhs=vb_t[64:128, :], start=True, stop=True), reads=[kt[i], vb_t], writes=[b5])
            yield
            S.op("dve", lambda g: g.scalar_tensor_tensor(out=Sh[:], in0=b5[:, 128:256], scalar=Ecol[i][:, 1:2], in1=U0[i][:], op0=ALU.mult, op1=ALU.add),
                 reads=[b5, Ecol[i], U0[i]], writes=[Sh])
            if not full:
                return
            S.op("act", lambda g: g.copy(out=Sb[:], in_=Sh[:]), reads=[Sh], writes=[Sb])
            S.op("act", lambda g: g.activation(out=junk[:, 0:128], in_=b4[:, 0:128], func=AF.Square, accum_out=sm[i][:, 0:1]), reads=[b4], writes=[junk, sm[i]])
            S.op("act", lambda g: g.activation(out=sm[i][:, 1:2], in_=sm[i][:, 0:1], func=AF.Ln, scale=1.0 / 128, bias=EPS_HG), reads=[sm[i]], writes=[sm[i]])
            S.op("act", lambda g: g.activation(out=sm[i][:, 2:3], in_=sm[i][:, 1:2], func=AF.Exp, scale=-0.5), reads=[sm[i]], writes=[sm[i]])
            S.op("dve", lambda g: g.scalar_tensor_tensor(out=ogb[i][:], in0=b4[:, 0:128], scalar=sm[i][:, 2:3], in1=gsn[i][:], op0=ALU.mult, op1=ALU.mult),
                 reads=[b4, sm[i], gsn[i]], writes=[ogb[i]])
            S.op("pe", lambda g: g.transpose(out=B2b[:, 0:128], in_=ogb[i][:], identity=ident_b[:]), reads=[ogb[i], ident_b], writes=[banks[2]])
            yield
            S.op("act", lambda g: g.copy(out=omixT[:, h, t * 128:(t + 1) * 128], in_=B2b[:, 0:128]), reads=[banks[2]], writes=[omixT])

        def exhaust(gen):
            if gen is None:
                return
            for _ in gen:
                pass

        def roundrobin(gens, steps):
            live = [g is not None for g in gens]
            while any(live[:-1]):
                for k, g in enumerate(gens):
                    if not live[k]:
                        continue
                    for _ in range(steps[k]):
                        try:
                            next(g)
                        except StopIteration:
                            live[k] = False
                            break

        def run_heads(full):
            exhaust(gen_inproj(0, full, 0))
            units = [(h, t) for h in range(8) for t in range(4)]
            nxt = 0
            pres = []
            ready = []
            cur_state = None
            n_state_done = 0
            bg = None
            bg_head = -1
            while n_state_done < len(units):
                while len(pres) < 2 and nxt < len(units) and nxt - n_state_done < NTMP - 1:
                    h, t = units[nxt]
                    if t == 0 and bg is not None and bg_head == h:
                        exhaust(bg)
                        bg = None
                    if t == 1 and h + 1 < 8 and bg is None:
                        bg = gen_inproj(h + 1, full, (h + 1) % 2)
                        bg_head = h + 1
                    pres.append((nxt, gen_pre(h, t, full, h % 2, nxt % NTMP)))
                    nxt += 1
                if cur_state is None and ready and ready[0] == n_state_done:
                    u = ready.pop(0)
                    h, t = units[u]
                    cur_state = gen_state(h, t, full, h % 2, u % NTMP)
                progressed = False
                if cur_state is not None:
                    progressed = True
                    try:
                        next(cur_state)
                    except StopIteration:
                        cur_state = None
                        n_state_done += 1
                for item in list(pres):
                    progressed = True
                    try:
                        next(item[1])
                    except StopIteration:
                        pres.remove(item)
                        ready.append(item[0])
                        ready.sort()
                if bg is not None:
                    for _ in range(3):
                        try:
                            next(bg)
                        except StopIteration:
                            bg = None
                            break
                assert progressed or bg is not None or cur_state is not None or ready, "pipeline stalled"
            exhaust(bg)

        sc_ctr = {"n": 0}

        def sc_chunk(j, tail_only=False):
            ws = next_slab(384)
            bset = [banks[0], banks[1], banks[2]] if sc_ctr["n"] % 2 == 0 else [banks[3], banks[4], banks[5]]
            sc_ctr["n"] += 1
            for part in range(3):
                if tail_only and part == 0:
                    continue
                bk = bset[part]
                for c in range(16):
                    S.op("pe", (lambda c, part, bk: lambda g: g.matmul(bk[:, :], lhsT=ws[:, c, part * 128:(part + 1) * 128], rhs=xb[:, c, :],
                                                                        start=(c == 0), stop=(c == 15)))(c, part, bk),
                         reads=[ws, xb], writes=[bk])
            S.op("pool", lambda g: g.tensor_copy(out=ubuf[:, 0:2], in_=tails[:, j, :]), reads=[tails], writes=[ubuf])
            S.op("dve", lambda g: g.tensor_tensor(out=t1[:], in0=bset[1][:, :], in1=rstd2_bc[:], op=ALU.mult),
                 reads=[bset[1], rstd2_bc], writes=[t1])
            S.op("dve", lambda g: g.tensor_tensor(out=ubuf[:, 2:514], in0=t1[:], in1=bset[2][:, :], op=ALU.mult),
                 reads=[bset[2], t1], writes=[ubuf])
            S.op("pool", lambda g: g.tensor_copy(out=tails[:, j, :], in_=ubuf[:, 512:514]), reads=[ubuf], writes=[tails])
            if tail_only:
                return
            S.op("act", lambda g: g.activation(out=yv[:], in_=ubuf[:, 0:512], func=AF.Copy, scale=cw_c(j, 0)), reads=[ubuf, cols], writes=[yv])
            S.op("dve", lambda g: g.scalar_tensor_tensor(out=yv[:], in0=ubuf[:, 1:513], scalar=cw_c(j, 1), in1=yv[:], op0=ALU.mult, op1=ALU.add),
                 reads=[ubuf, cols, yv], writes=[yv])
            S.op("dve", lambda g: g.scalar_tensor_tensor(out=yv[:], in0=ubuf[:, 2:514], scalar=cw_c(j, 2), in1=yv[:], op0=ALU.mult, op1=ALU.add),
                 reads=[ubuf, cols, yv], writes=[yv])
            S.op("dve", lambda g: g.tensor_tensor(out=yv[:], in0=yv[:], in1=rstd_bc[:], op=ALU.mult), reads=[yv, rstd_bc], writes=[yv])
            S.op("dve", lambda g: g.tensor_tensor(out=byb[:, j, :], in0=bset[0][:, :], in1=yv[:], op=ALU.mult),
                 reads=[bset[0], yv], writes=[byb])
            S.op("act", lambda g: g.activation(out=sqy[:], in_=byb[:, j, :], func=AF.Square), reads=[byb], writes=[sqy])
            S.op("pe", lambda g: g.matmul(banks[7][:, :], lhsT=ones_b[:], rhs=sqy[:], start=(j == 0), stop=(j == 7)),
                 reads=[ones_b, sqy], writes=[banks[7]])

        def sc_finish():
            S.op("act", lambda g: g.activation(out=lnbc[:], in_=banks[7][:, :], func=AF.Ln, scale=1.0 / 1024, bias=EPS),
                 reads=[banks[7]], writes=[lnbc])
            S.op("act", lambda g: g.activation(out=rstd_sc[:], in_=lnbc[:], func=AF.Exp, scale=-0.5), reads=[lnbc], writes=[rstd_sc])
            for j in range(8):
                S.op("dve", (lambda j: lambda g: g.scalar_tensor_tensor(out=omixT[:, 8 + j, :], in0=byb[:, j, :], scalar=scn_c(j), in1=rstd_sc[:],
                                                                        op0=ALU.mult, op1=ALU.mult))(j),
                     reads=[byb, cols, rstd_sc], writes=[omixT])

        def outproj_and_route(g0):
            for t in range(4):
                r0 = g0 * 512 + t * 128
                S.dma("sp", (lambda t, r0: lambda g: g.dma_start(out=xmid[t][:], in_=xtok_d.ap()[r0:r0 + 128, :]))(t, r0), writes=[xmid[t]])
            k = 0
            for n in range(4):
                ws = next_slab(512)
                for t in range(4):
                    bk = banks[k % 3]
                    k += 1
                    for c in range(16):
                        S.op("pe", (lambda c, t, bk, ws: lambda g: g.matmul(bk[:, :], lhsT=omixT[:, c, t * 128:(t + 1) * 128], rhs=ws[:, c, :],
                                                                            start=(c == 0), stop=(c == 15)))(c, t, bk, ws),
                             reads=[omixT, ws], writes=[bk])
                    S.op("dve", (lambda t, n, bk: lambda g: g.tensor_tensor(out=xmid[t][:, n * 512:(n + 1) * 512], in0=bk[:, :],
                                                                           in1=xmid[t][:, n * 512:(n + 1) * 512], op=ALU.add))(t, n, bk),
                         reads=[bk, xmid[t]], writes=[xmid[t]])
            for t in range(4):
                tile = g0 * 4 + t
                r0 = tile * 128
                xm = xmid[t]
                S.dma("act", (lambda xm, r0: lambda g: g.dma_start(out=xmid_d.ap()[r0:r0 + 128, :], in_=xm[:]))(xm, r0), reads=[xm])
                S.op("act", (lambda xm: lambda g: g.activation(out=junk[:], in_=xm[:], func=AF.Square, accum_out=rt[:, 0:1]))(xm),
                     reads=[xm], writes=[junk, rt])
                S.op("act", lambda g: g.activation(out=rt[:, 1:2], in_=rt[:, 0:1], func=AF.Ln, scale=1.0 / D, bias=EPS), reads=[rt], writes=[rt])
                S.op("act", lambda g: g.activation(out=rt[:, 2:3], in_=rt[:, 1:2], func=AF.Exp, scale=-0.5), reads=[rt], writes=[rt])
                S.op("dve", (lambda xm: lambda g: g.scalar_tensor_tensor(out=hnb[:], in0=xm[:], scalar=rt[:, 2:3], in1=gffn[:],
                                                                        op0=ALU.mult, op1=ALU.mult))(xm), reads=[xm, rt, gffn], writes=[hnb])
                S.dma("act", (lambda r0: lambda g: g.dma_start(out=hn_d.ap()[r0:r0 + 128, :], in_=hnb[:]))(r0), reads=[hnb])
                for q4 in range(4):
                    bk = banks[q4 % 3]
                    for cc in range(4):
                        c = q4 * 4 + cc
                        S.op("pe", (lambda c, cc, bk, xm: lambda g: g.transpose(out=bk[:, cc * 128:(cc + 1) * 128], in_=xm[:, c * 128:(c + 1) * 128],
                                                                              identity=ident_f[:]))(c, cc, bk, xm),
                             reads=[xm, ident_f], writes=[bk])
                    eng = "act" if q4 % 2 == 0 else "dve"
                    if eng == "act":
                        S.op("act", (lambda q4, bk: lambda g: g.copy(out=xmT[:, q4 * 4:(q4 + 1) * 4, :], in_=bk[:, :].rearrange("p (a b) -> p a b", a=4)))(q4, bk),
                             reads=[bk], writes=[xmT])
                    else:
                        S.op("dve", (lambda q4, bk: lambda g: g.tensor_copy(out=xmT[:, q4 * 4:(q4 + 1) * 4, :], in_=bk[:, :].rearrange("p (a b) -> p a b", a=4)))(q4, bk),
                             reads=[bk], writes=[xmT])
                for c in range(16):
                    S.op("pe", (lambda c: lambda g: g.matmul(banks[7][:, 0:36], lhsT=xmT[:, c, :], rhs=wr[:, c, :], start=(c == 0), stop=(c == 15)))(c),
                         reads=[xmT, wr], writes=[banks[7]])
                S.op("dve", lambda g: g.tensor_scalar(out=lg[:], in0=banks[7][:, 0:36], scalar1=rt[:, 2:3], scalar2=None, op0=ALU.mult),
                     reads=[banks[7], rt], writes=[lg])
                route_tile(tile)

        def route_tile(tile):
            P = "dve"
            R = [lg, rt]
            S.op("dve", lambda g: g.tensor_reduce(out=rt[:, 3:4], in_=lg[:, 0:4], axis=AX.X, op=ALU.max), reads=R, writes=[rt])
            S.op("dve", lambda g: g.tensor_scalar(out=rt[:, 12:16], in0=lg[:, 0:4], scalar1=rt[:, 3:4], scalar2=None, op0=ALU.is_equal), reads=R, writes=[rt])
            S.op("dve", lambda g: g.tensor_scalar(out=rt[:, 48:52], in0=lg[:, 0:4], scalar1=rt[:, 3:4], scalar2=None, op0=ALU.subtract), reads=R, writes=[rt])
            S.op("act", lambda g: g.activation(out=rt[:, 48:52], in_=rt[:, 48:52], func=AF.Exp, accum_out=rt[:, 4:5]), reads=R, writes=[rt])
            S.op("dve", lambda g: g.reciprocal(out=rt[:, 5:6], in_=rt[:, 4:5]), reads=R, writes=[rt])
            S.op("dve", lambda g: g.tensor_scalar(out=rt[:, 16:24], in0=lg[:, 4:12], scalar1=rt[:, 12:13], scalar2=None, op0=ALU.mult), reads=R, writes=[rt])
            for gi in range(1, 4):
                S.op("dve", (lambda gi: lambda g: g.scalar_tensor_tensor(out=rt[:, 16:24], in0=lg[:, 4 + 8 * gi:12 + 8 * gi], scalar=rt[:, 12 + gi:13 + gi],
                                                                         in1=rt[:, 16:24], op0=ALU.mult, op1=ALU.add))(gi), reads=R, writes=[rt])
            S.op("dve", lambda g: g.tensor_reduce(out=rt[:, 6:7], in_=rt[:, 16:24], axis=AX.X, op=ALU.max), reads=R, writes=[rt])
            S.op("dve", lambda g: g.tensor_scalar(out=rt[:, 24:32], in0=rt[:, 16:24], scalar1=rt[:, 6:7], scalar2=None, op0=ALU.is_equal), reads=R, writes=[rt])
            S.op("dve", lambda g: g.scalar_tensor_tensor(out=rt[:, 32:40], in0=rt[:, 24:32], scalar=-1e30, in1=rt[:, 16:24], op0=ALU.mult, op1=ALU.add),
                 reads=R, writes=[rt])
            S.op("dve", lambda g: g.tensor_reduce(out=rt[:, 7:8], in_=rt[:, 32:40], axis=AX.X, op=ALU.max), reads=R, writes=[rt])
            S.op("dve", lambda g: g.tensor_scalar(out=rt[:, 40:48], in0=rt[:, 32:40], scalar1=rt[:, 7:8], scalar2=None, op0=ALU.is_equal), reads=R, writes=[rt])
            S.op("dve", lambda g: g.tensor_tensor(out=rt[:, 8:9], in0=rt[:, 7:8], in1=rt[:, 6:7], op=ALU.subtract), reads=R, writes=[rt])
            S.op("act", lambda g: g.activation(out=rt[:, 8:9], in_=rt[:, 8:9], func=AF.Exp), reads=R, writes=[rt])
            S.op("dve", lambda g: g.tensor_scalar(out=rt[:, 8:9], in0=rt[:, 8:9], scalar1=1.0, scalar2=None, op0=ALU.add), reads=R, writes=[rt])
            S.op("dve", lambda g: g.reciprocal(out=rt[:, 8:9], in_=rt[:, 8:9]), reads=R, writes=[rt])
            S.op("dve", lambda g: g.tensor_tensor(out=w12_all[:, 0, tile:tile + 1], in0=rt[:, 8:9], in1=rt[:, 5:6], op=ALU.mult), reads=R, writes=[w12_all])
            S.op("dve", lambda g: g.tensor_tensor(out=w12_all[:, 1, tile:tile + 1], in0=rt[:, 5:6], in1=w12_all[:, 0, tile:tile + 1], op=ALU.subtract),
                 reads=R + [w12_all], writes=[w12_all])
            for gi in range(4):
                S.op(P, (lambda gi: lambda g: g.tensor_scalar(out=oh1_all[:, tile, gi * 8:(gi + 1) * 8], in0=rt[:, 24:32], scalar1=rt[:, 12 + gi:13 + gi],
                                                               scalar2=None, op0=ALU.mult))(gi), reads=R, writes=[oh1_all])
                S.op(P, (lambda gi: lambda g: g.tensor_scalar(out=oh2_all[:, tile, gi * 8:(gi + 1) * 8], in0=rt[:, 40:48], scalar1=rt[:, 12 + gi:13 + gi],
                                                               scalar2=None, op0=ALU.mult))(gi), reads=R, writes=[oh2_all])
            S.op(P, lambda g: g.tensor_tensor(out=Ct[:], in0=oh1_all[:, tile, :], in1=oh2_all[:, tile, :], op=ALU.add), reads=[oh1_all, oh2_all], writes=[Ct])
            S.op("pe", lambda g: g.matmul(banks[3][:, 400:432], lhsT=stri[:], rhs=Ct[:], start=True, stop=False), reads=[stri, Ct], writes=[banks[3]])
            S.op("pe", lambda g: g.matmul(banks[3][:, 400:432], lhsT=ones_f[:], rhs=Csum[:], start=False, stop=True), reads=[ones_f, Csum], writes=[banks[3]])
            S.op("dve", lambda g: g.tensor_copy(out=rank_all[:, tile, :], in_=banks[3][:, 400:432]), reads=[banks[3]], writes=[rank_all])
            S.op(P, lambda g: g.tensor_tensor(out=Csum[:], in0=Csum[:], in1=Ct[:], op=ALU.add), reads=[Csum, Ct], writes=[Csum])

        if stop <= 0:
            S.emit()
            return nc
        import os
        _dbg = os.environ.get("K_DBG", "")
        plan_slabs()
        for wg in range(NWG):
            load_x_group(xTp_d, wg)
            if _dbg == "lx":
                S.emit()
                return nc
            run_heads(False)
            if _dbg == "h8":
                S.emit()
                return nc
            if wg == NWG - 1:
                for j in range(8):
                    sc_chunk(j, tail_only=True)
        if stop <= 1:
            S.emit()
            return nc
        for h in range(8):
            S.op("act", (lambda h: lambda g: g.copy(out=Sbf[h][:], in_=Sst[h][:]))(h), reads=[Sst[h]], writes=[Sbf[h]])
        for zb in range(NB):
            S.dma("act", (lambda zb: lambda g: g.dma_start(out=xs_d.ap()[zb * 256:(zb + 1) * 256, :], in_=zeros_d.ap()))(zb))
        for g0 in range(NG):
            load_x_group(xT_d, g0)
            run_heads(True)
            for j in range(8):
                sc_chunk(j)
            sc_finish()
            outproj_and_route(g0)

        if stop <= 2:
            S.emit()
            return nc
        S.barrier()
        A.off = persist_mark
        fin = A.alloc("fin", [512])
        fin_i = A.alloc("fin_i", [128], I32)
        b3 = banks[3]
        S.op("pe", lambda g: g.matmul(b3[0:32, 0:128], lhsT=Csum[:], rhs=ones_f[:], start=True, stop=True), reads=[Csum, ones_f], writes=[b3])
        S.op("dve", lambda g: g.tensor_scalar(out=fin[0:32, 0:128], in0=b3[0:32, 0:128], scalar1=255.0, scalar2=None, op0=ALU.add), reads=[b3], writes=[fin])
        S.op("dve", lambda g: g.tensor_copy(out=fin_i[0:32, :], in_=fin[0:32, 0:128]), reads=[fin], writes=[fin_i])
        S.op("dve", lambda g: g.tensor_scalar(out=fin_i[0:32, :], in0=fin_i[0:32, :], scalar1=8, scalar2=8, op0=ALU.arith_shift_right,
                                              op1=ALU.logical_shift_left), reads=[fin_i], writes=[fin_i])
        S.op("dve", lambda g: g.tensor_copy(out=fin[0:32, 0:128], in_=fin_i[0:32, :]), reads=[fin_i], writes=[fin])
        S.op("pe", lambda g: g.matmul(b3[:, 128:160], lhsT=fin[0:32, 0:128], rhs=tri2[0:32, 0:32], start=True, stop=True), reads=[fin, tri2], writes=[b3])
        S.op("pe", lambda g: g.matmul(b3[:, 160:192], lhsT=fin[0:32, 0:128], rhs=stri[0:32, 0:32], start=True, stop=True), reads=[fin, stri], writes=[b3])
        S.op("dve", lambda g: g.tensor_copy(out=fin[:, 128:192], in_=b3[:, 128:192]), reads=[b3], writes=[fin])
        pend = fin[:, 128:160]
        pstart = fin[:, 160:192]
        tmp32 = fin[:, 192:224]
        for tile in range(NT):
            for k, oh in ((0, oh1_all), (1, oh2_all)):
                S.op("dve", (lambda tile: lambda g: g.tensor_tensor(out=tmp32, in0=rank_all[:, tile, :], in1=pstart, op=ALU.add))(tile),
                     reads=[rank_all, fin], writes=[fin])
                S.op("dve", (lambda tile, oh: lambda g: g.tensor_tensor(out=tmp32, in0=tmp32, in1=oh[:, tile, :], op=ALU.mult))(tile, oh),
                     reads=[oh, fin], writes=[fin])
                S.op("dve", (lambda tile, k: lambda g: g.tensor_reduce(out=dest_f[:, k, tile:tile + 1], in_=tmp32, axis=AX.X, op=ALU.add))(tile, k),
                     reads=[fin], writes=[dest_f])
        S.op("dve", lambda g: g.tensor_copy(out=dest_i[:], in_=dest_f[:]), reads=[dest_f], writes=[dest_i])
        bthr = A.alloc("bthr", [NB])
        bacc = A.alloc("bacc", [NB])
        idxf = A.alloc("idxf", [4, NB])
        S.op("pool", lambda g: g.iota(bthr[:], pattern=[[256, NB]], base=0, channel_multiplier=0, allow_small_or_imprecise_dtypes=True), writes=[bthr])
        memset("pool", bacc, bacc[:], 0.0)
        for e in range(NEXP):
            S.op("dve", (lambda e: lambda g: g.scalar_tensor_tensor(out=bacc[:], in0=bthr[:], scalar=fin[:, 128 + e:129 + e], in1=bacc[:],
                                                                   op0=ALU.is_ge, op1=ALU.add))(e), reads=[bthr, fin, bacc], writes=[bacc])
        S.op("dve", lambda g: g.tensor_scalar(out=bacc[:], in0=bacc[:], scalar1=float(NEXP - 1), scalar2=None, op0=ALU.min), reads=[bacc], writes=[bacc])
        for cc in range(4):
            S.op("dve", (lambda cc: lambda g: g.tensor_scalar(out=idxf[:, cc, :], in0=bacc[:], scalar1=512.0, scalar2=p4[:, cc:cc + 1],
                                                             op0=ALU.mult, op1=ALU.add))(cc), reads=[bacc, p4], writes=[idxf])
        S.op("dve", lambda g: g.tensor_copy(out=idxw[:], in_=idxf[:]), reads=[idxf], writes=[idxw])
        if debug:
            dbgt = A.alloc("dbgt", [8, NT])
            S.op("dve", lambda g: g.tensor_copy(out=dbgt[:, 0:2, :], in_=dest_f[:]), reads=[dest_f], writes=[dbgt])
            S.op("dve", lambda g: g.tensor_copy(out=dbgt[:, 2:4, :], in_=w12_all[:]), reads=[w12_all], writes=[dbgt])
            S.op("dve", lambda g: g.memset(dbgt[:, 4:8, :], 0.0), writes=[dbgt])
            S.dma("sp", lambda g: g.dma_start(out=dbg_d.ap(), in_=dbgt[:]), reads=[dbgt])
            S.dma("sp", lambda g: g.dma_start(out=dbgb_d.ap(), in_=bacc[:]), reads=[bacc])
        S.barrier()

        if stop <= 3:
            S.emit()
            return nc
        A.off = true_persist_mark
        hnt = [A.alloc("hnt%d" % i, [D], BF16) for i in range(2)]
        for tile in range(NT):
            ht = hnt[tile % 2]
            r0 = tile * 128
            S.dma("sp", (lambda ht, r0: lambda g: g.dma_start(out=ht[:], in_=hn_d.ap()[r0:r0 + 128, :]))(ht, r0), writes=[ht])
            for k in range(2):
                S.dma("pool", (lambda ht, k, tile: lambda g: g.indirect_dma_start(
                    out=xs_d.ap(), out_offset=bass.IndirectOffsetOnAxis(ap=dest_i[:, k, tile:tile + 1], axis=0),
                    in_=ht[:], in_offset=None))(ht, k, tile), reads=[ht, dest_i])
        S.barrier()
        if stop <= 4:
            S.emit()
            return nc
        A.off = true_persist_mark
        wgb = [A.alloc("wgb%d" % i, [16, 512], BF16) for i in range(2)]
        wub = [A.alloc("wub%d" % i, [16, 512], BF16) for i in range(2)]
        wdb = [A.alloc("wdb%d" % i, [4, 2048], BF16) for i in range(2)]
        xt = [A.alloc("xt%d" % i, [D], BF16) for i in range(2)]
        xsT2 = [A.alloc("xsT", [16, 256], BF16)]
        actT2 = [A.alloc("actT%d" % i, [4, 256], BF16) for i in range(2)]
        ee = A.alloc("ee", [256])
        ga = A.alloc("ga", [256])
        yrow = [A.alloc("yrow0", [D])]
        yrow.append(yrow[0])
        NSTG = min(8, (A.words - A.off) // 2048)
        assert NSTG >= 3, NSTG
        stage = [A.alloc("stage%d" % i, [2048]) for i in range(NSTG)]
        stg = {"n": 0}
        cast_engs = ["act", "dve"]

        assert NSTG == 8, NSTG

        def w_piece(b, i):
            k = b % 2
            if i < 4:
                return wgt_d, wgb[k], i, True
            if i < 8:
                return wut_d, wub[k], i - 4, True
            return wdt_d, wdb[k], i - 8, False

        def gather_w(b, i):
            tab, dst, cc, gu = w_piece(b, i)
            st = stage[i % NSTG]
            S.dma("pool", (lambda st, tab, cc, b: lambda g: g.indirect_dma_start(
                out=st[:], out_offset=None, in_=tab.ap(),
                in_offset=bass.IndirectOffsetOnAxis(ap=idxw[:, cc, b:b + 1], axis=0)))(st, tab, cc, b), reads=[idxw], writes=[st])

        def cast_w(b, i):
            tab, dst, cc, gu = w_piece(b, i)
            st = stage[i % NSTG]
            if gu:
                dview = dst[:, 4 * cc:4 * cc + 4, :]
                sview = st[:].rearrange("p (a b) -> p a b", a=4)
            else:
                dview = dst[:, cc, :]
                sview = st[:]
            if i % 2 == 0:
                S.op("act", (lambda dview, sview: lambda g: g.copy(out=dview, in_=sview))(dview, sview), reads=[st], writes=[dst])
            else:
                S.op("dve", (lambda dview, sview: lambda g: g.tensor_copy(out=dview, in_=sview))(dview, sview), reads=[st], writes=[dst])
            if i + NSTG < 12:
                gather_w(b, i + NSTG)

        for i in range(NSTG):
            gather_w(0, i)
        for i in range(12):
            cast_w(0, i)
        for b in range(NB):
            k = b % 2
            xsT = xsT2[b % len(xsT2)]
            actT = actT2[b % 2]
            if b + 1 < NB:
                for i in range(NSTG):
                    gather_w(b + 1, i)
            for r in range(2):
                r0 = b * 256 + r * 128
                S.dma("sp", (lambda r, r0: lambda g: g.dma_start(out=xt[r][:], in_=xs_d.ap()[r0:r0 + 128, :]))(r, r0), writes=[xt[r]])
                for hh in range(2):
                    bkb, bk = (B6b, banks[6]) if hh == 0 else (B5b, banks[5])
                    for cc in range(8):
                        c = hh * 8 + cc
                        S.op("pe", (lambda c, cc, r, bkb: lambda g: g.transpose(out=bkb[:, cc * 128:(cc + 1) * 128], in_=xt[r][:, c * 128:(c + 1) * 128],
                                                                             identity=ident_b[:]))(c, cc, r, bkb), reads=[xt[r], ident_b], writes=[bk])
                    if hh == 0:
                        S.op("act", (lambda r, bkb, xsT: lambda g: g.copy(out=xsT[:, 0:8, r * 128:(r + 1) * 128], in_=bkb.rearrange("p (a b) -> p a b", a=8)))(r, bkb, xsT),
                             reads=[bk], writes=[xsT])
                    else:
                        S.op("dve", (lambda r, bkb, xsT: lambda g: g.tensor_copy(out=xsT[:, 8:16, r * 128:(r + 1) * 128], in_=bkb.rearrange("p (a b) -> p a b", a=8)))(r, bkb, xsT),
                             reads=[bk], writes=[xsT])
            for fc in range(4):
                bg, bu = banks[0 + 2 * (fc % 2)], banks[1 + 2 * (fc % 2)]
                for c in range(16):
                    S.op("pe", (lambda c, fc, bg, k, xsT: lambda g: g.matmul(bg[:, 0:256], lhsT=wgb[k][:, c, fc * 128:(fc + 1) * 128], rhs=xsT[:, c, :],
                                                                     start=(c == 0), stop=(c == 15)))(c, fc, bg, k, xsT), reads=[wgb[k], xsT], writes=[bg])
                for c in range(16):
                    S.op("pe", (lambda c, fc, bu, k, xsT: lambda g: g.matmul(bu[:, 0:256], lhsT=wub[k][:, c, fc * 128:(fc + 1) * 128], rhs=xsT[:, c, :],
                                                                     start=(c == 0), stop=(c == 15)))(c, fc, bu, k, xsT), reads=[wub[k], xsT], writes=[bu])
                S.op("act", (lambda bg: lambda g: g.activation(out=ee[:], in_=bg[:, 0:256], func=AF.Exp, scale=-1.0))(bg), reads=[bg], writes=[ee])
                S.op("act", lambda g: g.activation(out=ee[:], in_=ee[:], func=AF.Ln, bias=1.0), reads=[ee], writes=[ee])
                S.op("act", lambda g: g.activation(out=ee[:], in_=ee[:], func=AF.Exp, scale=-1.0), reads=[ee], writes=[ee])
                S.op("dve", (lambda bg: lambda g: g.tensor_tensor(out=ga[:], in0=bg[:, 0:256], in1=ee[:], op=ALU.mult))(bg), reads=[bg, ee], writes=[ga])
                S.op("dve", (lambda bu, fc, actT: lambda g: g.tensor_tensor(out=actT[:, fc, :], in0=bu[:, 0:256], in1=ga[:], op=ALU.mult))(bu, fc, actT),
                     reads=[bu, ga], writes=[actT])
                if b + 1 < NB:
                    for i in (3 * fc, 3 * fc + 1, 3 * fc + 2):
                        cast_w(b + 1, i)
            kk2 = 0
            for r in range(2):
                for n in range(4):
                    bk = banks[4 + (kk2 % 2) * 3]
                    kk2 += 1
                    for fc in range(4):
                        S.op("pe", (lambda fc, r, n, bk, k, actT: lambda g: g.matmul(bk[:, :], lhsT=actT[:, fc, r * 128:(r + 1) * 128],
                                                                            rhs=wdb[k][:, fc, n * 512:(n + 1) * 512], start=(fc == 0), stop=(fc == 3)))(fc, r, n, bk, k, actT),
                             reads=[actT, wdb[k]], writes=[bk])
                    S.op("act", (lambda r, n, bk: lambda g: g.copy(out=yrow[r][:, n * 512:(n + 1) * 512], in_=bk[:, :]))(r, n, bk), reads=[bk], writes=[yrow[r]])
                r0 = b * 256 + r * 128
                S.dma("act", (lambda r, r0: lambda g: g.dma_start(out=ys_d.ap()[r0:r0 + 128, :], in_=yrow[r][:]))(r, r0), reads=[yrow[r]])
        S.barrier()

        if stop <= 5:
            S.emit()
            return nc
        A.off = true_persist_mark
        wpg = A.alloc("wpg", [16, D], BF16)
        wple = A.alloc("wple", [2, D], BF16)
        gple = A.alloc("gple", [D])
        gfin = A.alloc("gfin", [D])
        xm3s = [A.alloc("xm3_%d" % i, [D]) for i in range(2)]
        y12 = [A.alloc("y12_%d" % i, [D]) for i in range(2)]
        pTfs = [A.alloc("pTf%d" % i, [2, 128]) for i in range(2)]
        pTbs = [A.alloc("pTb%d" % i, [2, 128], BF16) for i in range(2)]
        x2bs = [A.alloc("x2b%d" % i, [D], BF16) for i in range(2)]
        x2Ts = [A.alloc("x2T%d" % i, [16, 128], BF16) for i in range(2)]
        plers = [A.alloc("pler%d" % i, [D]) for i in range(2)]
        egs = [A.alloc("eg%d" % i, [512]) for i in range(2)]
        junk3 = A.alloc("junk3", [D], BF16)
        s3s = [A.alloc("s3_%d" % i, [8]) for i in range(2)]
        for c4 in range(4):
            S.dma("pool", (lambda c4: lambda g: g.dma_start(out=wpg[:, 4 * c4:4 * c4 + 4, :],
                                                           in_=wpg_d.ap()[c4 * 512:(c4 + 1) * 512, :].rearrange("(c p) n -> p c n", p=128)))(c4), writes=[wpg])
        S.dma("pool", lambda g: g.dma_start(out=wple[:], in_=wple_d.ap().rearrange("(c p) n -> p c n", p=128)), writes=[wple])
        S.dma("sp", lambda g: g.dma_start(out=gple[:], in_=gple_d.ap().partition_broadcast(128)), writes=[gple])
        S.dma("sp", lambda g: g.dma_start(out=gfin[:], in_=gfin_d.ap().partition_broadcast(128)), writes=[gfin])

        def p3_load(tile):
            xm3, pTf = xm3s[tile % 2], pTfs[tile % 2]
            r0 = tile * 128
            S.dma("sp", (lambda r0, xm3: lambda g: g.dma_start(out=xm3[:], in_=xmid_d.ap()[r0:r0 + 128, :]))(r0, xm3), writes=[xm3])
            S.dma("sp", (lambda r0, pTf: lambda g: g.dma_start(out=pTf[:], in_=pT_d.ap()[:, r0:r0 + 128].rearrange("(c p) t -> p c t", p=128)))(r0, pTf),
                  writes=[pTf])

        def p3_gather(tile):
            for k in range(2):
                S.dma("pool", (lambda k, tile: lambda g: g.indirect_dma_start(
                    out=y12[k][:], out_offset=None, in_=ys_d.ap(),
                    in_offset=bass.IndirectOffsetOnAxis(ap=dest_i[:, k, tile:tile + 1], axis=0)))(k, tile), reads=[dest_i], writes=[y12[k]])

        p3_load(0)
        p3_gather(0)
        for tile in range(NT):
            r0 = tile * 128
            xm3, pTf, pTb, s3 = xm3s[tile % 2], pTfs[tile % 2], pTbs[tile % 2], s3s[tile % 2]
            x2b, x2T, pler = x2bs[tile % 2], x2Ts[tile % 2], plers[tile % 2]
            if tile + 1 < NT:
                p3_load(tile + 1)
            S.op("act", (lambda pTb, pTf: lambda g: g.copy(out=pTb[:], in_=pTf[:]))(pTb, pTf), reads=[pTf], writes=[pTb])
            S.op("dve", (lambda tile, xm3: lambda g: g.scalar_tensor_tensor(out=xm3[:], in0=y12[0][:], scalar=w12_all[:, 0, tile:tile + 1], in1=xm3[:],
                                                                           op0=ALU.mult, op1=ALU.add))(tile, xm3), reads=[y12[0], w12_all, xm3], writes=[xm3])
            S.op("dve", (lambda tile, xm3: lambda g: g.scalar_tensor_tensor(out=xm3[:], in0=y12[1][:], scalar=w12_all[:, 1, tile:tile + 1], in1=xm3[:],
                                                                           op0=ALU.mult, op1=ALU.add))(tile, xm3), reads=[y12[1], w12_all, xm3], writes=[xm3])
            if tile + 1 < NT:
                p3_gather(tile + 1)
            S.op("act", (lambda xm3, x2b: lambda g: g.copy(out=x2b[:], in_=xm3[:]))(xm3, x2b), reads=[xm3], writes=[x2b])
            for hh in range(2):
                bkb, bk = (B6b, banks[6]) if hh == 0 else (B5b, banks[5])
                for cc in range(8):
                    c = hh * 8 + cc
                    S.op("pe", (lambda c, cc, bkb, x2b: lambda g: g.transpose(out=bkb[:, cc * 128:(cc + 1) * 128], in_=x2b[:, c * 128:(c + 1) * 128],
                                                                      identity=ident_b[:]))(c, cc, bkb, x2b), reads=[x2b, ident_b], writes=[bk])
                if hh == 0:
                    S.op("act", (lambda bkb, x2T: lambda g: g.copy(out=x2T[:, 0:8, :], in_=bkb.rearrange("p (a b) -> p a b", a=8)))(bkb, x2T), reads=[bk], writes=[x2T])
                else:
                    S.op("dve", (lambda bkb, x2T: lambda g: g.tensor_copy(out=x2T[:, 8:16, :], in_=bkb.rearrange("p (a b) -> p a b", a=8)))(bkb, x2T), reads=[bk], writes=[x2T])
            for n in range(4):
                bk = banks[n % 2]
                for kc in range(2):
                    S.op("pe", (lambda kc, n, bk, pTb: lambda g: g.matmul(bk[:, :], lhsT=pTb[:, kc, :], rhs=wple[:, kc, n * 512:(n + 1) * 512],
                                                                          start=(kc == 0), stop=(kc == 1)))(kc, n, bk, pTb), reads=[pTb, wple], writes=[bk])
                S.op("act", (lambda n, bk, pler: lambda g: g.copy(out=pler[:, n * 512:(n + 1) * 512], in_=bk[:, :]))(n, bk, pler), reads=[bk], writes=[pler])
            S.op("act", (lambda s3, pler: lambda g: g.activation(out=junk3[:], in_=pler[:], func=AF.Square, accum_out=s3[:, 0:1]))(s3, pler), reads=[pler], writes=[junk3, s3])
            S.op("act", (lambda s3: lambda g: g.activation(out=s3[:, 1:2], in_=s3[:, 0:1], func=AF.Ln, scale=1.0 / D, bias=EPS))(s3), reads=[s3], writes=[s3])
            S.op("act", (lambda s3: lambda g: g.activation(out=s3[:, 2:3], in_=s3[:, 1:2], func=AF.Exp, scale=-0.5))(s3), reads=[s3], writes=[s3])
            S.op("dve", (lambda s3, pler: lambda g: g.scalar_tensor_tensor(out=pler[:], in0=pler[:], scalar=s3[:, 2:3], in1=gple[:], op0=ALU.mult, op1=ALU.mult))(s3, pler),
                 reads=[pler, s3, gple], writes=[pler])
            for n in range(4):
                bk = banks[2 + n % 2]
                eg = egs[n % 2]
                for c in range(16):
                    S.op("pe", (lambda c, n, bk, x2T: lambda g: g.matmul(bk[:, :], lhsT=x2T[:, c, :], rhs=wpg[:, c, n * 512:(n + 1) * 512],
                                                                    start=(c == 0), stop=(c == 15)))(c, n, bk, x2T), reads=[x2T, wpg], writes=[bk])
                S.op("act", (lambda bk, eg: lambda g: g.activation(out=eg[:], in_=bk[:, :], func=AF.Exp, scale=-1.0))(bk, eg), reads=[bk], writes=[eg])
                S.op("act", (lambda eg: lambda g: g.activation(out=eg[:], in_=eg[:], func=AF.Ln, bias=1.0))(eg), reads=[eg], writes=[eg])
                S.op("act", (lambda eg: lambda g: g.activation(out=eg[:], in_=eg[:], func=AF.Exp, scale=-1.0))(eg), reads=[eg], writes=[eg])
                S.op("dve", (lambda n, eg, pler: lambda g: g.tensor_tensor(out=eg[:], in0=eg[:], in1=pler[:, n * 512:(n + 1) * 512], op=ALU.mult))(n, eg, pler),
                     reads=[eg, pler], writes=[eg])
                S.op("dve", (lambda n, xm3, eg: lambda g: g.tensor_tensor(out=xm3[:, n * 512:(n + 1) * 512], in0=eg[:], in1=xm3[:, n * 512:(n + 1) * 512], op=ALU.add))(n, xm3, eg),
                     reads=[eg, xm3], writes=[xm3])
            S.op("act", (lambda s3, xm3: lambda g: g.activation(out=junk3[:], in_=xm3[:], func=AF.Square, accum_out=s3[:, 4:5]))(s3, xm3), reads=[xm3], writes=[junk3, s3])
            S.op("act", (lambda s3: lambda g: g.activation(out=s3[:, 5:6], in_=s3[:, 4:5], func=AF.Ln, scale=1.0 / D, bias=EPS))(s3), reads=[s3], writes=[s3])
            S.op("act", (lambda s3: lambda g: g.activation(out=s3[:, 6:7], in_=s3[:, 5:6], func=AF.Exp, scale=-0.5))(s3), reads=[s3], writes=[s3])
            S.op("dve", (lambda s3, xm3: lambda g: g.scalar_tensor_tensor(out=xm3[:], in0=xm3[:], scalar=s3[:, 6:7], in1=gfin[:], op0=ALU.mult, op1=ALU.mult))(s3, xm3),
                 reads=[xm3, s3, gfin], writes=[xm3])
            S.dma("act", (lambda r0, xm3: lambda g: g.dma_start(out=out_d.ap()[r0:r0 + 128, :], in_=xm3[:]))(r0, xm3), reads=[xm3])
        S.emit()
    return nc


def prep_weights(g_mix, w_in, lb_logits, hg_norm, conv_w, sc_norm, w_out, g_ffn, w_router_group, w_router_expert,
                 w_gate, w_up, w_down, w_ple, g_ple, w_ple_gate, g_final):
    f = np.float32
    w_in = np.asarray(w_in[0], f)
    q, fz, iz, gz = (w_in[:, k * 1024:(k + 1) * 1024].reshape(D, 8, 128) for k in range(4))
    whg = np.ascontiguousarray(np.stack([q, gz, fz, iz], axis=2).transpose(1, 0, 2, 3).reshape(8, D, 512))
    Bw, Cw, Hw = (w_in[:, 4096 + k * 1024:4096 + (k + 1) * 1024].reshape(D, 8, 128) for k in range(3))
    wsc = np.ascontiguousarray(np.stack([Bw, Cw, Hw], axis=2).transpose(1, 0, 2, 3).reshape(8, D, 384))
    wr = np.concatenate([np.asarray(w_router_group[0], f), np.asarray(w_router_expert[0], f)], axis=1)
    wr = np.ascontiguousarray(wr.reshape(16, 128, 36).transpose(1, 0, 2))
    wg = np.asarray(w_gate[0], f).reshape(NEXP, 16, 128, 512).transpose(0, 2, 1, 3)
    wgt = np.ascontiguousarray(wg).reshape(NEXP * 128 * 4, 2048)
    wu = np.asarray(w_up[0], f).reshape(NEXP, 16, 128, 512).transpose(0, 2, 1, 3)
    wut = np.ascontiguousarray(wu).reshape(NEXP * 128 * 4, 2048)
    wd = np.asarray(w_down[0], f).reshape(NEXP, 4, 128, 2048).transpose(0, 2, 1, 3)
    wdt = np.ascontiguousarray(wd).reshape(NEXP * 128 * 4, 2048)
    cols = np.zeros((128, 64), f)
    cols[:, 0:16] = np.asarray(g_mix[0], f).reshape(16, 128).T
    cols[:, 16:32] = np.asarray(g_ffn[0], f).reshape(16, 128).T
    cols[:, 32:40] = np.asarray(sc_norm[0], f).reshape(8, 128).T
    cw = np.asarray(conv_w[0], f).reshape(3, 8, 128)
    cols[:, 40:64] = cw.transpose(2, 1, 0).reshape(128, 24)
    return {
        "whg": whg, "wsc": wsc, "wout": np.ascontiguousarray(np.asarray(w_out[0], f)), "wr": wr,
        "wgt": wgt, "wut": wut, "wdt": wdt,
        "wple": np.ascontiguousarray(np.asarray(w_ple[0], f)), "wpg": np.ascontiguousarray(np.asarray(w_ple_gate[0], f)),
        "lb": np.ascontiguousarray(np.asarray(lb_logits, f)), "hgn": np.asarray(hg_norm, f).reshape(1, 128),
        "gffn": np.asarray(g_ffn, f).reshape(1, D), "gple": np.asarray(g_ple, f).reshape(1, D),
        "gfin": np.asarray(g_final, f).reshape(1, D), "cols": cols,
        "zeros": np.zeros((256, D), ml_dtypes.bfloat16),
    }


_NC_CACHE = {}


def kernel(x, p, g_mix, w_in, lb_logits, hg_norm, conv_w, sc_norm, w_out, g_ffn, w_router_group, w_router_expert,
           w_gate, w_up, w_down, w_ple, g_ple, w_ple_gate, g_final):
    x = np.asarray(x, np.float32)
    p = np.asarray(p, np.float32)
    Bn, T, _ = x.shape
    half = T // 2
    wts = prep_weights(g_mix, w_in, lb_logits, hg_norm, conv_w, sc_norm, w_out, g_ffn, w_router_group, w_router_expert,
                       w_gate, w_up, w_down, w_ple, g_ple, w_ple_gate, g_final)
    if "nc" not in _NC_CACHE:
        _NC_CACHE["nc"] = build(NG=half // 512, NWG=half // 512)
    nc = _NC_CACHE["nc"]
    in_maps = []
    for c in range(8):
        b, hf = c // 2, c % 2
        rows = slice(hf * half, (hf + 1) * half)
        m = dict(wts)
        m["xT"] = np.ascontiguousarray(x[b, rows].T)
        m["xTp"] = np.ascontiguousarray(x[b, 0:half].T) if hf == 1 else np.zeros((D, half), np.float32)
        m["xtok"] = np.ascontiguousarray(x[b, rows])
        m["pT"] = np.ascontiguousarray(p[0, b, rows].T)
        in_maps.append(m)
    res = run_bass_kernel_spmd(nc, in_maps, core_ids=list(range(8)))
    out = np.empty((Bn, T, D), np.float32)
    for c in range(8):
        b, hf = c // 2, c % 2
        out[b, hf * half:(hf + 1) * half] = res.results[c]["out"]
    return out
```

```python
import numpy as np
import ml_dtypes
from contextlib import ExitStack
import concourse.bass as bass
import concourse.mybir as mybir
from concourse.bass_utils import run_bass_kernel_spmd

F32 = mybir.dt.float32
BF16 = mybir.dt.bfloat16
I32 = mybir.dt.int32
AF = mybir.ActivationFunctionType
ALU = mybir.AluOpType
AX = mybir.AxisListType

D = 2048
EPS = 1e-6
EPS_HG = 1e-6 * 128.0
NEXP = 32


class Buf:
    __slots__ = ("name", "t", "last_w", "readers", "aliases", "off", "excl")

    def __init__(self, name, t):
        self.name = name
        self.t = t
        self.last_w = None
        self.readers = {}
        self.aliases = []
        self.off = None
        self.excl = False

    def __getitem__(self, idx):
        return self.t[idx]


class Instr:
    __slots__ = ("eng", "fn", "deps", "is_dma", "sem", "val", "signal", "prewait")

    def __init__(self, eng, fn, deps, is_dma):
        self.eng = eng
        self.fn = fn
        self.deps = deps
        self.is_dma = is_dma
        self.sem = None
        self.val = None
        self.signal = False
        self.prewait = None


class Sched:
    ENGS = ("pe", "act", "dve", "pool", "sp")

    def __init__(self, nc, es, n_dma_sems=8):
        self.nc = nc
        self.streams = {e: [] for e in self.ENGS}
        self.esem = {e: es.enter_context(nc.semaphore("s_" + e)) for e in self.ENGS}
        self.dsems = {}
        for q in ("sp", "pool", "act"):
            self.dsems[q] = [es.enter_context(nc.semaphore("d_%s%d" % (q, i))) for i in range(n_dma_sems)]
        self.dcount = {q: 0 for q in self.dsems}
        self.duse = {q: [0] * n_dma_sems for q in self.dsems}
        self.dlast = {q: [None] * n_dma_sems for q in self.dsems}
        self.n_instr = 0

    @staticmethod
    def _expand(bufs):
        out = {}
        for b in bufs:
            out[id(b)] = b
            for a in b.aliases:
                out[id(a)] = a
        return list(out.values())

    def _deps(self, eng, reads, writes, is_dma):
        deps = {}
        for b in reads:
            if b.last_w is not None:
                deps[id(b.last_w)] = b.last_w
        for b in writes:
            if b.last_w is not None:
                deps[id(b.last_w)] = b.last_w
            for r in b.readers.values():
                deps[id(r)] = r
        out = []
        for d in deps.values():
            if (not is_dma) and (not d.is_dma) and d.eng == "pe" and eng == "pe":
                continue
            if not d.is_dma:
                d.signal = True
            out.append(d)
        return out

    def _commit(self, ins, reads, writes):
        wset = set(id(b) for b in writes)
        for b in reads:
            if id(b) in wset:
                continue
            key = ("dma", id(ins)) if ins.is_dma else ins.eng
            b.readers[key] = ins
        for b in writes:
            b.last_w = ins
            b.readers = {}
        self.n_instr += 1

    def op(self, eng, fn, reads=(), writes=()):
        reads = self._expand(reads)
        writes = self._expand(writes)
        ex = [b for b in reads if b.excl]
        if ex:
            writes = writes + [b for b in ex if all(b is not w for w in writes)]
        ins = Instr(eng, fn, self._deps(eng, reads, writes, False), False)
        self.streams[eng].append(ins)
        self._commit(ins, reads, writes)
        return ins

    def dma(self, q, fn, reads=(), writes=()):
        reads = self._expand(reads)
        writes = self._expand(writes)
        ins = Instr(q, fn, self._deps(q, reads, writes, True), True)
        n = self.dcount[q]
        k = n % len(self.dsems[q])
        self.dcount[q] += 1
        ins.prewait = self.dlast[q][k]
        self.duse[q][k] += 1
        ins.sem = self.dsems[q][k]
        ins.val = 16 * self.duse[q][k]
        self.dlast[q][k] = ins
        self.streams[q].append(ins)
        self._commit(ins, reads, writes)
        return ins

    def barrier(self):
        lasts = []
        for e in self.ENGS:
            for ins in reversed(self.streams[e]):
                if not ins.is_dma:
                    ins.signal = True
                    lasts.append(ins)
                    break
        for q in self.dsems:
            for last in self.dlast[q]:
                if last is not None:
                    lasts.append(last)
        for e in self.ENGS:
            ins = Instr(e, lambda h: h.nop(), list(lasts), False)
            self.streams[e].append(ins)

    def emit(self):
        nc = self.nc
        for e in self.ENGS:
            c = 0
            for ins in self.streams[e]:
                if not ins.is_dma and ins.signal:
                    c += 1
                    ins.sem = self.esem[e]
                    ins.val = c
        with nc.Block() as block:
            def run(e, h):
                waited = {}

                def wait(d):
                    k = id(d.sem)
                    if waited.get(k, 0) >= d.val:
                        return
                    waited[k] = d.val
                    h.wait_ge(d.sem, d.val)

                for ins in self.streams[e]:
                    if ins.is_dma and ins.prewait is not None:
                        wait(ins.prewait)
                    for d in ins.deps:
                        wait(d)
                    r = ins.fn(h)
                    if ins.is_dma:
                        r.then_inc(ins.sem, 16)
                    elif ins.signal:
                        r.then_inc(ins.sem, 1)
                if e in self.dsems:
                    for last in self.dlast[e]:
                        if last is not None:
                            wait(last)

            @block.tensor
            def _(h):
                run("pe", h)

            @block.scalar
            def _(h):
                run("act", h)

            @block.vector
            def _(h):
                run("dve", h)

            @block.gpsimd
            def _(h):
                run("pool", h)

            @block.sync
            def _(h):
                run("sp", h)


class Arena:
    def __init__(self, t, words):
        self.t = t
        self.words = words
        self.off = 0
        self.n = 0

    def alloc(self, name, shape, dt=F32, at=None):
        n = 1
        for s in shape:
            n *= s
        esz = 4 if dt in (F32, I32) else 2
        w = (n * esz + 3) // 4
        w = (w + 7) // 8 * 8
        off = self.off if at is None else at
        assert off + w <= self.words, "arena overflow at %s: %d + %d > %d" % (name, off, w, self.words)
        ap = self.t[:, off:off + w]
        if dt != F32:
            ap = ap.bitcast(dt)
        ap = ap[:, 0:n]
        if len(shape) == 2:
            ap = ap.rearrange("p (a b) -> p a b", a=shape[0])
        elif len(shape) == 3:
            ap = ap.rearrange("p (a b c) -> p a b c", a=shape[0], b=shape[1])
        if at is None:
            self.off += w
        self.n += 1
        b = Buf(name, ap)
        b.off = off
        return b


def build(NG=8, NWG=8, debug=False, stop=99):
    nc = bass.Bass("TRN2", target_bir_lowering=False)
    TOK = NG * 512
    WTOK = NWG * 512
    NT = TOK // 128
    NB = -(-(2 * TOK + NEXP * 255) // 256)
    PR = NB * 256
    okind = "ExternalOutput" if debug else "Internal"

    def din(name, shape, dt=F32):
        return nc.dram_tensor(name, shape, dt, kind="ExternalInput")

    xT_d = din("xT", [D, TOK])
    xTp_d = din("xTp", [D, WTOK])
    xtok_d = din("xtok", [TOK, D])
    pT_d = din("pT", [256, TOK])
    whg_d = din("whg", [8, D, 512])
    wsc_d = din("wsc", [8, D, 384])
    wout_d = din("wout", [D, D])
    wr_d = din("wr", [128, 16, 36])
    wgt_d = din("wgt", [NEXP * 128 * 4, 2048])
    wut_d = din("wut", [NEXP * 128 * 4, 2048])
    wdt_d = din("wdt", [NEXP * 128 * 4, 2048])
    wple_d = din("wple", [256, D])
    wpg_d = din("wpg", [D, D])
    lb_d = din("lb", [2, 1024])
    hgn_d = din("hgn", [1, 128])
    gffn_d = din("gffn", [1, D])
    gple_d = din("gple", [1, D])
    gfin_d = din("gfin", [1, D])
    cols_d = din("cols", [128, 64])
    zeros_d = din("zeros", [256, D], BF16)
    out_d = nc.dram_tensor("out", [TOK, D], F32, kind="ExternalOutput")
    xmid_d = nc.dram_tensor("xmid", [TOK, D], F32, kind=okind)
    hn_d = nc.dram_tensor("hn", [TOK, D], BF16, kind="Internal")
    xs_d = nc.dram_tensor("xs", [PR, D], BF16, kind="Internal")
    ys_d = nc.dram_tensor("ys", [PR, D], F32, kind="Internal")
    if debug:
        dbg_d = nc.dram_tensor("dbg", [128, 8, NT], F32, kind="ExternalOutput")
        dbgb_d = nc.dram_tensor("dbgb", [128, NB], F32, kind="ExternalOutput")

    with ExitStack() as es:
        S = Sched(nc, es)
        AW = 52480
        arena_t = es.enter_context(nc.sbuf_tensor("arena", [128, AW], F32))
        A = Arena(arena_t, AW)
        banks = [Buf("bank%d" % i, es.enter_context(nc.psum_tensor("bank%d" % i, [128, 512], F32))) for i in range(8)]
        for bk_ in banks:
            bk_.excl = True
        B6b = banks[6].t[:, :].bitcast(BF16)
        B5b = banks[5].t[:, :].bitcast(BF16)

        ident_f = A.alloc("ident_f", [128])
        ident_b = A.alloc("ident_b", [128], BF16)
        tri2 = A.alloc("tri2", [128])
        stri = A.alloc("stri", [128])
        ones_f = A.alloc("ones_f", [128])
        ones_b = A.alloc("ones_b", [128], BF16)
        w12_all = A.alloc("w12_all", [2, NT])
        dest_f = A.alloc("dest_f", [2, NT])
        dest_i = A.alloc("dest_i", [2, NT], I32)
        idxw = A.alloc("idxw", [4, NB], I32)
        p4 = A.alloc("p4", [4])
        true_persist_mark = A.off
        ind2 = A.alloc("ind2", [2])
        oml = A.alloc("oml", [1024])
        hgn = A.alloc("hgn", [128])
        gffn = A.alloc("gffn", [D])
        cols = A.alloc("cols", [64])
        wr = A.alloc("wr", [16, 36])
        Sst = [A.alloc("S%d" % h, [128]) for h in range(8)]
        Sbf = [A.alloc("Sb%d" % h, [128], BF16) for h in range(8)]
        tails = A.alloc("tails", [8, 2])
        oh1_all = A.alloc("oh1_all", [NT, 32])
        oh2_all = A.alloc("oh2_all", [NT, 32])
        rank_all = A.alloc("rank_all", [NT, 32])
        Csum = A.alloc("Csum", [32])
        gmix_c = lambda c: cols[:, c:c + 1]
        gffn_c = lambda c: cols[:, 16 + c:17 + c]
        scn_c = lambda j: cols[:, 32 + j:33 + j]
        cw_c = lambda j, k: cols[:, 40 + 3 * j + k:41 + 3 * j + k]

        def memset(eng, buf, ap, val):
            S.op(eng, lambda g: g.memset(ap, val), writes=[buf])

        memset("pool", ident_f, ident_f[:], 1.0)
        S.op("pool", lambda g: g.affine_select(out=ident_f[:], in_=ident_f[:], pattern=[[-1, 128]], compare_op=ALU.is_equal,
                                                fill=0.0, base=0, channel_multiplier=1), reads=[ident_f], writes=[ident_f])
        S.op("dve", lambda g: g.tensor_copy(out=ident_b[:], in_=ident_f[:]), reads=[ident_f], writes=[ident_b])
        memset("pool", tri2, tri2[:], 1.0)
        S.op("pool", lambda g: g.affine_select(out=tri2[:], in_=tri2[:], pattern=[[1, 128]], compare_op=ALU.is_ge,
                                                fill=0.0, base=0, channel_multiplier=-1), reads=[tri2], writes=[tri2])
        memset("pool", tri2, tri2[0:64, 64:128], 0.0)
        memset("pool", stri, stri[:], 1.0)
        S.op("pool", lambda g: g.affine_select(out=stri[:], in_=stri[:], pattern=[[1, 128]], compare_op=ALU.is_ge,
                                                fill=0.0, base=-1, channel_multiplier=-1), reads=[stri], writes=[stri])
        memset("pool", ones_f, ones_f[:], 1.0)
        memset("pool", ones_b, ones_b[:], 1.0)
        memset("pool", ind2, ind2[:], 0.0)
        memset("pool", ind2, ind2[0:64, 0:1], 1.0)
        memset("pool", ind2, ind2[64:128, 1:2], 1.0)
        memset("pool", tails, tails[:], 0.0)
        memset("pool", Csum, Csum[:], 0.0)
        for h in range(8):
            memset("pool", Sst[h], Sst[h][:], 0.0)
            memset("pool", Sbf[h], Sbf[h][:], 0.0)
        S.op("pool", lambda g: g.iota(p4[:], pattern=[[1, 4]], base=0, channel_multiplier=4,
                                      allow_small_or_imprecise_dtypes=True), writes=[p4])
        tmp_lb = A.alloc("tmp_lb", [2, 1024])
        S.dma("sp", lambda g: g.dma_start(out=tmp_lb[:, 0, :], in_=lb_d.ap()[0:1, :].partition_broadcast(128)), writes=[tmp_lb])
        S.dma("sp", lambda g: g.dma_start(out=tmp_lb[:, 1, :], in_=lb_d.ap()[1:2, :].partition_broadcast(128)), writes=[tmp_lb])
        S.dma("sp", lambda g: g.dma_start(out=hgn[:], in_=hgn_d.ap().partition_broadcast(128)), writes=[hgn])
        S.dma("sp", lambda g: g.dma_start(out=gffn[:], in_=gffn_d.ap().partition_broadcast(128)), writes=[gffn])
        S.dma("sp", lambda g: g.dma_start(out=cols[:], in_=cols_d.ap()), writes=[cols])
        S.dma("sp", lambda g: g.dma_start(out=wr[:], in_=wr_d.ap()), writes=[wr])
        S.op("dve", lambda g: g.tensor_tensor(out=oml[:], in0=tmp_lb[:, 0, :], in1=tmp_lb[:, 1, :], op=ALU.subtract),
             reads=[tmp_lb], writes=[oml])
        S.op("act", lambda g: g.activation(out=oml[:], in_=oml[:], func=AF.Exp), reads=[oml], writes=[oml])
        S.op("dve", lambda g: g.tensor_scalar(out=oml[:], in0=oml[:], scalar1=1.0, scalar2=None, op0=ALU.add), reads=[oml], writes=[oml])
        S.op("dve", lambda g: g.reciprocal(out=oml[:], in_=oml[:]), reads=[oml], writes=[oml])
        for c in range(16):
            S.op("pool", (lambda c: lambda g: g.tensor_scalar(out=wr[:, c, :], in0=wr[:, c, :], scalar1=gffn_c(c), scalar2=None,
                                                              op0=ALU.mult))(c), reads=[wr, cols], writes=[wr])
        A.off -= 2048 + 0
        S.barrier()
        persist_mark = A.off

        wslab = [A.alloc("wslab%d" % i, [16, 512], BF16) for i in range(2)]
        xb = A.alloc("xb", [16, 512], BF16)
        xp = [A.alloc("xp%d" % i, [2, 512]) for i in range(4)]
        xmid_off = None
        sq = [A.alloc("sq%d" % i, [2, 512], BF16) for i in range(2)]
        omixT = A.alloc("omixT", [16, 512], BF16)
        byb = A.alloc("byb", [8, 512], BF16)
        ubuf = A.alloc("ubuf", [514])
        t1 = A.alloc("t1", [512])
        yv = A.alloc("yv", [512])
        yv2 = A.alloc("yv2", [512])
        sqy = A.alloc("sqy", [512], BF16)
        rstd_bc = A.alloc("rstd_bc", [512])
        rstd2_bc = A.alloc("rstd2_bc", [512])
        rstd_sc = A.alloc("rstd_sc", [512])
        lnbc = A.alloc("lnbc", [512])
        rcol = A.alloc("rcol", [8])
        xmid = [A.alloc("xmid0", [D], at=xb.off), A.alloc("xmid1", [D], at=xb.off + 2048),
                A.alloc("xmid2", [D], at=xp[0].off), A.alloc("xmid3", [D], at=xp[2].off)]
        assert xp[1].off == xp[0].off + 1024 and xp[3].off == xp[2].off + 1024
        for xm_, al_ in ((xmid[0], [xb]), (xmid[1], [xb]), (xmid[2], [xp[0], xp[1]]), (xmid[3], [xp[2], xp[3]])):
            xm_.aliases = list(al_)
            for a_ in al_:
                a_.aliases.append(xm_)
        xmT = A.alloc("xmT", [16, 128])
        hnb = A.alloc("hnb", [D], BF16)
        junk = A.alloc("junk", [D], BF16)
        etmp = [A.alloc("etmp%d" % t, [256]) for t in range(2)]
        qg = [[A.alloc("qg%d_%d" % (p_, t), [256]) for t in range(4)] for p_ in range(2)]
        kk = [[A.alloc("kk%d_%d" % (p_, t), [128]) for t in range(4)] for p_ in range(2)]
        lf = [[A.alloc("lf%d_%d" % (p_, t), [128]) for t in range(4)] for p_ in range(2)]
        vb = [[A.alloc("vb%d_%d" % (p_, t), [128], BF16) for t in range(4)] for p_ in range(2)]
        NTMP = 4
        eA = [A.alloc("eA%d" % i, [128]) for i in range(NTMP)]
        enA = [A.alloc("enA%d" % i, [128]) for i in range(NTMP)]
        Ecol = [A.alloc("Ecol%d" % i, [2]) for i in range(NTMP)]
        qt = [A.alloc("qt%d" % i, [128], BF16) for i in range(NTMP)]
        kt = [A.alloc("kt%d" % i, [128], BF16) for i in range(NTMP)]
        qtT = [A.alloc("qtT%d" % i, [128], BF16) for i in range(NTMP)]
        ktT = [A.alloc("ktT%d" % i, [128], BF16) for i in range(NTMP)]
        scTm = [A.alloc("scTm%d" % i, [128], BF16) for i in range(NTMP)]
        U0 = [A.alloc("U0%d" % i, [128]) for i in range(NTMP)]
        S1 = [A.alloc("S1%d" % i, [128]) for i in range(NTMP)]
        S1b = [A.alloc("S1b%d" % i, [128], BF16) for i in range(NTMP)]
        gsn = [A.alloc("gsn%d" % i, [128]) for i in range(NTMP)]
        ogb = [A.alloc("ogb%d" % i, [128], BF16) for i in range(NTMP)]
        sm = [A.alloc("sm%d" % i, [8]) for i in range(NTMP)]
        lg = A.alloc("lg", [36])
        rt = A.alloc("rt", [64])
        Ct = A.alloc("Ct", [32])

        import os
        _dbg2 = os.environ.get("K_DBG2", "")
        _dbg3 = os.environ.get("K_DBG3", "")

        def load_x_group(src_d, g0):
            for i in range(8):
                xpi = xp[i % 4]
                sqi = sq[i % 2]
                S.dma("sp", (lambda i, xpi: lambda g: g.dma_start(
                    out=xpi[:], in_=src_d.ap()[2 * i * 128:(2 * i + 2) * 128, g0 * 512:(g0 + 1) * 512]
                    .rearrange("(c p) t -> p c t", p=128)))(i, xpi), writes=[xpi])
                S.op("act", (lambda xpi, sqi: lambda g: g.activation(out=sqi[:], in_=xpi[:], func=AF.Square))(xpi, sqi),
                     reads=[xpi], writes=[sqi])
                for cc in range(2):
                    c = 2 * i + cc
                    S.op("act", (lambda c, cc, xpi: lambda g: g.activation(out=xb[:, c, :], in_=xpi[:, cc, :], func=AF.Copy, scale=gmix_c(c)))(c, cc, xpi),
                         reads=[xpi, cols], writes=[xb])
                    S.op("pe", (lambda c, cc, sqi: lambda g: g.matmul(banks[7][:, :], lhsT=ones_b[:], rhs=sqi[:, cc, :],
                                                                      start=(c == 0), stop=(c == 15)))(c, cc, sqi),
                         reads=[sqi, ones_b], writes=[banks[7]])
            S.op("act", lambda g: g.activation(out=lnbc[:], in_=banks[7][:, :], func=AF.Ln, scale=1.0 / D, bias=EPS),
                 reads=[banks[7]], writes=[lnbc])
            S.op("act", lambda g: g.activation(out=rstd_bc[:], in_=lnbc[:], func=AF.Exp, scale=-0.5), reads=[lnbc], writes=[rstd_bc])
            S.op("act", lambda g: g.activation(out=rstd2_bc[:], in_=lnbc[:], func=AF.Exp, scale=-1.0), reads=[lnbc], writes=[rstd2_bc])
            if _dbg2 == "nok1":
                return
            for t in range(4):
                S.op("pe", (lambda t: lambda g: g.transpose(out=banks[3][:, 384 + 32 * t:384 + 32 * (t + 1)], in_=rstd_bc[0:32, t * 128:(t + 1) * 128],
                                                            identity=ident_f[0:32, 0:32]))(t),
                     reads=[rstd_bc, ident_f], writes=[banks[3]])
            S.op("dve", lambda g: g.tensor_copy(out=rcol[:, 0:4], in_=banks[3][:, 384:512].rearrange("p (a b) -> p a b", a=4)[:, :, 0]),
                 reads=[banks[3]], writes=[rcol])
            S.op("dve", lambda g: g.tensor_scalar(out=rcol[:, 4:8], in0=rcol[:, 0:4], scalar1=-1.0, scalar2=None, op0=ALU.mult),
                 reads=[rcol], writes=[rcol])


        slab_plan = []
        slab_state = {"i": 0, "issued": 0}

        def plan_slabs():
            for wg in range(NWG):
                for h in range(8):
                    slab_plan.append((whg_d.ap()[h, :, 256:512].rearrange("(c p) n -> p c n", p=128), 256))
                if wg == NWG - 1:
                    for j in range(8):
                        slab_plan.append((wsc_d.ap()[j, :, :].rearrange("(c p) n -> p c n", p=128), 384))
            for g0 in range(NG):
                for h in range(8):
                    slab_plan.append((whg_d.ap()[h, :, :].rearrange("(c p) n -> p c n", p=128), 512))
                for j in range(8):
                    slab_plan.append((wsc_d.ap()[j, :, :].rearrange("(c p) n -> p c n", p=128), 384))
                for n in range(4):
                    slab_plan.append((wout_d.ap()[:, n * 512:(n + 1) * 512].rearrange("(c p) n -> p c n", p=128), 512))

        def issue_slab(i):
            src_ap, ncols = slab_plan[i]
            ws = wslab[i % 2]
            S.dma("pool", (lambda ws, src_ap, ncols: lambda g: g.dma_start(out=ws[:, :, 0:ncols], in_=src_ap))(ws, src_ap, ncols), writes=[ws])

        def next_slab(ncols_expected):
            i = slab_state["i"]
            slab_state["i"] += 1
            while slab_state["issued"] <= min(i + 1, len(slab_plan) - 1):
                issue_slab(slab_state["issued"])
                slab_state["issued"] += 1
            assert slab_plan[i][1] == ncols_expected, (i, slab_plan[i][1], ncols_expected)
            return wslab[i % 2]

        tmp_ctr = {"n": 0, "e": 0}

        def gen_inproj(h, full, par):
            c0 = 0 if full else 256
            ncols = 512 if full else 256
            ws = next_slab(ncols)
            oml_h = oml[:, h * 128:(h + 1) * 128]
            fo = 256 - c0
            for t in range(4):
                bk = banks[t % 2]
                for c in range(16):
                    S.op("pe", (lambda c, t, bk: lambda g: g.matmul(bk[:, 0:ncols], lhsT=xb[:, c, t * 128:(t + 1) * 128],
                                                                     rhs=ws[:, c, 0:ncols], start=(c == 0), stop=(c == 15)))(c, t, bk),
                         reads=[xb, ws], writes=[bk])
                    if c % 2 == 1 and c < 15:
                        yield
                rs = rcol[:, t:t + 1]
                nrs = rcol[:, 4 + t:5 + t]
                qg_t, kk_t, vb_t, lf_t = qg[par][t], kk[par][t], vb[par][t], lf[par][t]
                et = etmp[tmp_ctr["e"] % 2]
                tmp_ctr["e"] += 1
                if full:
                    S.op("act", (lambda et, bk, nrs: lambda g: g.activation(out=et[:], in_=bk[:, 0:256], func=AF.Exp, scale=nrs))(et, bk, nrs),
                         reads=[bk, rcol], writes=[et])
                S.op("act", (lambda kk_t, bk, rs: lambda g: g.activation(out=kk_t[:], in_=bk[:, fo:fo + 128], func=AF.Exp, scale=rs))(kk_t, bk, rs),
                     reads=[bk, rcol], writes=[kk_t])
                S.op("dve", (lambda vb_t, bk, rs: lambda g: g.tensor_scalar(out=vb_t[:], in0=bk[:, fo + 128:fo + 256], scalar1=rs, scalar2=None,
                                                                        op0=ALU.mult))(vb_t, bk, rs), reads=[bk, rcol], writes=[vb_t])
                if full:
                    S.op("act", (lambda et: lambda g: g.activation(out=et[:], in_=et[:], func=AF.Ln, bias=1.0))(et), reads=[et], writes=[et])
                    S.op("act", (lambda et: lambda g: g.activation(out=et[:], in_=et[:], func=AF.Exp, scale=-1.0))(et), reads=[et], writes=[et])
                    S.op("dve", (lambda qg_t, bk, rs, et: lambda g: g.scalar_tensor_tensor(out=qg_t[:], in0=bk[:, 0:256], scalar=rs, in1=et[:],
                                                                                       op0=ALU.mult, op1=ALU.mult))(qg_t, bk, rs, et),
                         reads=[bk, rcol, et], writes=[qg_t])
                S.op("act", (lambda kk_t: lambda g: g.activation(out=kk_t[:], in_=kk_t[:], func=AF.Ln, bias=1.0))(kk_t), reads=[kk_t], writes=[kk_t])
                S.op("act", (lambda kk_t: lambda g: g.activation(out=kk_t[:], in_=kk_t[:], func=AF.Exp, scale=-1.0))(kk_t), reads=[kk_t], writes=[kk_t])
                S.op("dve", (lambda kk_t: lambda g: g.tensor_tensor(out=kk_t[:], in0=kk_t[:], in1=oml_h, op=ALU.mult))(kk_t),
                     reads=[kk_t, oml], writes=[kk_t])
                S.op("act", (lambda kk_t, lf_t: lambda g: g.activation(out=lf_t[:], in_=kk_t[:], func=AF.Ln, scale=-1.0, bias=1.0))(kk_t, lf_t),
                     reads=[kk_t], writes=[lf_t])
                yield

        B2b = banks[2].t[:, :].bitcast(BF16)

        def gen_pre(h, t, full, par, i):
            b3 = banks[3] if i % 2 == 0 else banks[7]
            tq = 0 if i % 2 == 0 else 256
            qg_t, kk_t, lf_t = qg[par][t], kk[par][t], lf[par][t]
            S.op("pe", lambda g: g.matmul(b3[:, 0:128], lhsT=tri2[:], rhs=lf_t[:], start=True, stop=True), reads=[tri2, lf_t], writes=[b3])
            S.op("pe", lambda g: g.matmul(b3[:, 128:130], lhsT=lf_t[:], rhs=ind2[:], start=True, stop=True), reads=[lf_t, ind2], writes=[b3])
            yield
            S.op("act", lambda g: g.activation(out=enA[i][:], in_=b3[:, 0:128], func=AF.Exp, scale=-1.0), reads=[b3], writes=[enA[i]])
            if full:
                S.op("act", lambda g: g.activation(out=eA[i][:], in_=b3[:, 0:128], func=AF.Exp), reads=[b3], writes=[eA[i]])
            S.op("act", lambda g: g.activation(out=Ecol[i][:], in_=b3[:, 128:130], func=AF.Exp), reads=[b3], writes=[Ecol[i]])
            S.op("dve", lambda g: g.tensor_tensor(out=kt[i][:], in0=kk_t[:], in1=enA[i][:], op=ALU.mult), reads=[kk_t, enA[i]], writes=[kt[i]])
            if not full:
                return
            S.op("dve", lambda g: g.tensor_tensor(out=qt[i][:], in0=qg_t[:, 0:128], in1=eA[i][:], op=ALU.mult), reads=[qg_t, eA[i]], writes=[qt[i]])
            S.op("pool", lambda g: g.tensor_tensor(out=gsn[i][:], in0=qg_t[:, 128:256], in1=hgn[:], op=ALU.mult), reads=[qg_t, hgn], writes=[gsn[i]])
            S.op("pe", lambda g: g.transpose(out=B6b[:, tq:tq + 128], in_=qt[i][:], identity=ident_b[:]), reads=[qt[i], ident_b], writes=[banks[6]])
            S.op("pe", lambda g: g.transpose(out=B6b[:, tq + 128:tq + 256], in_=kt[i][:], identity=ident_b[:]), reads=[kt[i], ident_b], writes=[banks[6]])
            yield
            S.op("act", lambda g: g.copy(out=qtT[i][:], in_=B6b[:, tq:tq + 128]), reads=[banks[6]], writes=[qtT[i]])
            S.op("act", lambda g: g.copy(out=ktT[i][:], in_=B6b[:, tq + 128:tq + 256]), reads=[banks[6]], writes=[ktT[i]])
            S.op("pe", lambda g: g.matmul(b3[:, 256:384], lhsT=ktT[i][:], rhs=qtT[i][:], start=True, stop=True), reads=[ktT[i], qtT[i]], writes=[b3])
            yield
            S.op("dve", lambda g: g.tensor_tensor(out=scTm[i][:], in0=b3[:, 256:384], in1=tri2[:], op=ALU.mult), reads=[b3, tri2], writes=[scTm[i]])

        def gen_state(h, t, full, par, i):
            Sh, Sb = Sst[h], Sbf[h]
            b4, b5 = banks[4], banks[5]
            vb_t = vb[par][t]
            S.op("dve", lambda g: g.tensor_scalar(out=U0[i][:], in0=Sh[:], scalar1=Ecol[i][:, 0:1], scalar2=None, op0=ALU.mult),
                 reads=[Sh, Ecol[i]], writes=[U0[i]])
            S.op("pe", lambda g: g.matmul(b5[:, 0:128], lhsT=kt[i][0:64, :], rhs=vb_t[0:64, :], start=True, stop=True), reads=[kt[i], vb_t], writes=[b5])
            yield
            S.op("dve", lambda g: g.scalar_tensor_tensor(out=S1[i][:], in0=b5[:, 0:128], scalar=Ecol[i][:, 0:1], in1=U0[i][:], op0=ALU.mult, op1=ALU.add),
                 reads=[b5, Ecol[i], U0[i]], writes=[S1[i]])
            S.op("dve", lambda g: g.tensor_scalar(out=U0[i][:], in0=S1[i][:], scalar1=Ecol[i][:, 1:2], scalar2=None, op0=ALU.mult),
                 reads=[S1[i], Ecol[i]], writes=[U0[i]])
            if full:
                S.op("act", lambda g: g.copy(out=S1b[i][:], in_=S1[i][:]), reads=[S1[i]], writes=[S1b[i]])
                S.op("pe", lambda g: g.matmul(b4[:, 0:128], lhsT=scTm[i][:], rhs=vb_t[:], start=True, stop=False), reads=[scTm[i], vb_t], writes=[b4])
                S.op("pe", lambda g: g.matmul(b4[0:64, 0:128], lhsT=qtT[i][:, 0:64], rhs=Sb[:], start=False, stop=True), reads=[qtT[i], Sb], writes=[b4])
                S.op("pe", lambda g: g.matmul(b4[64:128, 0:128], lhsT=qtT[i][:, 64:128], rhs=S1b[i][:], start=False, stop=True),
                     reads=[qtT[i], S1b[i]], writes=[b4])
            S.op("pe", lambda g: g.matmul(b5[:, 128:256], lhsT=kt[i][64:128, :], rhs=vb_t[64:128, :], start=True, stop=True), reads=[kt[i], vb_t], writes=[b5])
            yield
            S.op("dve", lambda g: g.scalar_tensor_tensor(out=Sh[:], in0=b5[:, 128:256], scalar=Ecol[i][:, 1:2], in1=U0[i][:], op0=ALU.mult, op1=ALU.add),
                 reads=[b5, Ecol[i], U0[i]], writes=[Sh])
            if not full:
                return
            S.op("act", lambda g: g.copy(out=Sb[:], in_=Sh[:]), reads=[Sh], writes=[Sb])
            S.op("act", lambda g: g.activation(out=junk[:, 0:128], in_=b4[:, 0:128], func=AF.Square, accum_out=sm[i][:, 0:1]), reads=[b4], writes=[junk, sm[i]])
            S.op("act", lambda g: g.activation(out=sm[i][:, 1:2], in_=sm[i][:, 0:1], func=AF.Ln, scale=1.0 / 128, bias=EPS_HG), reads=[sm[i]], writes=[sm[i]])
            S.op("act", lambda g: g.activation(out=sm[i][:, 2:3], in_=sm[i][:, 1:2], func=AF.Exp, scale=-0.5), reads=[sm[i]], writes=[sm[i]])
            S.op("dve", lambda g: g.scalar_tensor_tensor(out=ogb[i][:], in0=b4[:, 0:128], scalar=sm[i][:, 2:3], in1=gsn[i][:], op0=ALU.mult, op1=ALU.mult),
                 reads=[b4, sm[i], gsn[i]], writes=[ogb[i]])
            S.op("pe", lambda g: g.transpose(out=B2b[:, 0:128], in_=ogb[i][:], identity=ident_b[:]), reads=[ogb[i], ident_b], writes=[banks[2]])
            yield
            S.op("act", lambda g: g.copy(out=omixT[:, h, t * 128:(t + 1) * 128], in_=B2b[:, 0:128]), reads=[banks[2]], writes=[omixT])

        def exhaust(gen):
            if gen is None:
                return
            for _ in gen:
                pass

        def roundrobin(gens, steps):
            live = [g is not None for g in gens]
            while any(live[:-1]):
                for k, g in enumerate(gens):
                    if not live[k]:
                        continue
                    for _ in range(steps[k]):
                        try:
                            next(g)
                        except StopIteration:
                            live[k] = False
                            break

        def run_heads(full):
            exhaust(gen_inproj(0, full, 0))
            units = [(h, t) for h in range(8) for t in range(4)]
            nxt = 0
            pres = []
            ready = []
            cur_state = None
            n_state_done = 0
            bg = None
            bg_head = -1
            while n_state_done < len(units):
                while len(pres) < 2 and nxt < len(units) and nxt - n_state_done < NTMP - 1:
                    h, t = units[nxt]
                    if t == 0 and bg is not None and bg_head == h:
                        exhaust(bg)
                        bg = None
                    if t == 1 and h + 1 < 8 and bg is None:
                        bg = gen_inproj(h + 1, full, (h + 1) % 2)
                        bg_head = h + 1
                    pres.append((nxt, gen_pre(h, t, full, h % 2, nxt % NTMP)))
                    nxt += 1
                if cur_state is None and ready and ready[0] == n_state_done:
                    u = ready.pop(0)
                    h, t = units[u]
                    cur_state = gen_state(h, t, full, h % 2, u % NTMP)
                progressed = False
                if cur_state is not None:
                    progressed = True
                    try:
                        next(cur_state)
                    except StopIteration:
                        cur_state = None
                        n_state_done += 1
                for item in list(pres):
                    progressed = True
                    try:
                        next(item[1])
                    except StopIteration:
                        pres.remove(item)
                        ready.append(item[0])
                        ready.sort()
                if bg is not None:
                    for _ in range(3):
                        try:
                            next(bg)
                        except StopIteration:
                            bg = None
                            break
                assert progressed or bg is not None or cur_state is not None or ready, "pipeline stalled"
            exhaust(bg)

        sc_ctr = {"n": 0}

        def sc_chunk(j, tail_only=False):
            ws = next_slab(384)
            bset = [banks[0], banks[1], banks[2]] if sc_ctr["n"] % 2 == 0 else [banks[3], banks[4], banks[5]]
            sc_ctr["n"] += 1
            for part in range(3):
                if tail_only and part == 0:
                    continue
                bk = bset[part]
                for c in range(16):
                    S.op("pe", (lambda c, part, bk: lambda g: g.matmul(bk[:, :], lhsT=ws[:, c, part * 128:(part + 1) * 128], rhs=xb[:, c, :],
                                                                        start=(c == 0), stop=(c == 15)))(c, part, bk),
                         reads=[ws, xb], writes=[bk])
            S.op("pool", lambda g: g.tensor_copy(out=ubuf[:, 0:2], in_=tails[:, j, :]), reads=[tails], writes=[ubuf])
            S.op("dve", lambda g: g.tensor_tensor(out=t1[:], in0=bset[1][:, :], in1=rstd2_bc[:], op=ALU.mult),
                 reads=[bset[1], rstd2_bc], writes=[t1])
            S.op("dve", lambda g: g.tensor_tensor(out=ubuf[:, 2:514], in0=t1[:], in1=bset[2][:, :], op=ALU.mult),
                 reads=[bset[2], t1], writes=[ubuf])
            S.op("pool", lambda g: g.tensor_copy(out=tails[:, j, :], in_=ubuf[:, 512:514]), reads=[ubuf], writes=[tails])
            if tail_only:
                return
            S.op("act", lambda g: g.activation(out=yv[:], in_=ubuf[:, 0:512], func=AF.Copy, scale=cw_c(j, 0)), reads=[ubuf, cols], writes=[yv])
            S.op("dve", lambda g: g.scalar_tensor_tensor(out=yv[:], in0=ubuf[:, 1:513], scalar=cw_c(j, 1), in1=yv[:], op0=ALU.mult, op1=ALU.add),
                 reads=[ubuf, cols, yv], writes=[yv])
            S.op("dve", lambda g: g.scalar_tensor_tensor(out=yv[:], in0=ubuf[:, 2:514], scalar=cw_c(j, 2), in1=yv[:], op0=ALU.mult, op1=ALU.add),
                 reads=[ubuf, cols, yv], writes=[yv])
            S.op("dve", lambda g: g.tensor_tensor(out=yv[:], in0=yv[:], in1=rstd_bc[:], op=ALU.mult), reads=[yv, rstd_bc], writes=[yv])
            S.op("dve", lambda g: g.tensor_tensor(out=byb[:, j, :], in0=bset[0][:, :], in1=yv[:], op=ALU.mult),
                 reads=[bset[0], yv], writes=[byb])
            S.op("act", lambda g: g.activation(out=sqy[:], in_=byb[:, j, :], func=AF.Square), reads=[byb], writes=[sqy])
            S.op("pe", lambda g: g.matmul(banks[7][:, :], lhsT=ones_b[:], rhs=sqy[:], start=(j == 0), stop=(j == 7)),
                 reads=[ones_b, sqy], writes=[banks[7]])

        def sc_finish():
            S.op("act", lambda g: g.activation(out=lnbc[:], in_=banks[7][:, :], func=AF.Ln, scale=1.0 / 1024, bias=EPS),
                 reads=[banks[7]], writes=[lnbc])
            S.op("act", lambda g: g.activation(out=rstd_sc[:], in_=lnbc[:], func=AF.Exp, scale=-0.5), reads=[lnbc], writes=[rstd_sc])
            for j in range(8):
                S.op("dve", (lambda j: lambda g: g.scalar_tensor_tensor(out=omixT[:, 8 + j, :], in0=byb[:, j, :], scalar=scn_c(j), in1=rstd_sc[:],
                                                                        op0=ALU.mult, op1=ALU.mult))(j),
                     reads=[byb, cols, rstd_sc], writes=[omixT])

        def outproj_and_route(g0):
            for t in range(4):
                r0 = g0 * 512 + t * 128
                S.dma("sp", (lambda t, r0: lambda g: g.dma_start(out=xmid[t][:], in_=xtok_d.ap()[r0:r0 + 128, :]))(t, r0), writes=[xmid[t]])
            k = 0
            for n in range(4):
                ws = next_slab(512)
                for t in range(4):
                    bk = banks[k % 3]
                    k += 1
                    for c in range(16):
                        S.op("pe", (lambda c, t, bk, ws: lambda g: g.matmul(bk[:, :], lhsT=omixT[:, c, t * 128:(t + 1) * 128], rhs=ws[:, c, :],
                                                                            start=(c == 0), stop=(c == 15)))(c, t, bk, ws),
                             reads=[omixT, ws], writes=[bk])
                    S.op("dve", (lambda t, n, bk: lambda g: g.tensor_tensor(out=xmid[t][:, n * 512:(n + 1) * 512], in0=bk[:, :],
                                                                           in1=xmid[t][:, n * 512:(n + 1) * 512], op=ALU.add))(t, n, bk),
                         reads=[bk, xmid[t]], writes=[xmid[t]])
            for t in range(4):
                tile = g0 * 4 + t
                r0 = tile * 128
                xm = xmid[t]
                S.dma("act", (lambda xm, r0: lambda g: g.dma_start(out=xmid_d.ap()[r0:r0 + 128, :], in_=xm[:]))(xm, r0), reads=[xm])
                S.op("act", (lambda xm: lambda g: g.activation(out=junk[:], in_=xm[:], func=AF.Square, accum_out=rt[:, 0:1]))(xm),
                     reads=[xm], writes=[junk, rt])
                S.op("act", lambda g: g.activation(out=rt[:, 1:2], in_=rt[:, 0:1], func=AF.Ln, scale=1.0 / D, bias=EPS), reads=[rt], writes=[rt])
                S.op("act", lambda g: g.activation(out=rt[:, 2:3], in_=rt[:, 1:2], func=AF.Exp, scale=-0.5), reads=[rt], writes=[rt])
                S.op("dve", (lambda xm: lambda g: g.scalar_tensor_tensor(out=hnb[:], in0=xm[:], scalar=rt[:, 2:3], in1=gffn[:],
                                                                        op0=ALU.mult, op1=ALU.mult))(xm), reads=[xm, rt, gffn], writes=[hnb])
                S.dma("sp", (lambda r0: lambda g: g.dma_start(out=hn_d.ap()[r0:r0 + 128, :], in_=hnb[:]))(r0), reads=[hnb])
                for q4 in range(4):
                    bk = banks[q4 % 3]
                    for cc in range(4):
                        c = q4 * 4 + cc
                        S.op("pe", (lambda c, cc, bk, xm: lambda g: g.transpose(out=bk[:, cc * 128:(cc + 1) * 128], in_=xm[:, c * 128:(c + 1) * 128],
                                                                              identity=ident_f[:]))(c, cc, bk, xm),
                             reads=[xm, ident_f], writes=[bk])
                    eng = "act" if q4 % 2 == 0 else "dve"
                    if eng == "act":
                        S.op("act", (lambda q4, bk: lambda g: g.copy(out=xmT[:, q4 * 4:(q4 + 1) * 4, :], in_=bk[:, :].rearrange("p (a b) -> p a b", a=4)))(q4, bk),
                             reads=[bk], writes=[xmT])
                    else:
                        S.op("dve", (lambda q4, bk: lambda g: g.tensor_copy(out=xmT[:, q4 * 4:(q4 + 1) * 4, :], in_=bk[:, :].rearrange("p (a b) -> p a b", a=4)))(q4, bk),
                             reads=[bk], writes=[xmT])
                for c in range(16):
                    S.op("pe", (lambda c: lambda g: g.matmul(banks[7][:, 0:36], lhsT=xmT[:, c, :], rhs=wr[:, c, :], start=(c == 0), stop=(c == 15)))(c),
                         reads=[xmT, wr], writes=[banks[7]])
                S.op("dve", lambda g: g.tensor_scalar(out=lg[:], in0=banks[7][:, 0:36], scalar1=rt[:, 2:3], scalar2=None, op0=ALU.mult),
                     reads=[banks[7], rt], writes=[lg])
                route_tile(tile)

        def route_tile(tile):
            P = "dve"
            R = [lg, rt]
            S.op("dve", lambda g: g.tensor_reduce(out=rt[:, 3:4], in_=lg[:, 0:4], axis=AX.X, op=ALU.max), reads=R, writes=[rt])
            S.op("dve", lambda g: g.tensor_scalar(out=rt[:, 12:16], in0=lg[:, 0:4], scalar1=rt[:, 3:4], scalar2=None, op0=ALU.is_equal), reads=R, writes=[rt])
            S.op("dve", lambda g: g.tensor_scalar(out=rt[:, 48:52], in0=lg[:, 0:4], scalar1=rt[:, 3:4], scalar2=None, op0=ALU.subtract), reads=R, writes=[rt])
            S.op("act", lambda g: g.activation(out=rt[:, 48:52], in_=rt[:, 48:52], func=AF.Exp, accum_out=rt[:, 4:5]), reads=R, writes=[rt])
            S.op("dve", lambda g: g.reciprocal(out=rt[:, 5:6], in_=rt[:, 4:5]), reads=R, writes=[rt])
            S.op("dve", lambda g: g.tensor_scalar(out=rt[:, 16:24], in0=lg[:, 4:12], scalar1=rt[:, 12:13], scalar2=None, op0=ALU.mult), reads=R, writes=[rt])
            for gi in range(1, 4):
                S.op("dve", (lambda gi: lambda g: g.scalar_tensor_tensor(out=rt[:, 16:24], in0=lg[:, 4 + 8 * gi:12 + 8 * gi], scalar=rt[:, 12 + gi:13 + gi],
                                                                         in1=rt[:, 16:24], op0=ALU.mult, op1=ALU.add))(gi), reads=R, writes=[rt])
            S.op("dve", lambda g: g.tensor_reduce(out=rt[:, 6:7], in_=rt[:, 16:24], axis=AX.X, op=ALU.max), reads=R, writes=[rt])
            S.op("dve", lambda g: g.tensor_scalar(out=rt[:, 24:32], in0=rt[:, 16:24], scalar1=rt[:, 6:7], scalar2=None, op0=ALU.is_equal), reads=R, writes=[rt])
            S.op("dve", lambda g: g.scalar_tensor_tensor(out=rt[:, 32:40], in0=rt[:, 24:32], scalar=-1e30, in1=rt[:, 16:24], op0=ALU.mult, op1=ALU.add),
                 reads=R, writes=[rt])
            S.op("dve", lambda g: g.tensor_reduce(out=rt[:, 7:8], in_=rt[:, 32:40], axis=AX.X, op=ALU.max), reads=R, writes=[rt])
            S.op("dve", lambda g: g.tensor_scalar(out=rt[:, 40:48], in0=rt[:, 32:40], scalar1=rt[:, 7:8], scalar2=None, op0=ALU.is_equal), reads=R, writes=[rt])
            S.op("dve", lambda g: g.tensor_tensor(out=rt[:, 8:9], in0=rt[:, 7:8], in1=rt[:, 6:7], op=ALU.subtract), reads=R, writes=[rt])
            S.op("act", lambda g: g.activation(out=rt[:, 8:9], in_=rt[:, 8:9], func=AF.Exp), reads=R, writes=[rt])
            S.op("dve", lambda g: g.tensor_scalar(out=rt[:, 8:9], in0=rt[:, 8:9], scalar1=1.0, scalar2=None, op0=ALU.add), reads=R, writes=[rt])
            S.op("dve", lambda g: g.reciprocal(out=rt[:, 8:9], in_=rt[:, 8:9]), reads=R, writes=[rt])
            S.op("dve", lambda g: g.tensor_tensor(out=w12_all[:, 0, tile:tile + 1], in0=rt[:, 8:9], in1=rt[:, 5:6], op=ALU.mult), reads=R, writes=[w12_all])
            S.op("dve", lambda g: g.tensor_tensor(out=w12_all[:, 1, tile:tile + 1], in0=rt[:, 5:6], in1=w12_all[:, 0, tile:tile + 1], op=ALU.subtract),
                 reads=R + [w12_all], writes=[w12_all])
            for gi in range(4):
                S.op(P, (lambda gi: lambda g: g.tensor_scalar(out=oh1_all[:, tile, gi * 8:(gi + 1) * 8], in0=rt[:, 24:32], scalar1=rt[:, 12 + gi:13 + gi],
                                                               scalar2=None, op0=ALU.mult))(gi), reads=R, writes=[oh1_all])
                S.op(P, (lambda gi: lambda g: g.tensor_scalar(out=oh2_all[:, tile, gi * 8:(gi + 1) * 8], in0=rt[:, 40:48], scalar1=rt[:, 12 + gi:13 + gi],
                                                               scalar2=None, op0=ALU.mult))(gi), reads=R, writes=[oh2_all])
            S.op(P, lambda g: g.tensor_tensor(out=Ct[:], in0=oh1_all[:, tile, :], in1=oh2_all[:, tile, :], op=ALU.add), reads=[oh1_all, oh2_all], writes=[Ct])
            S.op("pe", lambda g: g.matmul(banks[3][:, 400:432], lhsT=stri[:], rhs=Ct[:], start=True, stop=False), reads=[stri, Ct], writes=[banks[3]])
            S.op("pe", lambda g: g.matmul(banks[3][:, 400:432], lhsT=ones_f[:], rhs=Csum[:], start=False, stop=True), reads=[ones_f, Csum], writes=[banks[3]])
            S.op("dve", lambda g: g.tensor_copy(out=rank_all[:, tile, :], in_=banks[3][:, 400:432]), reads=[banks[3]], writes=[rank_all])
            S.op(P, lambda g: g.tensor_tensor(out=Csum[:], in0=Csum[:], in1=Ct[:], op=ALU.add), reads=[Csum, Ct], writes=[Csum])

        if stop <= 0:
            S.emit()
            return nc
        import os
        _dbg = os.environ.get("K_DBG", "")
        plan_slabs()
        for wg in range(NWG):
            load_x_group(xTp_d, wg)
            if _dbg == "lx":
                S.emit()
                return nc
            run_heads(False)
            if _dbg == "h8":
                S.emit()
                return nc
            if wg == NWG - 1:
                for j in range(8):
                    sc_chunk(j, tail_only=True)
        if stop <= 1:
            S.emit()
            return nc
        for h in range(8):
            S.op("act", (lambda h: lambda g: g.copy(out=Sbf[h][:], in_=Sst[h][:]))(h), reads=[Sst[h]], writes=[Sbf[h]])
        for zb in range(NB):
            S.dma("act", (lambda zb: lambda g: g.dma_start(out=xs_d.ap()[zb * 256:(zb + 1) * 256, :], in_=zeros_d.ap()))(zb))
        for g0 in range(NG):
            load_x_group(xT_d, g0)
            run_heads(True)
            for j in range(8):
                sc_chunk(j)
            sc_finish()
            outproj_and_route(g0)

        if stop <= 2:
            S.emit()
            return nc
        S.barrier()
        A.off = persist_mark
        fin = A.alloc("fin", [512])
        fin_i = A.alloc("fin_i", [128], I32)
        b3 = banks[3]
        S.op("pe", lambda g: g.matmul(b3[0:32, 0:128], lhsT=Csum[:], rhs=ones_f[:], start=True, stop=True), reads=[Csum, ones_f], writes=[b3])
        S.op("dve", lambda g: g.tensor_scalar(out=fin[0:32, 0:128], in0=b3[0:32, 0:128], scalar1=255.0, scalar2=None, op0=ALU.add), reads=[b3], writes=[fin])
        S.op("dve", lambda g: g.tensor_copy(out=fin_i[0:32, :], in_=fin[0:32, 0:128]), reads=[fin], writes=[fin_i])
        S.op("dve", lambda g: g.tensor_scalar(out=fin_i[0:32, :], in0=fin_i[0:32, :], scalar1=8, scalar2=8, op0=ALU.arith_shift_right,
                                              op1=ALU.logical_shift_left), reads=[fin_i], writes=[fin_i])
        S.op("dve", lambda g: g.tensor_copy(out=fin[0:32, 0:128], in_=fin_i[0:32, :]), reads=[fin_i], writes=[fin])
        S.op("pe", lambda g: g.matmul(b3[:, 128:160], lhsT=fin[0:32, 0:128], rhs=tri2[0:32, 0:32], start=True, stop=True), reads=[fin, tri2], writes=[b3])
        S.op("pe", lambda g: g.matmul(b3[:, 160:192], lhsT=fin[0:32, 0:128], rhs=stri[0:32, 0:32], start=True, stop=True), reads=[fin, stri], writes=[b3])
        S.op("dve", lambda g: g.tensor_copy(out=fin[:, 128:192], in_=b3[:, 128:192]), reads=[b3], writes=[fin])
        pend = fin[:, 128:160]
        pstart = fin[:, 160:192]
        tmp32 = fin[:, 192:224]
        for tile in range(NT):
            for k, oh in ((0, oh1_all), (1, oh2_all)):
                S.op("dve", (lambda tile: lambda g: g.tensor_tensor(out=tmp32, in0=rank_all[:, tile, :], in1=pstart, op=ALU.add))(tile),
                     reads=[rank_all, fin], writes=[fin])
                S.op("dve", (lambda tile, oh: lambda g: g.tensor_tensor(out=tmp32, in0=tmp32, in1=oh[:, tile, :], op=ALU.mult))(tile, oh),
                     reads=[oh, fin], writes=[fin])
                S.op("dve", (lambda tile, k: lambda g: g.tensor_reduce(out=dest_f[:, k, tile:tile + 1], in_=tmp32, axis=AX.X, op=ALU.add))(tile, k),
                     reads=[fin], writes=[dest_f])
        S.op("dve", lambda g: g.tensor_copy(out=dest_i[:], in_=dest_f[:]), reads=[dest_f], writes=[dest_i])
        bthr = A.alloc("bthr", [NB])
        bacc = A.alloc("bacc", [NB])
        idxf = A.alloc("idxf", [4, NB])
        S.op("pool", lambda g: g.iota(bthr[:], pattern=[[256, NB]], base=0, channel_multiplier=0, allow_small_or_imprecise_dtypes=True), writes=[bthr])
        memset("pool", bacc, bacc[:], 0.0)
        for e in range(NEXP):
            S.op("dve", (lambda e: lambda g: g.scalar_tensor_tensor(out=bacc[:], in0=bthr[:], scalar=fin[:, 128 + e:129 + e], in1=bacc[:],
                                                                   op0=ALU.is_ge, op1=ALU.add))(e), reads=[bthr, fin, bacc], writes=[bacc])
        S.op("dve", lambda g: g.tensor_scalar(out=bacc[:], in0=bacc[:], scalar1=float(NEXP - 1), scalar2=None, op0=ALU.min), reads=[bacc], writes=[bacc])
        for cc in range(4):
            S.op("dve", (lambda cc: lambda g: g.tensor_scalar(out=idxf[:, cc, :], in0=bacc[:], scalar1=512.0, scalar2=p4[:, cc:cc + 1],
                                                             op0=ALU.mult, op1=ALU.add))(cc), reads=[bacc, p4], writes=[idxf])
        S.op("dve", lambda g: g.tensor_copy(out=idxw[:], in_=idxf[:]), reads=[idxf], writes=[idxw])
        if debug:
            dbgt = A.alloc("dbgt", [8, NT])
            S.op("dve", lambda g: g.tensor_copy(out=dbgt[:, 0:2, :], in_=dest_f[:]), reads=[dest_f], writes=[dbgt])
            S.op("dve", lambda g: g.tensor_copy(out=dbgt[:, 2:4, :], in_=w12_all[:]), reads=[w12_all], writes=[dbgt])
            S.op("dve", lambda g: g.memset(dbgt[:, 4:8, :], 0.0), writes=[dbgt])
            S.dma("sp", lambda g: g.dma_start(out=dbg_d.ap(), in_=dbgt[:]), reads=[dbgt])
            S.dma("sp", lambda g: g.dma_start(out=dbgb_d.ap(), in_=bacc[:]), reads=[bacc])
        S.barrier()

        if stop <= 3:
            S.emit()
            return nc
        A.off = true_persist_mark
        hnt = [A.alloc("hnt%d" % i, [D], BF16) for i in range(2)]
        for tile in range(NT):
            ht = hnt[tile % 2]
            r0 = tile * 128
            S.dma("sp", (lambda ht, r0: lambda g: g.dma_start(out=ht[:], in_=hn_d.ap()[r0:r0 + 128, :]))(ht, r0), writes=[ht])
            for k in range(2):
                S.dma("pool", (lambda ht, k, tile: lambda g: g.indirect_dma_start(
                    out=xs_d.ap(), out_offset=bass.IndirectOffsetOnAxis(ap=dest_i[:, k, tile:tile + 1], axis=0),
                    in_=ht[:], in_offset=None))(ht, k, tile), reads=[ht, dest_i])
        S.barrier()
        if stop <= 4:
            S.emit()
            return nc
        A.off = true_persist_mark
        wgb = [A.alloc("wgb%d" % i, [16, 512], BF16) for i in range(2)]
        wub = [A.alloc("wub%d" % i, [16, 512], BF16) for i in range(2)]
        wdb = [A.alloc("wdb%d" % i, [4, 2048], BF16) for i in range(2)]
        xt = [A.alloc("xt%d" % i, [D], BF16) for i in range(2)]
        xsT2 = [A.alloc("xsT", [16, 256], BF16)]
        actT2 = [A.alloc("actT%d" % i, [4, 256], BF16) for i in range(2)]
        ee = A.alloc("ee", [256])
        ga = A.alloc("ga", [256])
        yrow = [A.alloc("yrow%d" % i, [D]) for i in range(2)]
        NSTG = min(8, (A.words - A.off) // 2048)
        assert NSTG >= 3, NSTG
        stage = [A.alloc("stage%d" % i, [2048]) for i in range(NSTG)]
        stg = {"n": 0}
        cast_engs = ["act", "dve"]

        assert NSTG == 8, NSTG

        def w_piece(b, i):
            k = b % 2
            if i < 4:
                return wgt_d, wgb[k], i, True
            if i < 8:
                return wut_d, wub[k], i - 4, True
            return wdt_d, wdb[k], i - 8, False

        def gather_w(b, i):
            tab, dst, cc, gu = w_piece(b, i)
            st = stage[i % NSTG]
            S.dma("pool", (lambda st, tab, cc, b: lambda g: g.indirect_dma_start(
                out=st[:], out_offset=None, in_=tab.ap(),
                in_offset=bass.IndirectOffsetOnAxis(ap=idxw[:, cc, b:b + 1], axis=0)))(st, tab, cc, b), reads=[idxw], writes=[st])

        def cast_w(b, i):
            tab, dst, cc, gu = w_piece(b, i)
            st = stage[i % NSTG]
            if gu:
                dview = dst[:, 4 * cc:4 * cc + 4, :]
                sview = st[:].rearrange("p (a b) -> p a b", a=4)
            else:
                dview = dst[:, cc, :]
                sview = st[:]
            if i % 2 == 0:
                S.op("act", (lambda dview, sview: lambda g: g.copy(out=dview, in_=sview))(dview, sview), reads=[st], writes=[dst])
            else:
                S.op("dve", (lambda dview, sview: lambda g: g.tensor_copy(out=dview, in_=sview))(dview, sview), reads=[st], writes=[dst])
            if i + NSTG < 12:
                gather_w(b, i + NSTG)

        for i in range(NSTG):
            gather_w(0, i)
        for i in range(12):
            cast_w(0, i)
        for b in range(NB):
            k = b % 2
            xsT = xsT2[b % len(xsT2)]
            actT = actT2[b % 2]
            if b + 1 < NB:
                for i in range(NSTG):
                    gather_w(b + 1, i)
            for r in range(2):
                r0 = b * 256 + r * 128
                S.dma("sp", (lambda r, r0: lambda g: g.dma_start(out=xt[r][:], in_=xs_d.ap()[r0:r0 + 128, :]))(r, r0), writes=[xt[r]])
                for hh in range(2):
                    bkb, bk = (B6b, banks[6]) if hh == 0 else (B5b, banks[5])
                    for cc in range(8):
                        c = hh * 8 + cc
                        S.op("pe", (lambda c, cc, r, bkb: lambda g: g.transpose(out=bkb[:, cc * 128:(cc + 1) * 128], in_=xt[r][:, c * 128:(c + 1) * 128],
                                                                             identity=ident_b[:]))(c, cc, r, bkb), reads=[xt[r], ident_b], writes=[bk])
                    if hh == 0:
                        S.op("act", (lambda r, bkb, xsT: lambda g: g.copy(out=xsT[:, 0:8, r * 128:(r + 1) * 128], in_=bkb.rearrange("p (a b) -> p a b", a=8)))(r, bkb, xsT),
                             reads=[bk], writes=[xsT])
                    else:
                        S.op("dve", (lambda r, bkb, xsT: lambda g: g.tensor_copy(out=xsT[:, 8:16, r * 128:(r + 1) * 128], in_=bkb.rearrange("p (a b) -> p a b", a=8)))(r, bkb, xsT),
                             reads=[bk], writes=[xsT])
            for fc in range(4):
                bg, bu = banks[0 + 2 * (fc % 2)], banks[1 + 2 * (fc % 2)]
                for c in range(16):
                    S.op("pe", (lambda c, fc, bg, k, xsT: lambda g: g.matmul(bg[:, 0:256], lhsT=wgb[k][:, c, fc * 128:(fc + 1) * 128], rhs=xsT[:, c, :],
                                                                     start=(c == 0), stop=(c == 15)))(c, fc, bg, k, xsT), reads=[wgb[k], xsT], writes=[bg])
                for c in range(16):
                    S.op("pe", (lambda c, fc, bu, k, xsT: lambda g: g.matmul(bu[:, 0:256], lhsT=wub[k][:, c, fc * 128:(fc + 1) * 128], rhs=xsT[:, c, :],
                                                                     start=(c == 0), stop=(c == 15)))(c, fc, bu, k, xsT), reads=[wub[k], xsT], writes=[bu])
                S.op("act", (lambda bg: lambda g: g.activation(out=ee[:], in_=bg[:, 0:256], func=AF.Exp, scale=-1.0))(bg), reads=[bg], writes=[ee])
                S.op("act", lambda g: g.activation(out=ee[:], in_=ee[:], func=AF.Ln, bias=1.0), reads=[ee], writes=[ee])
                S.op("act", lambda g: g.activation(out=ee[:], in_=ee[:], func=AF.Exp, scale=-1.0), reads=[ee], writes=[ee])
                S.op("dve", (lambda bg: lambda g: g.tensor_tensor(out=ga[:], in0=bg[:, 0:256], in1=ee[:], op=ALU.mult))(bg), reads=[bg, ee], writes=[ga])
                S.op("dve", (lambda bu, fc, actT: lambda g: g.tensor_tensor(out=actT[:, fc, :], in0=bu[:, 0:256], in1=ga[:], op=ALU.mult))(bu, fc, actT),
                     reads=[bu, ga], writes=[actT])
                if b + 1 < NB:
                    for i in (3 * fc, 3 * fc + 1, 3 * fc + 2):
                        cast_w(b + 1, i)
            kk2 = 0
            for r in range(2):
                for n in range(4):
                    bk = banks[4 + (kk2 % 2) * 3]
                    kk2 += 1
                    for fc in range(4):
                        S.op("pe", (lambda fc, r, n, bk, k, actT: lambda g: g.matmul(bk[:, :], lhsT=actT[:, fc, r * 128:(r + 1) * 128],
                                                                            rhs=wdb[k][:, fc, n * 512:(n + 1) * 512], start=(fc == 0), stop=(fc == 3)))(fc, r, n, bk, k, actT),
                             reads=[actT, wdb[k]], writes=[bk])
                    S.op("act", (lambda r, n, bk: lambda g: g.copy(out=yrow[r][:, n * 512:(n + 1) * 512], in_=bk[:, :]))(r, n, bk), reads=[bk], writes=[yrow[r]])
                r0 = b * 256 + r * 128
                S.dma("act", (lambda r, r0: lambda g: g.dma_start(out=ys_d.ap()[r0:r0 + 128, :], in_=yrow[r][:]))(r, r0), reads=[yrow[r]])
        S.barrier()

        if stop <= 5:
            S.emit()
            return nc
        A.off = true_persist_mark
        wpg = A.alloc("wpg", [16, D], BF16)
        wple = A.alloc("wple", [2, D], BF16)
        gple = A.alloc("gple", [D])
        gfin = A.alloc("gfin", [D])
        xm3s = [A.alloc("xm3_%d" % i, [D]) for i in range(2)]
        y12 = [A.alloc("y12_%d" % i, [D]) for i in range(2)]
        pTfs = [A.alloc("pTf%d" % i, [2, 128]) for i in range(2)]
        pTbs = [A.alloc("pTb%d" % i, [2, 128], BF16) for i in range(2)]
        x2bs = [A.alloc("x2b%d" % i, [D], BF16) for i in range(2)]
        x2Ts = [A.alloc("x2T%d" % i, [16, 128], BF16) for i in range(2)]
        plers = [A.alloc("pler%d" % i, [D]) for i in range(2)]
        egs = [A.alloc("eg%d" % i, [512]) for i in range(2)]
        junk3 = A.alloc("junk3", [D], BF16)
        s3s = [A.alloc("s3_%d" % i, [8]) for i in range(2)]
        for c4 in range(4):
            S.dma("pool", (lambda c4: lambda g: g.dma_start(out=wpg[:, 4 * c4:4 * c4 + 4, :],
                                                           in_=wpg_d.ap()[c4 * 512:(c4 + 1) * 512, :].rearrange("(c p) n -> p c n", p=128)))(c4), writes=[wpg])
        S.dma("pool", lambda g: g.dma_start(out=wple[:], in_=wple_d.ap().rearrange("(c p) n -> p c n", p=128)), writes=[wple])
        S.dma("sp", lambda g: g.dma_start(out=gple[:], in_=gple_d.ap().partition_broadcast(128)), writes=[gple])
        S.dma("sp", lambda g: g.dma_start(out=gfin[:], in_=gfin_d.ap().partition_broadcast(128)), writes=[gfin])

        def p3_load(tile):
            xm3, pTf = xm3s[tile % 2], pTfs[tile % 2]
            r0 = tile * 128
            S.dma("sp", (lambda r0, xm3: lambda g: g.dma_start(out=xm3[:], in_=xmid_d.ap()[r0:r0 + 128, :]))(r0, xm3), writes=[xm3])
            S.dma("sp", (lambda r0, pTf: lambda g: g.dma_start(out=pTf[:], in_=pT_d.ap()[:, r0:r0 + 128].rearrange("(c p) t -> p c t", p=128)))(r0, pTf),
                  writes=[pTf])

        def p3_gather(tile):
            for k in range(2):
                S.dma("pool", (lambda k, tile: lambda g: g.indirect_dma_start(
                    out=y12[k][:], out_offset=None, in_=ys_d.ap(),
                    in_offset=bass.IndirectOffsetOnAxis(ap=dest_i[:, k, tile:tile + 1], axis=0)))(k, tile), reads=[dest_i], writes=[y12[k]])

        p3_load(0)
        p3_gather(0)
        for tile in range(NT):
            r0 = tile * 128
            xm3, pTf, pTb, s3 = xm3s[tile % 2], pTfs[tile % 2], pTbs[tile % 2], s3s[tile % 2]
            x2b, x2T, pler = x2bs[tile % 2], x2Ts[tile % 2], plers[tile % 2]
            if tile + 1 < NT:
                p3_load(tile + 1)
            S.op("act", (lambda pTb, pTf: lambda g: g.copy(out=pTb[:], in_=pTf[:]))(pTb, pTf), reads=[pTf], writes=[pTb])
            S.op("dve", (lambda tile, xm3: lambda g: g.scalar_tensor_tensor(out=xm3[:], in0=y12[0][:], scalar=w12_all[:, 0, tile:tile + 1], in1=xm3[:],
                                                                           op0=ALU.mult, op1=ALU.add))(tile, xm3), reads=[y12[0], w12_all, xm3], writes=[xm3])
            S.op("dve", (lambda tile, xm3: lambda g: g.scalar_tensor_tensor(out=xm3[:], in0=y12[1][:], scalar=w12_all[:, 1, tile:tile + 1], in1=xm3[:],
                                                                           op0=ALU.mult, op1=ALU.add))(tile, xm3), reads=[y12[1], w12_all, xm3], writes=[xm3])
            if tile + 1 < NT:
                p3_gather(tile + 1)
            S.op("act", (lambda xm3, x2b: lambda g: g.copy(out=x2b[:], in_=xm3[:]))(xm3, x2b), reads=[xm3], writes=[x2b])
            for hh in range(2):
                bkb, bk = (B6b, banks[6]) if hh == 0 else (B5b, banks[5])
                for cc in range(8):
                    c = hh * 8 + cc
                    S.op("pe", (lambda c, cc, bkb, x2b: lambda g: g.transpose(out=bkb[:, cc * 128:(cc + 1) * 128], in_=x2b[:, c * 128:(c + 1) * 128],
                                                                      identity=ident_b[:]))(c, cc, bkb, x2b), reads=[x2b, ident_b], writes=[bk])
                if hh == 0:
                    S.op("act", (lambda bkb, x2T: lambda g: g.copy(out=x2T[:, 0:8, :], in_=bkb.rearrange("p (a b) -> p a b", a=8)))(bkb, x2T), reads=[bk], writes=[x2T])
                else:
                    S.op("dve", (lambda bkb, x2T: lambda g: g.tensor_copy(out=x2T[:, 8:16, :], in_=bkb.rearrange("p (a b) -> p a b", a=8)))(bkb, x2T), reads=[bk], writes=[x2T])
            for n in range(4):
                bk = banks[n % 2]
                for kc in range(2):
                    S.op("pe", (lambda kc, n, bk, pTb: lambda g: g.matmul(bk[:, :], lhsT=pTb[:, kc, :], rhs=wple[:, kc, n * 512:(n + 1) * 512],
                                                                          start=(kc == 0), stop=(kc == 1)))(kc, n, bk, pTb), reads=[pTb, wple], writes=[bk])
                S.op("act", (lambda n, bk, pler: lambda g: g.copy(out=pler[:, n * 512:(n + 1) * 512], in_=bk[:, :]))(n, bk, pler), reads=[bk], writes=[pler])
            S.op("act", (lambda s3, pler: lambda g: g.activation(out=junk3[:], in_=pler[:], func=AF.Square, accum_out=s3[:, 0:1]))(s3, pler), reads=[pler], writes=[junk3, s3])
            S.op("act", (lambda s3: lambda g: g.activation(out=s3[:, 1:2], in_=s3[:, 0:1], func=AF.Ln, scale=1.0 / D, bias=EPS))(s3), reads=[s3], writes=[s3])
            S.op("act", (lambda s3: lambda g: g.activation(out=s3[:, 2:3], in_=s3[:, 1:2], func=AF.Exp, scale=-0.5))(s3), reads=[s3], writes=[s3])
            S.op("dve", (lambda s3, pler: lambda g: g.scalar_tensor_tensor(out=pler[:], in0=pler[:], scalar=s3[:, 2:3], in1=gple[:], op0=ALU.mult, op1=ALU.mult))(s3, pler),
                 reads=[pler, s3, gple], writes=[pler])
            for n in range(4):
                bk = banks[2 + n % 2]
                eg = egs[n % 2]
                for c in range(16):
                    S.op("pe", (lambda c, n, bk, x2T: lambda g: g.matmul(bk[:, :], lhsT=x2T[:, c, :], rhs=wpg[:, c, n * 512:(n + 1) * 512],
                                                                    start=(c == 0), stop=(c == 15)))(c, n, bk, x2T), reads=[x2T, wpg], writes=[bk])
                S.op("act", (lambda bk, eg: lambda g: g.activation(out=eg[:], in_=bk[:, :], func=AF.Exp, scale=-1.0))(bk, eg), reads=[bk], writes=[eg])
                S.op("act", (lambda eg: lambda g: g.activation(out=eg[:], in_=eg[:], func=AF.Ln, bias=1.0))(eg), reads=[eg], writes=[eg])
                S.op("act", (lambda eg: lambda g: g.activation(out=eg[:], in_=eg[:], func=AF.Exp, scale=-1.0))(eg), reads=[eg], writes=[eg])
                S.op("dve", (lambda n, eg, pler: lambda g: g.tensor_tensor(out=eg[:], in0=eg[:], in1=pler[:, n * 512:(n + 1) * 512], op=ALU.mult))(n, eg, pler),
                     reads=[eg, pler], writes=[eg])
                S.op("dve", (lambda n, xm3, eg: lambda g: g.tensor_tensor(out=xm3[:, n * 512:(n + 1) * 512], in0=eg[:], in1=xm3[:, n * 512:(n + 1) * 512], op=ALU.add))(n, xm3, eg),
                     reads=[eg, xm3], writes=[xm3])
            S.op("act", (lambda s3, xm3: lambda g: g.activation(out=junk3[:], in_=xm3[:], func=AF.Square, accum_out=s3[:, 4:5]))(s3, xm3), reads=[xm3], writes=[junk3, s3])
            S.op("act", (lambda s3: lambda g: g.activation(out=s3[:, 5:6], in_=s3[:, 4:5], func=AF.Ln, scale=1.0 / D, bias=EPS))(s3), reads=[s3], writes=[s3])
            S.op("act", (lambda s3: lambda g: g.activation(out=s3[:, 6:7], in_=s3[:, 5:6], func=AF.Exp, scale=-0.5))(s3), reads=[s3], writes=[s3])
            S.op("dve", (lambda s3, xm3: lambda g: g.scalar_tensor_tensor(out=xm3[:], in0=xm3[:], scalar=s3[:, 6:7], in1=gfin[:], op0=ALU.mult, op1=ALU.mult))(s3, xm3),
                 reads=[xm3, s3, gfin], writes=[xm3])
            S.dma("sp", (lambda r0, xm3: lambda g: g.dma_start(out=out_d.ap()[r0:r0 + 128, :], in_=xm3[:]))(r0, xm3), reads=[xm3])
        S.emit()
    return nc


def prep_weights(g_mix, w_in, lb_logits, hg_norm, conv_w, sc_norm, w_out, g_ffn, w_router_group, w_router_expert,
                 w_gate, w_up, w_down, w_ple, g_ple, w_ple_gate, g_final):
    f = np.float32
    w_in = np.asarray(w_in[0], f)
    q, fz, iz, gz = (w_in[:, k * 1024:(k + 1) * 1024].reshape(D, 8, 128) for k in range(4))
    whg = np.ascontiguousarray(np.stack([q, gz, fz, iz], axis=2).transpose(1, 0, 2, 3).reshape(8, D, 512))
    Bw, Cw, Hw = (w_in[:, 4096 + k * 1024:4096 + (k + 1) * 1024].reshape(D, 8, 128) for k in range(3))
    wsc = np.ascontiguousarray(np.stack([Bw, Cw, Hw], axis=2).transpose(1, 0, 2, 3).reshape(8, D, 384))
    wr = np.concatenate([np.asarray(w_router_group[0], f), np.asarray(w_router_expert[0], f)], axis=1)
    wr = np.ascontiguousarray(wr.reshape(16, 128, 36).transpose(1, 0, 2))
    wg = np.asarray(w_gate[0], f).reshape(NEXP, 16, 128, 512).transpose(0, 2, 1, 3)
    wgt = np.ascontiguousarray(wg).reshape(NEXP * 128 * 4, 2048)
    wu = np.asarray(w_up[0], f).reshape(NEXP, 16, 128, 512).transpose(0, 2, 1, 3)
    wut = np.ascontiguousarray(wu).reshape(NEXP * 128 * 4, 2048)
    wd = np.asarray(w_down[0], f).reshape(NEXP, 4, 128, 2048).transpose(0, 2, 1, 3)
    wdt = np.ascontiguousarray(wd).reshape(NEXP * 128 * 4, 2048)
    cols = np.zeros((128, 64), f)
    cols[:, 0:16] = np.asarray(g_mix[0], f).reshape(16, 128).T
    cols[:, 16:32] = np.asarray(g_ffn[0], f).reshape(16, 128).T
    cols[:, 32:40] = np.asarray(sc_norm[0], f).reshape(8, 128).T
    cw = np.asarray(conv_w[0], f).reshape(3, 8, 128)
    cols[:, 40:64] = cw.transpose(2, 1, 0).reshape(128, 24)
    return {
        "whg": whg, "wsc": wsc, "wout": np.ascontiguousarray(np.asarray(w_out[0], f)), "wr": wr,
        "wgt": wgt, "wut": wut, "wdt": wdt,
        "wple": np.ascontiguousarray(np.asarray(w_ple[0], f)), "wpg": np.ascontiguousarray(np.asarray(w_ple_gate[0], f)),
        "lb": np.ascontiguousarray(np.asarray(lb_logits, f)), "hgn": np.asarray(hg_norm, f).reshape(1, 128),
        "gffn": np.asarray(g_ffn, f).reshape(1, D), "gple": np.asarray(g_ple, f).reshape(1, D),
        "gfin": np.asarray(g_final, f).reshape(1, D), "cols": cols,
        "zeros": np.zeros((256, D), ml_dtypes.bfloat16),
    }


_NC_CACHE = {}


def kernel(x, p, g_mix, w_in, lb_logits, hg_norm, conv_w, sc_norm, w_out, g_ffn, w_router_group, w_router_expert,
           w_gate, w_up, w_down, w_ple, g_ple, w_ple_gate, g_final):
    x = np.asarray(x, np.float32)
    p = np.asarray(p, np.float32)
    Bn, T, _ = x.shape
    half = T // 2
    wts = prep_weights(g_mix, w_in, lb_logits, hg_norm, conv_w, sc_norm, w_out, g_ffn, w_router_group, w_router_expert,
                       w_gate, w_up, w_down, w_ple, g_ple, w_ple_gate, g_final)
    if "nc" not in _NC_CACHE:
        _NC_CACHE["nc"] = build(NG=half // 512, NWG=half // 512)
    nc = _NC_CACHE["nc"]
    in_maps = []
    for c in range(8):
        b, hf = c // 2, c % 2
        rows = slice(hf * half, (hf + 1) * half)
        m = dict(wts)
        m["xT"] = np.ascontiguousarray(x[b, rows].T)
        m["xTp"] = np.ascontiguousarray(x[b, 0:half].T) if hf == 1 else np.zeros((D, half), np.float32)
        m["xtok"] = np.ascontiguousarray(x[b, rows])
        m["pT"] = np.ascontiguousarray(p[0, b, rows].T)
        in_maps.append(m)
    res = run_bass_kernel_spmd(nc, in_maps, core_ids=list(range(8)))
    out = np.empty((Bn, T, D), np.float32)
    for c in range(8):
        b, hf = c // 2, c % 2
        out[b, hf * half:(hf + 1) * half] = res.results[c]["out"]
    return out
```
